# Optimizing a Trainium2 kernel written in Bass

```python
import math
import jax, jax.numpy as jnp
from jax import lax
import numpy as np

D_MODEL = 1024
BATCH = 8
SEQ = 2048
DEPTH = 2

N_BRANCH = 3
BRANCH_W = 512
GLA_HEADS = 4
GLA_DK = 64
GLA_DV = BRANCH_W // GLA_HEADS
GLA_RANK = 16
GLA_TAU = 16.0
GLA_CHUNK = 64
S5_GROUP = 16
S5_GROUPS = BRANCH_W // S5_GROUP
S5_STATE = 64
ML_HEADS = 4
ML_DK = 64
ML_DV = BRANCH_W // ML_HEADS
ML_CONV = 4
ML_CHUNK = 64
D_FF = 2816
N_EXPERTS = 8
TOP_K = 2
D_FF_EXPERT = 3584
PLE_DIM = 256
N_DENSE = (DEPTH + 1) // 2
N_MOE = DEPTH // 2
DN_ALPHA = (2.0 * DEPTH) ** 0.25
DN_BETA = (8.0 * DEPTH) ** -0.25
LN_EPS = 1e-5

IN_WIDTHS = (
    GLA_HEADS * GLA_DK, GLA_HEADS * GLA_DK, BRANCH_W, GLA_RANK, BRANCH_W,
    BRANCH_W,
    ML_HEADS * ML_DK, ML_HEADS * ML_DK, BRANCH_W, ML_HEADS, ML_HEADS, BRANCH_W,
    N_BRANCH * D_MODEL,
)
D_IN = int(sum(IN_WIDTHS))
IN_OFFSETS = tuple(int(o) for o in np.cumsum(IN_WIDTHS)[:-1])
ML_F_START = IN_OFFSETS[9]

kernel_name = 'hybrid_gla_s5_mlstm_moe_deepnorm'


def layer_norm(x, g, b):
    xf = x.astype(jnp.float32)
    mu = jnp.mean(xf, axis=-1, keepdims=True)
    var = jnp.mean(jnp.square(xf - mu), axis=-1, keepdims=True)
    y = (xf - mu) * lax.rsqrt(var + LN_EPS) * g.astype(jnp.float32) + b.astype(jnp.float32)
    return y.astype(x.dtype)


def head_norm(t, g):
    mu = jnp.mean(t, axis=-1, keepdims=True)
    var = jnp.mean(jnp.square(t - mu), axis=-1, keepdims=True)
    b, s, h, d = t.shape
    return ((t - mu) * lax.rsqrt(var + LN_EPS)).reshape(b, s, h * d) * g.astype(jnp.float32)


def to_chunks(t, heads, chunk):
    b, s, w = t.shape
    return t.reshape(b, s // chunk, chunk, heads, w // heads).transpose(1, 0, 3, 2, 4)


def gate_chunks(t, chunk):
    b, s, h = t.shape
    return t.reshape(b, s // chunk, chunk, h).transpose(1, 0, 3, 2)


def from_chunks(t):
    nc, b, h, l, d = t.shape
    return t.transpose(1, 0, 3, 2, 4).reshape(b, nc * l, h, d)


def causal_conv_silu(t, w, b):
    k_w = w.shape[0]
    s = t.shape[1]
    tp = jnp.pad(t, ((0, 0), (k_w - 1, 0), (0, 0)))
    y = b.astype(jnp.float32)
    for j in range(k_w):
        y = y + tp[:, j:j + s] * w[j].astype(jnp.float32)
    return jax.nn.silu(y)


def gla_mixer(q, k, v, a_lr, g, w_a2, b_a2, norm_g):
    f32 = jnp.float32
    bsz = q.shape[0]
    log_a = jax.nn.log_sigmoid(a_lr.astype(f32) @ w_a2.astype(f32) + b_a2.astype(f32)) / GLA_TAU
    qc = to_chunks(q.astype(f32) * GLA_DK ** -0.5, GLA_HEADS, GLA_CHUNK)
    kc = to_chunks(k.astype(f32), GLA_HEADS, GLA_CHUNK)
    vc = to_chunks(v.astype(f32), GLA_HEADS, GLA_CHUNK)
    ac = to_chunks(log_a, GLA_HEADS, GLA_CHUNK)
    causal = jnp.tril(jnp.ones((GLA_CHUNK, GLA_CHUNK), dtype=bool))

    def step(state, blk):
        qb, kb, vb, ab = blk
        cum = jnp.cumsum(ab, axis=2)
        last = cum[:, :, -1, :]
        q_dec = qb * jnp.exp(cum)
        k_inv = kb * jnp.exp(-cum)
        att = jnp.where(causal, jnp.einsum('bhld,bhmd->bhlm', q_dec, k_inv), 0.0)
        out = jnp.einsum('bhlm,bhme->bhle', att, vb) + jnp.einsum('bhld,bhde->bhle', q_dec, state)
        k_tail = kb * jnp.exp(last[:, :, None, :] - cum)
        state = jnp.exp(last)[..., None] * state + jnp.einsum('bhld,bhle->bhde', k_tail, vb)
        return state, out

    s0 = jnp.zeros((bsz, GLA_HEADS, GLA_DK, GLA_DV), f32)
    _, o = lax.scan(step, s0, (qc, kc, vc, ac))
    o = head_norm(from_chunks(o), norm_g)
    return o * jax.nn.silu(g.astype(f32))


def s5_mixer(u, a_re, a_im, log_dt, b_re, b_im, c_re, c_im, d_skip, w_glu, b_glu):
    f32 = jnp.float32
    bsz, s, _ = u.shape
    ug = u.astype(f32).reshape(bsz, s, S5_GROUPS, S5_GROUP)
    lam = lax.complex(a_re.astype(f32), a_im.astype(f32))
    dt = jnp.exp(log_dt.astype(f32))[:, None]
    lam_bar = jnp.exp(lam * dt)
    b_c = lax.complex(b_re.astype(f32), b_im.astype(f32))
    b_bar = ((lam_bar - 1.0) / lam)[..., None] * b_c
    bu = jnp.einsum('gpn,bsgn->bsgp', b_bar, ug.astype(jnp.complex64))
    a_seq = jnp.broadcast_to(lam_bar, bu.shape)

    def combine(e1, e2):
        a1, x1 = e1
        a2, x2 = e2
        return a1 * a2, a2 * x1 + x2

    _, states = lax.associative_scan(combine, (a_seq, bu), axis=1)
    c_c = lax.complex(c_re.astype(f32), c_im.astype(f32))
    y = jnp.einsum('gnp,bsgp->bsgn', c_c, states).real + d_skip.astype(f32).reshape(S5_GROUPS, S5_GROUP) * ug
    y = jax.nn.gelu(y.reshape(bsz, s, BRANCH_W))
    return y * jax.nn.sigmoid(y @ w_glu.astype(f32) + b_glu.astype(f32))


def mlstm_mixer(q, k, v, i_pre, f_pre, o_pre, conv_w, conv_b, norm_g):
    f32 = jnp.float32
    bsz = q.shape[0]
    qk = causal_conv_silu(jnp.concatenate([q, k], axis=-1).astype(f32), conv_w, conv_b)
    qf, kf = jnp.split(qk, 2, axis=-1)
    qc = to_chunks(qf, ML_HEADS, ML_CHUNK)
    kc = to_chunks(kf * ML_DK ** -0.5, ML_HEADS, ML_CHUNK)
    vc = to_chunks(v.astype(f32), ML_HEADS, ML_CHUNK)
    ic = gate_chunks(i_pre.astype(f32), ML_CHUNK)
    fc = gate_chunks(jax.nn.log_sigmoid(f_pre.astype(f32)), ML_CHUNK)
    causal = jnp.tril(jnp.ones((ML_CHUNK, ML_CHUNK), dtype=bool))

    def step(carry, blk):
        c_st, n_st, m_st = carry
        qb, kb, vb, ib, lfb = blk
        bcum = jnp.cumsum(lfb, axis=-1)
        dmat = jnp.where(causal, bcum[..., :, None] - bcum[..., None, :] + ib[..., None, :], -jnp.inf)
        inter = bcum + m_st[..., None]
        m_row = jnp.maximum(inter, jnp.max(dmat, axis=-1))
        wts = jnp.exp(dmat - m_row[..., None])
        sc = jnp.einsum('bhld,bhmd->bhlm', qb, kb) * wts
        w_inter = jnp.exp(inter - m_row)
        num = jnp.einsum('bhlm,bhme->bhle', sc, vb) + w_inter[..., None] * jnp.einsum('bhld,bhde->bhle', qb, c_st)
        den = jnp.sum(sc, axis=-1) + w_inter * jnp.einsum('bhld,bhd->bhl', qb, n_st)
        h = num / jnp.maximum(jnp.abs(den), jnp.exp(-m_row))[..., None]
        g_tot = bcum[..., -1]
        tail = g_tot[..., None] - bcum + ib
        m_new = jnp.maximum(g_tot + m_st, jnp.max(tail, axis=-1))
        wt = jnp.exp(tail - m_new[..., None])
        decay = jnp.exp(g_tot + m_st - m_new)
        c_st = decay[..., None, None] * c_st + jnp.einsum('bhl,bhld,bhle->bhde', wt, kb, vb)
        n_st = decay[..., None] * n_st + jnp.einsum('bhl,bhld->bhd', wt, kb)
        return (c_st, n_st, m_new), h

    init = (jnp.zeros((bsz, ML_HEADS, ML_DK, ML_DV), f32),
            jnp.zeros((bsz, ML_HEADS, ML_DK), f32),
            jnp.zeros((bsz, ML_HEADS), f32))
    _, h = lax.scan(step, init, (qc, kc, vc, ic, fc))
    h = head_norm(from_chunks(h), norm_g)
    return h * jax.nn.sigmoid(o_pre.astype(f32))


def swiglu(x, w_gate, w_up, w_down):
    return (jax.nn.silu(x @ w_gate) * (x @ w_up)) @ w_down


def moe_swiglu(x, w_router, b_router, w_gate, w_up, w_down):
    logits = (x @ w_router + b_router).astype(jnp.float32)
    top_v, top_i = lax.top_k(logits, TOP_K)
    probs = jax.nn.softmax(top_v, axis=-1)
    comb = jnp.sum(jax.nn.one_hot(top_i, N_EXPERTS, dtype=jnp.float32) * probs[..., None], axis=-2)
    out = jnp.zeros_like(x)
    for e in range(N_EXPERTS):
        out = out + comb[..., e:e + 1].astype(x.dtype) * swiglu(x, w_gate[e], w_up[e], w_down[e])
    return out


def setup_inputs(seed: int = 0) -> dict:
    key = jax.random.key(seed)
    ks = iter(jax.random.split(key, 48))
    f32 = jnp.float32
    L = DEPTH

    def nrm(shape, scale):
        return jax.random.normal(next(ks), shape, f32) * scale

    def gain(shape):
        return 1.0 + nrm(shape, 0.02)

    x = nrm((BATCH, SEQ, D_MODEL), 1.0)
    p = nrm((DEPTH, BATCH, SEQ, PLE_DIM), 1.0)
    w_in = nrm((L, D_MODEL, D_IN), D_MODEL ** -0.5)
    b_in = nrm((L, D_IN), 0.02)
    b_in = b_in.at[:, ML_F_START:ML_F_START + ML_HEADS].add(jnp.linspace(3.0, 6.0, ML_HEADS, dtype=f32))
    gla_w_a2 = nrm((L, GLA_RANK, GLA_HEADS * GLA_DK), GLA_RANK ** -0.5)
    gla_b_a2 = nrm((L, GLA_HEADS * GLA_DK), 0.1)
    gla_norm_g = gain((L, BRANCH_W))
    s5_a_re = -0.5 + nrm((L, S5_GROUPS, S5_STATE), 0.01)
    s5_a_im = math.pi * jnp.arange(S5_STATE, dtype=f32) + nrm((L, S5_GROUPS, S5_STATE), 0.01)
    s5_log_dt = jax.random.uniform(next(ks), (L, S5_GROUPS), f32, math.log(1e-3), math.log(1e-1))
    s5_b_re = nrm((L, S5_GROUPS, S5_STATE, S5_GROUP), (2.0 * S5_GROUP) ** -0.5)
    s5_b_im = nrm((L, S5_GROUPS, S5_STATE, S5_GROUP), (2.0 * S5_GROUP) ** -0.5)
    s5_c_re = nrm((L, S5_GROUPS, S5_GROUP, S5_STATE), S5_STATE ** -0.5)
    s5_c_im = nrm((L, S5_GROUPS, S5_GROUP, S5_STATE), S5_STATE ** -0.5)
    s5_d = nrm((L, BRANCH_W), 1.0)
    s5_w_glu = nrm((L, BRANCH_W, BRANCH_W), BRANCH_W ** -0.5)
    s5_b_glu = nrm((L, BRANCH_W), 0.02)
    ml_conv_w = nrm((L, ML_CONV, 2 * ML_HEADS * ML_DK), ML_CONV ** -0.5)
    ml_conv_b = nrm((L, 2 * ML_HEADS * ML_DK), 0.02)
    ml_norm_g = gain((L, BRANCH_W))
    w_up = nrm((L, N_BRANCH, BRANCH_W, D_MODEL), BRANCH_W ** -0.5)
    w_o = nrm((L, D_MODEL, D_MODEL), DN_BETA * D_MODEL ** -0.5)
    ln1_g = gain((L, D_MODEL))
    ln1_b = nrm((L, D_MODEL), 0.02)
    ffn_wg = nrm((N_DENSE, D_MODEL, D_FF), D_MODEL ** -0.5)
    ffn_wu = nrm((N_DENSE, D_MODEL, D_FF), D_MODEL ** -0.5)
    ffn_wd = nrm((N_DENSE, D_FF, D_MODEL), DN_BETA * D_FF ** -0.5)
    moe_router = nrm((N_MOE, D_MODEL, N_EXPERTS), D_MODEL ** -0.5)
    moe_router_b = nrm((N_MOE, N_EXPERTS), 0.01)
    moe_wg = nrm((N_MOE, N_EXPERTS, D_MODEL, D_FF_EXPERT), D_MODEL ** -0.5)
    moe_wu = nrm((N_MOE, N_EXPERTS, D_MODEL, D_FF_EXPERT), D_MODEL ** -0.5)
    moe_wd = nrm((N_MOE, N_EXPERTS, D_FF_EXPERT, D_MODEL), DN_BETA * D_FF_EXPERT ** -0.5)
    ple_w_gate = nrm((L, D_MODEL, D_MODEL), D_MODEL ** -0.5)
    ple_w_proj = nrm((L, PLE_DIM, D_MODEL), DN_BETA * PLE_DIM ** -0.5)
    ln2_g = gain((L, D_MODEL))
    ln2_b = nrm((L, D_MODEL), 0.02)
    return {
        'x': x, 'p': p, 'w_in': w_in, 'b_in': b_in,
        'gla_w_a2': gla_w_a2, 'gla_b_a2': gla_b_a2, 'gla_norm_g': gla_norm_g,
        's5_a_re': s5_a_re, 's5_a_im': s5_a_im, 's5_log_dt': s5_log_dt,
        's5_b_re': s5_b_re, 's5_b_im': s5_b_im, 's5_c_re': s5_c_re, 's5_c_im': s5_c_im,
        's5_d': s5_d, 's5_w_glu': s5_w_glu, 's5_b_glu': s5_b_glu,
        'ml_conv_w': ml_conv_w, 'ml_conv_b': ml_conv_b, 'ml_norm_g': ml_norm_g,
        'w_up': w_up, 'w_o': w_o, 'ln1_g': ln1_g, 'ln1_b': ln1_b,
        'ffn_wg': ffn_wg, 'ffn_wu': ffn_wu, 'ffn_wd': ffn_wd,
        'moe_router': moe_router, 'moe_router_b': moe_router_b,
        'moe_wg': moe_wg, 'moe_wu': moe_wu, 'moe_wd': moe_wd,
        'ple_w_gate': ple_w_gate, 'ple_w_proj': ple_w_proj, 'ln2_g': ln2_g, 'ln2_b': ln2_b,
    }


def reference(x, p, w_in, b_in, gla_w_a2, gla_b_a2, gla_norm_g,
              s5_a_re, s5_a_im, s5_log_dt, s5_b_re, s5_b_im, s5_c_re, s5_c_im,
              s5_d, s5_w_glu, s5_b_glu, ml_conv_w, ml_conv_b, ml_norm_g,
              w_up, w_o, ln1_g, ln1_b, ffn_wg, ffn_wu, ffn_wd,
              moe_router, moe_router_b, moe_wg, moe_wu, moe_wd,
              ple_w_gate, ple_w_proj, ln2_g, ln2_b):
    bsz, s, _ = x.shape
    for i in range(DEPTH):
        h = x @ w_in[i] + b_in[i]
        (g_q, g_k, g_v, g_a, g_g, s_u, m_q, m_k, m_v, m_i, m_f, m_o, gate_pre) = jnp.split(h, IN_OFFSETS, axis=-1)
        y_gla = gla_mixer(g_q, g_k, g_v, g_a, g_g, gla_w_a2[i], gla_b_a2[i], gla_norm_g[i])
        y_s5 = s5_mixer(s_u, s5_a_re[i], s5_a_im[i], s5_log_dt[i], s5_b_re[i], s5_b_im[i],
                        s5_c_re[i], s5_c_im[i], s5_d[i], s5_w_glu[i], s5_b_glu[i])
        y_ml = mlstm_mixer(m_q, m_k, m_v, m_i, m_f, m_o, ml_conv_w[i], ml_conv_b[i], ml_norm_g[i])
        ys = jnp.stack([y_gla, y_s5, y_ml], axis=2).astype(x.dtype)
        ups = jnp.einsum('bsrc,rcd->bsrd', ys, w_up[i])
        gates = jax.nn.sigmoid(gate_pre).reshape(bsz, s, N_BRANCH, D_MODEL)
        mix = jnp.einsum('bsrd,bsrd->bsd', gates, ups) @ w_o[i]
        x = layer_norm(DN_ALPHA * x + mix, ln1_g[i], ln1_b[i])
        if i % 2 == 0:
            j = i // 2
            f = swiglu(x, ffn_wg[j], ffn_wu[j], ffn_wd[j])
        else:
            j = i // 2
            f = moe_swiglu(x, moe_router[j], moe_router_b[j], moe_wg[j], moe_wu[j], moe_wd[j])
        ple = jax.nn.sigmoid(x @ ple_w_gate[i]) * (p[i] @ ple_w_proj[i])
        x = layer_norm(DN_ALPHA * x + f + ple, ln2_g[i], ln2_b[i])
    return x
```

```python
import numpy as np
from contextlib import ExitStack
import concourse.bass as bass
import concourse.mybir as mybir
from concourse.bass_utils import run_bass_kernel_spmd

dt = mybir.dt
F32, BF16, I32 = dt.float32, dt.bfloat16, dt.int32
AF = mybir.ActivationFunctionType
ALU = mybir.AluOpType
AX = mybir.AxisListType

SPARSE_PE_INC = False
SAME_ENGINE_SYNC = True


class Region:
    __slots__ = ("name", "writer", "readers", "dma_sem", "dma_n", "dma_base", "uid", "excl", "dma_q")
    _next = [0]

    def __init__(self, name):
        self.name = name
        self.writer = None
        self.readers = {}
        self.dma_sem = None
        self.dma_n = 0
        self.dma_base = 0
        self.excl = False
        Region._next[0] += 1
        self.uid = Region._next[0]


class V:
    __slots__ = ("ap", "regs")

    def __init__(self, ap, regs):
        self.ap = ap
        self.regs = regs

    def __getitem__(self, idx):
        return V(self.ap[idx], self.regs)

    def bitcast(self, d):
        return V(self.ap.bitcast(d), self.regs)

    def bcast(self, shape):
        return V(self.ap.broadcast_to(shape), self.regs)


class Tile:
    _n = [0]

    def __init__(self, prog, name, shape, dtype, space="sbuf", nreg=1):
        Tile._n[0] += 1
        name = "%s_%d" % (name, Tile._n[0])
        self.name = name
        self.shape = shape
        if space == "sbuf":
            self.t = prog.es.enter_context(prog.nc.sbuf_tensor(name, shape, dtype))
        elif space == "psum":
            self.t = prog.es.enter_context(prog.nc.psum_tensor(name, shape, dtype))
        else:
            self.t = space
        self.regs = [Region(f"{name}.{i}") for i in range(nreg)]
        if space == "psum":
            for r in self.regs:
                r.excl = True
        prog.scope_regions[-1].extend(self.regs)

    def __getitem__(self, idx):
        return V(self.t[idx], self.regs)

    def sub(self, j):
        return V(self.t[:, j], [self.regs[j]])

    def reg(self, j, idx):
        return V(self.t[idx], [self.regs[j]])


ENG = ("pe", "act", "dve", "pool", "sp")


class Prog:
    def __init__(self, nc, es):
        self.nc = nc
        self.es = es
        self.eng = {"pe": nc.tensor, "act": nc.scalar, "dve": nc.vector, "pool": nc.gpsimd, "sp": nc.sync}
        self.sem = {e: es.enter_context(nc.semaphore("sem_" + e)) for e in ENG}
        self.cnt = {e: 0 for e in ENG}
        self.waited = {e: {f: 0 for f in ENG} for e in ENG}
        self.dma_waited = {e: {} for e in ENG}
        self.es0 = es
        self.scope_regions = [[]]
        self.sem_pool = {'pool': [], 'sp': [], 'act': []}
        self.n_dsem = 0
        self.dma_regions = []
        self.n_ops = 0
        self.n_waits = 0
        self.mute = None

    def _collect(self, e, reads, writes):
        deps = {}
        dmadeps = {}

        def add(w):
            if w is None:
                return
            if w[0] == "dma":
                r, n = w[1], w[2]
                k = r.uid
                if k not in dmadeps or dmadeps[k][1] < n:
                    dmadeps[k] = (r, n)
            else:
                f, i = w
                if f == e and (e == "pe" or not SAME_ENGINE_SYNC):
                    return
                if deps.get(f, 0) < i:
                    deps[f] = i

        for v in reads:
            for r in v.regs:
                add(r.writer)
                if r.excl:
                    for k, val in r.readers.items():
                        if not isinstance(k, tuple) and k != e:
                            add((k, val))
        for v in writes:
            for r in v.regs:
                add(r.writer)
                for k, val in r.readers.items():
                    if isinstance(k, tuple):
                        add(("dma", val[0], val[1]))
                    else:
                        add((k, val))
        return deps, dmadeps

    def _emit_waits(self, e, deps, dmadeps):
        eng = self.eng[e]
        for f, i in deps.items():
            if self.waited[e][f] >= i:
                continue
            if f == e and i < self.cnt[e]:
                pass
            eng.wait_ge(self.sem[f], i)
            self.n_waits += 1
            self.waited[e][f] = i
        for k, (r, n) in dmadeps.items():
            if self.dma_waited[e].get(k, 0) >= n:
                continue
            eng.wait_ge(r.dma_sem, r.dma_base + 16 * n)
            self.n_waits += 1
            self.dma_waited[e][k] = n

    def op(self, e, fn, reads=(), writes=(), inc=True):
        if self.mute is not None and self.mute[0]:
            return None
        deps, dmadeps = self._collect(e, reads, writes)
        self._emit_waits(e, deps, dmadeps)
        inst = fn(self.eng[e])
        if inc:
            self.cnt[e] += 1
            n = self.cnt[e]
            inst.then_inc(self.sem[e], 1)
        else:
            n = self.cnt[e] + 1
        for v in reads:
            for r in v.regs:
                if r.readers.get(e, 0) < n:
                    r.readers[e] = n
        for v in writes:
            for r in v.regs:
                r.writer = (e, n)
                r.readers = {}
        self.n_ops += 1
        return inst

    def dma(self, q, out, in_, sb, **kw):
        if self.mute is not None and self.mute[0]:
            return None
        deps, dmadeps = self._collect(q, [in_], [out])
        self._emit_waits(q, deps, dmadeps)
        r0 = sb.regs[0]
        if r0.dma_sem is None:
            r0.dma_q = q
            if self.sem_pool[q]:
                r0.dma_sem, r0.dma_base = self.sem_pool[q].pop()
            else:
                r0.dma_sem = self.es0.enter_context(self.nc.semaphore("dsem%d" % self.n_dsem))
                self.n_dsem += 1
                r0.dma_base = 0
            self.dma_regions.append(r0)
        assert r0.dma_q == q, (r0.name, r0.dma_q, q)
        inst = self.eng[q].dma_start(out=out.ap, in_=in_.ap, **kw)
        inst.then_inc(r0.dma_sem, 16)
        r0.dma_n += 1
        n = r0.dma_n
        for r in in_.regs:
            r.readers[("dma", r0.uid)] = (r0, n)
        for r in out.regs:
            r.writer = ("dma", r0, n)
            r.readers = {}
        return inst

    def push_scope(self):
        self.scope_regions.append([])

    def pop_scope(self):
        for r in self.scope_regions.pop():
            if r.dma_sem is not None:
                self.sem_pool[r.dma_q].append((r.dma_sem, r.dma_base + 16 * r.dma_n))
                self.dma_regions.remove(r)
                r.dma_sem = None

    def barrier(self):
        for e in ENG:
            deps = {f: self.cnt[f] for f in ENG if (f != e or e != 'pe') and self.cnt[f] > 0}
            dmadeps = {r.uid: (r, r.dma_n) for r in self.dma_regions}
            self._emit_waits(e, deps, dmadeps)

    def mm(self, out, lhsT, rhs, start=True, stop=True, **kw):
        return self.op("pe", lambda g: g.matmul(out.ap, lhsT.ap, rhs.ap, start=start, stop=stop, **kw),
                       reads=[lhsT, rhs] + ([] if start else [out]), writes=[out], inc=(stop or not SPARSE_PE_INC))

    def tr(self, out, in_, ident):
        return self.op("pe", lambda g: g.transpose(out.ap, in_.ap, ident.ap), reads=[in_, ident], writes=[out])

    def act(self, out, in_, func, bias=None, scale=None, accum=None, e="act"):
        kw = {}
        reads = [in_]
        if bias is not None:
            if isinstance(bias, V):
                kw["bias"] = bias.ap
                reads.append(bias)
            else:
                kw["bias"] = bias
        if scale is not None:
            if isinstance(scale, V):
                kw["scale"] = scale.ap
                reads.append(scale)
            else:
                kw["scale"] = scale
        writes = [out]
        if accum is not None:
            kw["accum_out"] = accum.ap
            writes.append(accum)
        return self.op(e, lambda g: g.activation(out.ap, in_.ap, func, **kw), reads=reads, writes=writes)

    def tt(self, out, a, b, op, e="dve"):
        return self.op(e, lambda g: g.tensor_tensor(out.ap, a.ap, b.ap, op), reads=[a, b], writes=[out])

    def ts(self, out, a, s1, s2, op0, op1=None, e="dve", accum=None):
        reads = [a]
        s1a, s2a = s1, s2
        if isinstance(s1, V):
            reads.append(s1)
            s1a = s1.ap
        if isinstance(s2, V):
            reads.append(s2)
            s2a = s2.ap
        kw = {}
        writes = [out]
        if accum is not None:
            kw["accum_out"] = accum.ap
            writes.append(accum)
        if op1 is None:
            return self.op(e, lambda g: g.tensor_scalar(out.ap, a.ap, s1a, s2a, op0, **kw), reads=reads, writes=writes)
        return self.op(e, lambda g: g.tensor_scalar(out.ap, a.ap, s1a, s2a, op0, op1, **kw), reads=reads, writes=writes)

    def stt(self, out, a, s, b, op0, op1, e="dve", accum=None):
        reads = [a, b]
        sa = s
        if isinstance(s, V):
            reads.append(s)
            sa = s.ap
        kw = {}
        writes = [out]
        if accum is not None:
            kw["accum_out"] = accum.ap
            writes.append(accum)
        return self.op(e, lambda g: g.scalar_tensor_tensor(out.ap, a.ap, sa, b.ap, op0, op1, **kw), reads=reads, writes=writes)

    def copy(self, out, in_, e="dve"):
        if e == "act":
            return self.op(e, lambda g: g.copy(out.ap, in_.ap), reads=[in_], writes=[out])
        return self.op(e, lambda g: g.tensor_copy(out.ap, in_.ap), reads=[in_], writes=[out])

    def memset(self, out, val, e="pool"):
        return self.op(e, lambda g: g.memset(out.ap, val), reads=[], writes=[out])

    def reduce(self, out, in_, op, axis=AX.X, e="dve", **kw):
        return self.op(e, lambda g: g.tensor_reduce(out.ap, in_.ap, axis, op, **kw), reads=[in_], writes=[out])

    def recip(self, out, in_, e="dve"):
        return self.op(e, lambda g: g.reciprocal(out.ap, in_.ap), reads=[in_], writes=[out])

    def finish(self):
        self.barrier()


SEQ = 2048
DM = 1024
NT = 16
DEPTH = 2
import os
NCHUNK = int(os.environ.get('NCHUNK', '16'))
S5_STOP = int(os.environ.get('S5_STOP', '99'))
GLA_STOP = int(os.environ.get('GLA_STOP', '0'))
S5_ORDER = int(os.environ.get('S5_ORDER', '0'))
GLA_VAR = int(os.environ.get('GLA_VAR', '0'))
PHASES = os.environ.get('PHASES', 'gla,s5,ml').split(',')
OFF = dict(gq=0, gk=256, gv=512, ga=1024, gg=1040, su=1552, mq=2064, mk=2320, mv=2576, mi=3088, mf=3092, mo=3096, gate=3608, end=6680)
DN_ALPHA = (2.0 * DEPTH) ** 0.25
LN_EPS = 1e-5

INPUT_SHAPES = {
    'x': (SEQ, DM), 'p': (2, SEQ, 256), 'w_in': (2, 1024, 6680), 'b_in': (2, 6680), 'gla_w_a2': (2, 16, 256),
    'gla_b_a2': (2, 256), 'gla_norm_g': (2, 512), 's5_a_re': (2, 32, 64), 's5_a_im': (2, 32, 64), 's5_log_dt': (2, 32),
    's5_b_re': (2, 32, 64, 16), 's5_b_im': (2, 32, 64, 16), 's5_c_re': (2, 32, 16, 64), 's5_c_im': (2, 32, 16, 64),
    's5_d': (2, 512), 's5_w_glu': (2, 512, 512), 's5_b_glu': (2, 512), 'ml_conv_w': (2, 4, 512), 'ml_conv_b': (2, 512),
    'ml_norm_g': (2, 512), 'w_up': (2, 3, 512, 1024), 'w_o': (2, 1024, 1024), 'ln1_g': (2, 1024), 'ln1_b': (2, 1024),
    'ffn_wg': (1, 1024, 2816), 'ffn_wu': (1, 1024, 2816), 'ffn_wd': (1, 2816, 1024), 'moe_router': (1, 1024, 8),
    'moe_router_b': (1, 8), 'moe_wg': (1, 8, 1024, 3584), 'moe_wu': (1, 8, 1024, 3584), 'moe_wd': (1, 8, 3584, 1024),
    'ple_w_gate': (2, 1024, 1024), 'ple_w_proj': (2, 256, 1024), 'ln2_g': (2, 1024), 'ln2_b': (2, 1024),
}


class Scope:
    def __init__(self, P):
        self.P = P

    def __enter__(self):
        self.es = ExitStack()
        self.es.__enter__()
        self.saved = self.P.es
        self.P.es = self.es
        self.P.push_scope()
        return self

    def __exit__(self, *a):
        self.P.barrier()
        self.P.pop_scope()
        self.P.es = self.saved
        return self.es.__exit__(*a)


def D(ap):
    return V(ap, [])


class K:
    pass


def setup_consts(P, k):
    k.ones = Tile(P, "ones", [128, 128], F32)
    k.ident = Tile(P, "ident", [128, 128], F32)
    k.identb = Tile(P, "identb", [128, 128], BF16)
    k.onesb = Tile(P, "onesb", [1, 512], BF16)
    k.mask01 = Tile(P, "mask01", [128, 128], F32)
    k.mask01b = Tile(P, "mask01b", [128, 128], BF16)
    k.triu_s = Tile(P, "triu_s", [128, 128], F32)
    k.tril_s = Tile(P, "tril_s", [128, 128], F32)
    P.memset(k.ones[:], 1.0)
    P.op("pool", lambda g: g.affine_select(k.ident[:].ap, k.ones[:, 0:128].ap, [[-1, 128]], ALU.is_equal, 0.0,
                                           base=0, channel_multiplier=1), reads=[k.ones[:]], writes=[k.ident[:]])
    P.copy(k.identb[:], k.ident[:], e="pool")
    P.memset(k.onesb[:], 1.0)
    P.op("pool", lambda g: g.affine_select(k.mask01[:].ap, k.ones[:, 0:128].ap, [[1, 128]], ALU.is_ge, 0.0,
                                           base=0, channel_multiplier=-1), reads=[k.ones[:]], writes=[k.mask01[:]])
    P.copy(k.mask01b[:], k.mask01[:], e="pool")
    P.ts(k.triu_s[:], k.mask01[:], -1.0 / 16, None, ALU.mult, e="pool")
    P.ts(k.tril_s[:], k.mask01[:], 1.0 / 16, -1.0 / 16, ALU.mult, ALU.add, e="pool")
    k.ps = [Tile(P, f"ps{i}", [128, 512], F32, space="psum") for i in range(8)]


def load_x(P, k, x_dram):
    for j in range(NT):
        P.dma("sp", k.xres.sub(j), D(x_dram[j * 128:(j + 1) * 128, :]), k.xres.sub(j))
        make_xT(P, k, j)


def make_xT(P, k, j, xtf=None, banks=(0, 1)):
    for half in range(2):
        pst = k.ps[banks[half]]
        for q in range(4):
            kt = half * 4 + q
            P.tr(pst[:, q * 128:(q + 1) * 128], k.xres.sub(j)[:, kt * 128:(kt + 1) * 128], k.ident[:])
        dst = k.xT.reg(j, (slice(None), slice(half * 4, half * 4 + 4), slice(j * 128, (j + 1) * 128)))
        src = pst[:].ap.rearrange("p (q t) -> p q t", q=4)
        srcv = V(src, pst.regs)
        P.copy(dst, srcv, e="act" if half == 0 else "dve")
        if xtf is not None:
            P.copy(xtf[:, half * 4:half * 4 + 4, :], srcv, e="dve" if half == 0 else "act")


def proj_fm(P, k, ps_out, W, wc, brow, bc, tsl, n):
    P.mm(ps_out, brow[0:1, bc], k.ones[0:1, 0:n], start=True, stop=False)
    for kt in range(8):
        P.mm(ps_out, W[:, kt, wc], k.xT[:, kt, tsl], start=False, stop=(kt == 7))


def proj_tm(P, k, ps_out, W, wc, brow, bc, tsl):
    P.mm(ps_out, k.ones[0:1, 0:128], brow[0:1, bc], start=True, stop=False)
    for kt in range(8):
        P.mm(ps_out, k.xT[:, kt, tsl], W[:, kt, wc], start=False, stop=(kt == 7))


def rstd_from_var(P, out, var, eps, tmp):
    P.ts(tmp, var, eps, None, ALU.add)
    P.act(tmp, tmp, AF.Ln)
    P.act(out, tmp, AF.Exp, scale=-0.5)


def gla_phase(P, k, L, w):
    ps = k.ps
    with Scope(P):
        WG = Tile(P, "WG", [128, 8, 1552], BF16)
        brow = Tile(P, "g_brow", [1, 1552], BF16)
        wa2 = Tile(P, "wa2", [16, 256], F32)
        ba2 = Tile(P, "ba2", [1, 256], F32)
        ng = Tile(P, "g_ng", [128, 512], F32)
        P.dma("pool", WG[:], D(w['w_in'][L, :, 0:1552].rearrange("(kt p) n -> p kt n", p=128)), WG[:])
        P.dma("pool", brow[:], D(w['b_in'][L:L + 1, 0:1552]), brow[:])
        P.dma("sp", wa2[:], D(w['gla_w_a2'][L]), wa2[:])
        P.dma("sp", ba2[:], D(w['gla_b_a2'][L:L + 1, :]), ba2[:])
        P.dma("sp", ng[:], D(w['gla_norm_g'][L].partition_broadcast(128)), ng[:])
        qk_raw = Tile(P, "g_qkraw", [128, 4, 512], BF16)
        alr_all = Tile(P, "g_alr", [16, 512], F32)
        sp = Tile(P, "g_sp", [128, 256], F32)
        eT = Tile(P, "g_eT", [128, 2, 128], F32)
        einvT = Tile(P, "g_einvT", [128, 2, 128], F32)
        erev = Tile(P, "g_erev", [128, 256], F32)
        dec = [Tile(P, f"g_dec{i}", [128, 2], F32) for i in range(2)]
        qdT = [Tile(P, f"g_qdT{i}", [128, 2, 128], BF16) for i in range(2)]
        kiT = [Tile(P, f"g_kiT{i}", [128, 2, 128], BF16) for i in range(2)]
        ktail = [Tile(P, f"g_ktail{i}", [128, 256], BF16) for i in range(2)]
        vbf = [Tile(P, f"g_vbf{i}", [128, 512], BF16) for i in range(2)]
        sg = [Tile(P, f"g_sg{i}", [128, 512], BF16) for i in range(2)]
        sgf = Tile(P, "g_sgf", [128, 512], F32)
        attT = Tile(P, "g_attT", [128, 4, 128], BF16)
        S32 = Tile(P, "g_S32", [128, 2, 128], F32)
        Sbf = Tile(P, "g_Sbf", [128, 2, 128], BF16)
        st = Tile(P, "g_st", [128, 4, 6], F32)
        mv = Tile(P, "g_mv", [128, 4, 2], F32)
        rstd = Tile(P, "g_rstd", [128, 4], F32)
        tmp4 = Tile(P, "g_tmp4", [128, 4], F32)
        yn = Tile(P, "g_yn", [128, 512], F32)
        ybf = Tile(P, "g_ybf", [128, 512], BF16)
        P.memset(S32[:], 0.0)
        P.memset(Sbf[:], 0.0)

        def bias_fm(ps_out, bc, n):
            P.mm(ps_out, brow[0:1, bc], k.onesb[0:1, 0:n], start=True, stop=False)

        def bias_tm(ps_out, bc):
            P.mm(ps_out, k.onesb[0:1, 0:128], brow[0:1, bc], start=True, stop=False)

        def stage_a(c):
            par = c % 2
            tsl = slice(c * 128, (c + 1) * 128)
            off = (c % 4) * 128
            osl = slice(off, off + 128)
            if c % 4 == 0:
                t4 = slice(c * 128, c * 128 + 512)
                for i in range(4):
                    cols = slice(i * 128, (i + 1) * 128)
                    bias_fm(ps[0][:], cols, 512)
                    for kt in range(8):
                        P.mm(ps[0][:], WG[:, kt, cols], k.xT[:, kt, t4], start=False, stop=(kt == 7))
                    P.copy(qk_raw[:, i, :], ps[0][:], e="act")
                ac = slice(OFF['ga'], OFF['ga'] + 16)
                bias_fm(ps[0][0:16, :], ac, 512)
                for kt in range(8):
                    P.mm(ps[0][0:16, :], WG[:, kt, ac], k.xT[:, kt, t4], start=False, stop=(kt == 7))
                P.copy(alr_all[:], ps[0][0:16, :], e="act")
            P.mm(ps[1][:, 0:256], k.ones[0:1, 0:128], ba2[:], start=True, stop=False)
            P.mm(ps[1][:, 0:256], alr_all[:, osl], wa2[:], start=False, stop=True)
            P.act(sp[:], ps[1][:, 0:256], AF.Exp, scale=-1.0)
            P.act(sp[:], sp[:], AF.Ln, bias=1.0)
            P.mm(ps[1][:, 256:512], k.tril_s[:], sp[:], start=True, stop=True)
            for i in range(2):
                P.mm(ps[2][:, i * 128:(i + 1) * 128], sp[:, i * 128:(i + 1) * 128], k.triu_s[:], start=True, stop=True)
            cumT = v3(ps[2][:, 0:256], "p (i t) -> p i t", i=2)
            P.act(eT[:], cumT, AF.Exp)
            P.act(einvT[:], cumT, AF.Exp, scale=-1.0)
            P.act(erev[:], ps[1][:, 256:512], AF.Exp)
            P.copy(dec[par][:], eT[:, :, 127], e="act")
            P.stt(qdT[par][:], qk_raw[:, 0:2, osl], 0.125, eT[:], ALU.mult, ALU.mult)
            P.tt(kiT[par][:], qk_raw[:, 2:4, osl], einvT[:], ALU.mult, e="pool")
            kc = slice(OFF['gk'], OFF['gk'] + 256)
            bias_tm(ps[2][:, 256:512], kc)
            for kt in range(8):
                P.mm(ps[2][:, 256:512], k.xT[:, kt, tsl], WG[:, kt, kc], start=False, stop=(kt == 7))
            vc = slice(OFF['gv'], OFF['gv'] + 512)
            bias_tm(ps[3][:], vc)
            for kt in range(8):
                P.mm(ps[3][:], k.xT[:, kt, tsl], WG[:, kt, vc], start=False, stop=(kt == 7))
            gc = slice(OFF['gg'], OFF['gg'] + 512)
            bias_tm(ps[4][:], gc)
            for kt in range(8):
                P.mm(ps[4][:], k.xT[:, kt, tsl], WG[:, kt, gc], start=False, stop=(kt == 7))
            P.tt(ktail[par][:], ps[2][:, 256:512], erev[:], ALU.mult)
            P.copy(vbf[par][:], ps[3][:], e="act")
            P.act(sgf[:], ps[4][:], AF.Silu)
            P.tt(sg[par][:], sgf[:], ng[:], ALU.mult, e="pool")

        def stage_b(c):
            par = c % 2
            tsl = slice(c * 128, (c + 1) * 128)
            abank = {0: (ps[5], 0), 2: (ps[5], 128), 1: (ps[6], 0), 3: (ps[6], 128)}
            for h in (0, 2, 1, 3):
                hp, ho = h // 2, (h % 2) * 64
                bk, co = abank[h]
                P.mm(bk[:, co:co + 128], kiT[par][ho:ho + 64, hp, :], qdT[par][ho:ho + 64, hp, :], start=True, stop=True)
            for h in range(4):
                bk, co = abank[h]
                P.tt(attT[:, h, :], bk[:, co:co + 128], k.mask01[:], ALU.mult)
            for h in range(4):
                hp, ho = h // 2, (h % 2) * 64
                P.mm(ps[7][:, h * 128:(h + 1) * 128], attT[:, h, :], vbf[par][:, h * 128:(h + 1) * 128], start=True, stop=False)
                P.mm(ps[7][:, h * 128:(h + 1) * 128], qdT[par][ho:ho + 64, hp, :], Sbf[ho:ho + 64, hp, :], start=False, stop=True)
            sbank = [ps[5], ps[6]]
            for hp in range(2):
                P.mm(sbank[hp][:, 256:512], ktail[par][:, hp * 128:(hp + 1) * 128], vbf[par][:, hp * 256:(hp + 1) * 256], start=True, stop=True)
            for hp in range(2):
                for hh in range(2):
                    ho = hh * 64
                    P.stt(S32[ho:ho + 64, hp, :], S32[ho:ho + 64, hp, :], dec[par][ho:ho + 64, hp:hp + 1],
                          sbank[hp][ho:ho + 64, 256 + hh * 128:256 + (hh + 1) * 128], ALU.mult, ALU.add)
            P.copy(Sbf[:], S32[:], e="pool")
            for h in range(4):
                P.op("dve", lambda g, h=h: g.bn_stats(st[:, h, :].ap, ps[7][:, h * 128:(h + 1) * 128].ap),
                     reads=[ps[7][:]], writes=[st[:]])
                P.op("dve", lambda g, h=h: g.bn_aggr(mv[:, h, :].ap, st[:, h, :].ap), reads=[st[:]], writes=[mv[:]])
            rstd_from_var(P, rstd[:], mv[:, :, 1], LN_EPS, tmp4[:])
            for h in range(4):
                P.ts(yn[:, h * 128:(h + 1) * 128], ps[7][:, h * 128:(h + 1) * 128], mv[:, h, 0:1], rstd[:, h:h + 1],
                     ALU.subtract, ALU.mult)
            P.tt(ybf[:], yn[:], sg[par][:], ALU.mult, e="pool")
            psb = V(ps[5][:].ap.bitcast(BF16), ps[5].regs)
            for q in range(4):
                P.tr(psb[:, q * 128:(q + 1) * 128], ybf[:, q * 128:(q + 1) * 128], k.identb[:])
            P.copy(k.yT[0][:, :, tsl],
                   V(psb[:, 0:512].ap.rearrange("p (q t) -> p q t", q=4), ps[5].regs), e="act")

        stage_a(0)
        for c in range(NCHUNK):
            if c + 1 < NCHUNK:
                stage_a(c + 1)
            stage_b(c)


def v3(v, pat, **kw):
    return V(v.ap.rearrange(pat, **kw), v.regs)


def mlstm_phase(P, k, L, w):
    ps = k.ps
    o0 = OFF['mq']
    with Scope(P):
        WM = Tile(P, "WM", [128, 8, 1544], BF16)
        brow = Tile(P, "m_brow", [1, 1544], BF16)
        gb8 = Tile(P, "m_gb8", [1, 8], F32)
        WIF = Tile(P, "WIF", [128, 8, 512], BF16)
        browIF = Tile(P, "m_browIF", [1, 512], F32)
        cw = Tile(P, "m_cw", [128, 4, 4], F32)
        cb = Tile(P, "m_cb", [128, 4], F32)
        ngm = Tile(P, "m_ng", [128, 512], F32)
        P.dma("sp", gb8[:], D(w['b_in'][L:L + 1, o0 + 1024:o0 + 1032]), gb8[:])
        P.dma("pool", WM[:], D(w['w_in'][L, :, o0:o0 + 1544].rearrange("(kt p) n -> p kt n", p=128)), WM[:])
        P.dma("pool", brow[:], D(w['b_in'][L:L + 1, o0:o0 + 1544]), brow[:])
        for tap in range(4):
            P.dma("sp", cw[:, tap, :], D(w['ml_conv_w'][L, tap].rearrange("(j p) -> p j", p=128)), cw[:], allow_slow_non_contiguous=True)
        P.dma("sp", cb[:], D(w['ml_conv_b'][L].rearrange("(j p) -> p j", p=128)), cb[:], allow_slow_non_contiguous=True)
        P.dma("sp", ngm[:], D(w['ml_norm_g'][L].partition_broadcast(128)), ngm[:])
        for gi in range(2):
            for h in range(4):
                col = 1024 + gi * 4 + h
                dst = slice(gi * 256 + h * 64, gi * 256 + (h + 1) * 64)
                P.copy(WIF[:, :, dst], WM[:, :, col:col + 1].bcast([128, 8, 64]), e="dve")
                P.copy(browIF[0:1, dst], gb8[0:1, gi * 4 + h:gi * 4 + h + 1].bcast([1, 64]), e="dve")
        bif_hi = Tile(P, "m_bifhi", [1, 512], BF16)
        bif_lo = Tile(P, "m_biflo", [1, 512], BF16)
        P.copy(bif_hi[:], browIF[:], e="dve")
        P.tt(browIF[:], browIF[:], bif_hi[:], ALU.subtract)
        P.copy(bif_lo[:], browIF[:], e="dve")
        raw = Tile(P, "m_raw4", [128, 4, 515], BF16)
        gif = Tile(P, "m_gif", [128, 4, 512], F32)
        acc = Tile(P, "m_acc", [128, 4, 128], F32)
        kf = Tile(P, "m_kf", [128, 2, 128], F32)
        spf = Tile(P, "m_spf", [128, 2, 128], F32)
        halo = Tile(P, "m_halo", [128, 4, 3], BF16)
        ncum = Tile(P, "m_ncum", [128, 2, 128], F32)
        aT = Tile(P, "m_aT", [128, 2, 128], F32)
        ksc = spf
        clT = Tile(P, "m_clT", [128, 2, 128], F32)
        gfacf = Tile(P, "m_gfacf", [128, 512], BF16)
        mst = Tile(P, "m_mst", [128, 2], F32)
        sm = Tile(P, "m_sm", [128, 16], F32)
        qfT = [Tile(P, f"m_qfT{i}", [128, 2, 128], BF16) for i in range(2)]
        kpT = [Tile(P, f"m_kpT{i}", [128, 2, 128], BF16) for i in range(2)]
        kptok = [Tile(P, f"m_kptok{i}", [128, 256], BF16) for i in range(2)]
        clampv = [Tile(P, f"m_clampv{i}", [128, 4], F32) for i in range(2)]
        vaug = [Tile(P, f"m_vaug{i}", [128, 4, 129], BF16) for i in range(2)]
        gfac = [Tile(P, f"m_gfac{i}", [128, 512], BF16) for i in range(2)]
        wdec = [Tile(P, f"m_wdec{i}", [128, 2], F32) for i in range(2)]
        C32 = Tile(P, "m_C32", [128, 2, 129], F32)
        Cbf = Tile(P, "m_Cbf", [128, 2, 129], BF16)
        attTb = Tile(P, "m_attTb", [128, 4, 128], BF16)
        st = Tile(P, "m_st", [128, 4, 6], F32)
        mv = Tile(P, "m_mv", [128, 4, 2], F32)
        sm2 = Tile(P, "m_sm2", [128, 6, 4], F32)
        yn = Tile(P, "m_yn", [128, 512], F32)
        ybf = Tile(P, "m_ybf", [128, 512], BF16)
        P.memset(raw[:], 0.0)
        P.memset(C32[:], 0.0)
        P.memset(mst[:], 0.0)
        for i in range(2):
            P.memset(vaug[i][:], 1.0)

        def stage_a(c):
            par = c % 2
            tsl = slice(c * 128, (c + 1) * 128)
            off = (c % 4) * 128
            osl = slice(off, off + 128)
            if c % 4 == 0:
                t4 = slice(c * 128, c * 128 + 512)
                P.copy(halo[:], raw[:, :, 512:515], e="pool")
                for i in range(8):
                    cols = slice((i % 4) * 128, (i % 4 + 1) * 128)
                    bank = ps[i % 2]
                    if i < 4:
                        P.mm(bank[:], brow[0:1, cols], k.onesb[0:1, 0:512], start=True, stop=False)
                        Wsrc = WM
                    else:
                        P.mm(bank[:], bif_hi[0:1, cols], k.onesb[0:1, 0:512], start=True, stop=False)
                        P.mm(bank[:], bif_lo[0:1, cols], k.onesb[0:1, 0:512], start=False, stop=False)
                        Wsrc = WIF
                    for kt in range(8):
                        P.mm(bank[:], Wsrc[:, kt, cols], k.xT[:, kt, t4], start=False, stop=(kt == 7))
                    if i < 4:
                        P.copy(raw[:, i, 3:515], bank[:], e="act")
                    else:
                        P.copy(gif[:, i - 4, :], bank[:], e="act")
                P.copy(raw[:, :, 0:3], halo[:], e="pool")
            for i in range(4):
                P.ts(acc[:, i, :], raw[:, i, off:off + 128], cw[:, 0, i:i + 1], cb[:, i:i + 1], ALU.mult, ALU.add)
                for tap in range(1, 4):
                    P.stt(acc[:, i, :], raw[:, i, off + tap:off + tap + 128], cw[:, tap, i:i + 1], acc[:, i, :], ALU.mult, ALU.add)
            P.act(qfT[par][:], acc[:, 0:2, :], AF.Silu)
            P.act(kf[:], acc[:, 2:4, :], AF.Silu)
            gi_ps = gif[:, 0:2, osl]
            gf_ps = gif[:, 2:4, osl]
            P.act(spf[:], gf_ps, AF.Exp, scale=-1.0)
            P.act(spf[:], spf[:], AF.Ln, bias=1.0)
            for i in range(2):
                P.op("dve", lambda g, i=i: g.tensor_tensor_scan(ncum[:, i, :].ap, k.ones[:, 0:128].ap, spf[:, i, :].ap, 0.0,
                                                                 ALU.mult, ALU.add),
                     reads=[k.ones[:], spf[:]], writes=[ncum[:]])
            P.tt(aT[:], gi_ps, ncum[:], ALU.add)
            P.reduce(sm[:, 0:2], aT[:], ALU.max)
            P.tt(sm[:, 2:4], sm[:, 0:2], mst[:], ALU.max)
            P.ts(sm[:, 4:6], sm[:, 2:4], -1.0, None, ALU.mult)
            P.tt(sm[:, 8:10], mst[:], sm[:, 2:4], ALU.subtract)
            P.act(wdec[par][:], sm[:, 8:10], AF.Exp)
            for i in range(2):
                P.act(ksc[:, i, :], aT[:, i, :], AF.Exp, bias=sm[:, 4 + i:5 + i])
                P.act(clT[:, i, :], ncum[:, i, :], AF.Exp, bias=sm[:, 4 + i:5 + i])
            P.stt(kpT[par][:], kf[:], 0.125, ksc[:], ALU.mult, ALU.mult)
            P.tt(mst[:], sm[:, 2:4], ncum[:, :, 127], ALU.subtract)
            for i in range(2):
                P.tr(ps[1][:, i * 128:(i + 1) * 128], clT[:, i, :], k.ident[:])
            psb1 = V(ps[1][:].ap.bitcast(BF16), ps[1].regs)
            for i in range(2):
                P.tr(psb1[:, 512 + i * 128:512 + (i + 1) * 128], kpT[par][:, i, :], k.identb[:])
            P.copy(clampv[par][:], v3(ps[1][:, 0:256], "p (h r) -> p h r", r=64)[:, :, 0], e="act")
            P.copy(kptok[par][:], psb1[:, 512:768], e="act")
            vc = slice(512, 1024)
            P.mm(ps[2][:], k.onesb[0:1, 0:128], brow[0:1, vc], start=True, stop=False)
            for kt in range(8):
                P.mm(ps[2][:], k.xT[:, kt, tsl], WM[:, kt, vc], start=False, stop=(kt == 7))
            oc = slice(1032, 1544)
            P.mm(ps[3][:], k.onesb[0:1, 0:128], brow[0:1, oc], start=True, stop=False)
            for kt in range(8):
                P.mm(ps[3][:], k.xT[:, kt, tsl], WM[:, kt, oc], start=False, stop=(kt == 7))
            P.copy(vaug[par][:, :, 0:128], v3(ps[2][:], "p (h e) -> p h e", h=4), e="act")
            P.act(gfacf[:], ps[3][:], AF.Sigmoid)
            P.tt(gfac[par][:], gfacf[:], ngm[:], ALU.mult, e="pool")

        def stage_b(c):
            par = c % 2
            tsl = slice(c * 128, (c + 1) * 128)
            for i in range(2):
                P.ts(C32[:, i, :], C32[:, i, :], wdec[par][:, i:i + 1], None, ALU.mult)
            P.copy(Cbf[:], C32[:], e="pool")
            abank = {0: (ps[4], 0), 2: (ps[4], 128), 1: (ps[5], 0), 3: (ps[5], 128)}
            for h in (0, 2, 1, 3):
                hp, ho = h // 2, (h % 2) * 64
                bk, co = abank[h]
                P.mm(bk[:, co:co + 128], kpT[par][ho:ho + 64, hp, :], qfT[par][ho:ho + 64, hp, :], start=True, stop=True)
            for h in range(4):
                bk, co = abank[h]
                P.tt(attTb[:, h, :], bk[:, co:co + 128], k.mask01[:], ALU.mult)
            oa = []
            for h in range(4):
                hp, ho = h // 2, (h % 2) * 64
                bank = ps[6] if h < 2 else ps[7]
                o_ = bank[:, (h % 2) * 129:(h % 2) * 129 + 129]
                oa.append(o_)
                P.mm(o_, attTb[:, h, :], vaug[par][:, h, :], start=True, stop=False)
                P.mm(o_, qfT[par][ho:ho + 64, hp, :], Cbf[ho:ho + 64, hp, :], start=False, stop=True)
            for hp in range(2):
                pss = ps[4 + hp]
                P.mm(pss[:, 0:258], kptok[par][:, hp * 128:(hp + 1) * 128],
                     v3(vaug[par][:, 2 * hp:2 * hp + 2, :], "p h e -> p (h e)"), start=True, stop=True)
            for hp in range(2):
                pss = ps[4 + hp]
                for hh in range(2):
                    ho = hh * 64
                    P.tt(C32[ho:ho + 64, hp, :], C32[ho:ho + 64, hp, :], pss[ho:ho + 64, hh * 129:(hh + 1) * 129], ALU.add)
            for h in range(4):
                P.act(sm2[:, 0, h:h + 1], oa[h][:, 128:129], AF.Abs)
            for h in range(4):
                P.op("dve", lambda g, h=h: g.bn_stats(st[:, h, :].ap, oa[h][:, 0:128].ap), reads=[oa[h]], writes=[st[:]])
                P.op("dve", lambda g, h=h: g.bn_aggr(mv[:, h, :].ap, st[:, h, :].ap), reads=[st[:]], writes=[mv[:]])
            P.tt(sm2[:, 0, :], sm2[:, 0, :], clampv[par][:], ALU.max)
            P.recip(sm2[:, 1, :], sm2[:, 0, :])
            P.tt(sm2[:, 2, :], sm2[:, 1, :], sm2[:, 1, :], ALU.mult)
            P.tt(sm2[:, 3, :], mv[:, :, 1], sm2[:, 2, :], ALU.mult)
            rstd_from_var(P, sm2[:, 4, :], sm2[:, 3, :], LN_EPS, sm2[:, 4, :])
            P.tt(sm2[:, 5, :], sm2[:, 4, :], sm2[:, 1, :], ALU.mult)
            for h in range(4):
                P.ts(yn[:, h * 128:(h + 1) * 128], oa[h][:, 0:128], mv[:, h, 0:1], sm2[:, 5, h:h + 1], ALU.subtract, ALU.mult)
            P.tt(ybf[:], yn[:], gfac[par][:], ALU.mult, e="pool")
            psb = V(ps[4][:].ap.bitcast(BF16), ps[4].regs)
            for q in range(4):
                P.tr(psb[:, q * 128:(q + 1) * 128], ybf[:, q * 128:(q + 1) * 128], k.identb[:])
            P.copy(k.yT[2][:, :, tsl],
                   V(psb[:, 0:512].ap.rearrange("p (q t) -> p q t", q=4), ps[4].regs), e="act")

        stage_a(0)
        for c in range(NCHUNK):
            if c + 1 < NCHUNK:
                stage_a(c + 1)
            stage_b(c)


import math
TWO_PI = 2.0 * math.pi


class StopBuild(Exception):
    pass


def stop_at(n):
    if S5_STOP <= n:
        MUTE[0] = True


MUTE = [False]


def s5_phase(P, k, L, w):
    ps = k.ps
    T16 = [128, 16]
    with Scope(P):
        ETre = Tile(P, "s_ETre", [128, 16, 128], BF16)
        ETim = Tile(P, "s_ETim", [128, 16, 128], BF16)
        Epre = Tile(P, "s_Epre", [128, 16, 128], BF16)
        Epim = Tile(P, "s_Epim", [128, 16, 128], BF16)
        lam_re = Tile(P, "s_lamre", T16, F32)
        lam_im = Tile(P, "s_lamim", T16, F32)
        with Scope(P):
            are = Tile(P, "s_are", T16, F32)
            aim = Tile(P, "s_aim", T16, F32)
            ldt = Tile(P, "s_ldt", T16, F32)
            P.dma("sp", are[:], D(w['s5_a_re'][L].rearrange("(j gl) p -> (gl p) j", gl=2)), are[:], allow_slow_non_contiguous=True)
            P.dma("sp", aim[:], D(w['s5_a_im'][L].rearrange("(j gl) p -> (gl p) j", gl=2)), aim[:], allow_slow_non_contiguous=True)
            ldt_src = w['s5_log_dt'][L].rearrange("(j gl) -> gl j", gl=2)
            for gl in range(2):
                P.dma("sp", ldt[gl * 64:(gl + 1) * 64, :], D(ldt_src[gl].partition_broadcast(64)), ldt[:], allow_slow_non_contiguous=True)
            sm = {n: Tile(P, "sq_" + n, T16, F32) for n in
                  ["dt", "re", "im", "mag", "magi", "t0", "t1", "sinv", "cosv", "li_re", "li_im", "cf_re", "cf_im", "pw_re", "pw_im", "q_re", "q_im"]}
            ni = Tile(P, "s_ni", T16, I32)
            e = "dve"
            P.act(sm["dt"][:], ldt[:], AF.Exp)
            P.tt(sm["re"][:], are[:], sm["dt"][:], ALU.mult)
            P.tt(sm["im"][:], aim[:], sm["dt"][:], ALU.mult)
            P.act(sm["mag"][:], sm["re"][:], AF.Exp)
            P.act(sm["magi"][:], sm["re"][:], AF.Exp, scale=-1.0)

            def wrap(t):
                P.ts(sm["t1"][:], t, math.pi, TWO_PI, ALU.is_gt, ALU.mult)
                P.tt(t, t, sm["t1"][:], ALU.subtract)
                P.ts(sm["t1"][:], t, -math.pi, TWO_PI, ALU.is_lt, ALU.mult)
                P.tt(t, t, sm["t1"][:], ALU.add)

            P.ts(sm["t0"][:], sm["im"][:], 1.0 / TWO_PI, None, ALU.mult)
            P.copy(ni[:], sm["t0"][:])
            P.copy(sm["t0"][:], ni[:])
            P.stt(sm["im"][:], sm["t0"][:], -TWO_PI, sm["im"][:], ALU.mult, ALU.add)
            wrap(sm["im"][:])
            P.act(sm["sinv"][:], sm["im"][:], AF.Sin)
            P.ts(sm["t0"][:], sm["im"][:], math.pi / 2, None, ALU.add)
            wrap(sm["t0"][:])
            P.act(sm["cosv"][:], sm["t0"][:], AF.Sin)
            P.tt(lam_re[:], sm["mag"][:], sm["cosv"][:], ALU.mult)
            P.tt(lam_im[:], sm["mag"][:], sm["sinv"][:], ALU.mult)
            P.tt(sm["li_re"][:], sm["magi"][:], sm["cosv"][:], ALU.mult)
            P.stt(sm["li_im"][:], sm["magi"][:], -1.0, sm["sinv"][:], ALU.mult, ALU.mult)
            P.ts(sm["q_re"][:], lam_re[:], -1.0, None, ALU.add)
            P.tt(sm["t0"][:], are[:], are[:], ALU.mult)
            P.tt(sm["t1"][:], aim[:], aim[:], ALU.mult)
            P.tt(sm["t0"][:], sm["t0"][:], sm["t1"][:], ALU.add)
            P.recip(sm["q_im"][:], sm["t0"][:])
            P.tt(sm["t0"][:], sm["q_re"][:], are[:], ALU.mult)
            P.tt(sm["t1"][:], lam_im[:], aim[:], ALU.mult)
            P.tt(sm["t0"][:], sm["t0"][:], sm["t1"][:], ALU.add)
            P.tt(sm["cf_re"][:], sm["t0"][:], sm["q_im"][:], ALU.mult)
            P.tt(sm["t0"][:], lam_im[:], are[:], ALU.mult)
            P.tt(sm["t1"][:], sm["q_re"][:], aim[:], ALU.mult)
            P.tt(sm["t0"][:], sm["t0"][:], sm["t1"][:], ALU.subtract)
            P.tt(sm["cf_im"][:], sm["t0"][:], sm["q_im"][:], ALU.mult)
            if S5_STOP <= 1:
                return
            Tre = Tile(P, "s_Tre", [128, 16, 128], F32)
            Tim = Tile(P, "s_Tim", [128, 16, 128], F32)
            tA = Tile(P, "s_tA", [128, 16, 64], F32)
            tB = Tile(P, "s_tB", [128, 16, 64], F32)

            def build_table(bre, bim):
                P.memset(Tre[:, :, 0:1], 1.0)
                P.memset(Tim[:, :, 0:1], 0.0)
                P.copy(Tre[:, :, 1], bre)
                P.copy(Tim[:, :, 1], bim)
                P.copy(sm["pw_re"][:], bre)
                P.copy(sm["pw_im"][:], bim)
                for kk in range(1, 7):
                    n = 1 << kk
                    P.tt(sm["t0"][:], sm["pw_re"][:], sm["pw_re"][:], ALU.mult)
                    P.tt(sm["t1"][:], sm["pw_im"][:], sm["pw_im"][:], ALU.mult)
                    P.tt(sm["q_re"][:], sm["pw_re"][:], sm["pw_im"][:], ALU.mult)
                    P.tt(sm["pw_re"][:], sm["t0"][:], sm["t1"][:], ALU.subtract)
                    P.ts(sm["pw_im"][:], sm["q_re"][:], 2.0, None, ALU.mult)
                    pr = v3(sm["pw_re"][:], "p (j o) -> p j o", o=1).bcast([128, 16, n])
                    pi_ = v3(sm["pw_im"][:], "p (j o) -> p j o", o=1).bcast([128, 16, n])
                    P.tt(tA[:, :, 0:n], Tre[:, :, 0:n], pr, ALU.mult)
                    P.tt(tB[:, :, 0:n], Tim[:, :, 0:n], pi_, ALU.mult, e="pool")
                    P.tt(Tre[:, :, n:2 * n], tA[:, :, 0:n], tB[:, :, 0:n], ALU.subtract)
                    P.tt(tA[:, :, 0:n], Tre[:, :, 0:n], pi_, ALU.mult)
                    P.tt(tB[:, :, 0:n], Tim[:, :, 0:n], pr, ALU.mult, e="pool")
                    P.tt(Tim[:, :, n:2 * n], tA[:, :, 0:n], tB[:, :, 0:n], ALU.add)

            build_table(lam_re[:], lam_im[:])
            P.copy(ETre[:], Tre[:], e="act")
            P.copy(ETim[:], Tim[:], e="act")
            build_table(sm["li_re"][:], sm["li_im"][:])
            cr = v3(sm["cf_re"][:], "p (j o) -> p j o", o=1)
            ci = v3(sm["cf_im"][:], "p (j o) -> p j o", o=1)
            for hh in range(2):
                sl = slice(hh * 64, (hh + 1) * 64)
                crb, cib = cr.bcast([128, 16, 64]), ci.bcast([128, 16, 64])
                P.tt(tA[:], Tre[:, :, sl], crb, ALU.mult)
                P.tt(tB[:], Tim[:, :, sl], cib, ALU.mult, e="pool")
                P.tt(tA[:], tA[:], tB[:], ALU.subtract)
                P.tt(tB[:], Tre[:, :, sl], cib, ALU.mult, e="pool")
                P.tt(Tim[:, :, sl], Tim[:, :, sl], crb, ALU.mult)
                P.tt(Tim[:, :, sl], Tim[:, :, sl], tB[:], ALU.add)
                P.copy(Tre[:, :, sl], tA[:])
            for (src, dst) in ((Tre, Epre), (Tim, Epim)):
                for g4 in range(4):
                    bank = ps[g4 % 2]
                    for jj in range(4):
                        P.tr(bank[:, jj * 128:(jj + 1) * 128], src[:, g4 * 4 + jj, :], k.ident[:])
                    P.copy(dst[:, g4 * 4:g4 * 4 + 4, :], v3(bank[:], "p (j q) -> p j q", j=4), e="act" if g4 % 2 == 0 else "dve")
        if S5_STOP <= 2:
            return
        BTre = Tile(P, "s_BTre", [128, 16, 128], BF16)
        BTim = Tile(P, "s_BTim", [128, 16, 128], BF16)
        CTre = Tile(P, "s_CTre", [128, 16, 128], BF16)
        CTimn = Tile(P, "s_CTimn", [128, 16, 128], BF16)
        Wu = Tile(P, "s_Wu", [128, 8, 512], BF16)
        Wglu = Tile(P, "s_Wglu", [128, 4, 512], BF16)
        browu = Tile(P, "s_browu", [1, 512], F32)
        bglu = Tile(P, "s_bglu", [128, 4], F32)
        dvec = Tile(P, "s_dvec", [128, 4], F32)
        o0 = OFF['su']
        P.dma("pool", Wu[:], D(w['w_in'][L, :, o0:o0 + 512].rearrange("(kt p) n -> p kt n", p=128)), Wu[:])
        P.dma("pool", Wglu[:], D(w['s5_w_glu'][L].rearrange("(kt p) n -> p kt n", p=128)), Wglu[:])
        P.dma("sp", browu[:], D(w['b_in'][L:L + 1, o0:o0 + 512]), browu[:])
        P.dma("sp", bglu[:], D(w['s5_b_glu'][L].rearrange("(j p) -> p j", p=128)), bglu[:], allow_slow_non_contiguous=True)
        P.dma("sp", dvec[:], D(w['s5_d'][L].rearrange("(j p) -> p j", p=128)), dvec[:], allow_slow_non_contiguous=True)
        with Scope(P):
            Bnat = Tile(P, "s_Bnat", [128, 16, 16], F32)
            Bpad = Tile(P, "s_Bpad", [128, 16, 128], F32)
            Cnat = Tile(P, "s_Cnat", [128, 4, 128], F32)
            Cfull = Tile(P, "s_Cfull", [128, 4, 128], F32)
            P.memset(Bpad[:], 0.0)
            P.memset(CTre[:], 0.0)
            P.memset(CTimn[:], 0.0)
            for (nm, BT) in (("s5_b_re", BTre), ("s5_b_im", BTim)):
                P.dma("sp", Bnat[:], D(w[nm][L].rearrange("g p n -> (g p) n").rearrange("(j q) n -> q j n", q=128)), Bnat[:])
                Bp4 = v3(Bpad[:], "q (kt jj) c -> q kt jj c", jj=4)
                Bn4 = v3(Bnat[:], "q (kt jj) n -> q kt jj n", jj=4)
                for jj in range(4):
                    for half in range(2):
                        c0 = (2 * jj + half) * 16
                        P.copy(Bp4[half * 64:(half + 1) * 64, :, jj, c0:c0 + 16], Bn4[half * 64:(half + 1) * 64, :, jj, :])
                for g4 in range(4):
                    bank = ps[g4 % 2]
                    for jj in range(4):
                        P.tr(bank[:, jj * 128:(jj + 1) * 128], Bpad[:, g4 * 4 + jj, :], k.ident[:])
                    P.copy(BT[:, g4 * 4:g4 * 4 + 4, :], v3(bank[:], "p (j q) -> p j q", j=4), e="act" if g4 % 2 == 0 else "dve")
            for (nm, CT, sgn) in (("s5_c_re", CTre, 1.0), ("s5_c_im", CTimn, -1.0)):
                csrc = w[nm][L].rearrange("(kt g8) n p -> (g8 n) kt p", g8=8)
                P.dma("sp", Cnat[:, :, 0:64], D(csrc), Cnat[:])
                P.dma("sp", Cnat[:, :, 64:128], D(csrc), Cnat[:])
                for kt in range(4):
                    P.tr(ps[2][:, kt * 128:(kt + 1) * 128], Cnat[:, kt, :], k.ident[:])
                P.copy(Cfull[:], v3(ps[2][:], "p (kt c) -> p kt c", kt=4), e="act")
                C4 = v3(CT[:], "q (kt jj) c -> q kt jj c", jj=4)
                for jj in range(4):
                    for half in range(2):
                        c0 = (2 * jj + half) * 16
                        P.ts(C4[half * 64:(half + 1) * 64, :, jj, c0:c0 + 16], Cfull[half * 64:(half + 1) * 64, :, c0:c0 + 16],
                             sgn, None, ALU.mult)
        if S5_STOP <= 3:
            return
        uT32s = [Tile(P, f"s_uT32{i}", [128, 4, 128], F32) for i in range(2)]
        uTbs = [Tile(P, f"s_uTb{i}", [128, 4, 128], BF16) for i in range(2)]
        bure = Tile(P, "s_bure", [128, 4, 128], F32)
        buim = Tile(P, "s_buim", [128, 4, 128], F32)
        t1 = Tile(P, "s_t1", [128, 4, 128], F32)
        t2 = Tile(P, "s_t2", [128, 4, 128], F32)
        t3 = Tile(P, "s_t3", [128, 4, 128], F32)
        t4 = Tile(P, "s_t4", [128, 4, 128], F32)
        Wre = Tile(P, "s_Wre", [128, 4, 128], BF16)
        Wim = Tile(P, "s_Wim", [128, 4, 128], BF16)
        Zre = Tile(P, "s_Zre", [128, 4, 128], F32)
        Zim = Tile(P, "s_Zim", [128, 4, 128], F32)
        Xre = Tile(P, "s_Xre", [128, 4, 128], BF16)
        Xim = Tile(P, "s_Xim", [128, 4, 128], BF16)
        xe_re = Tile(P, "s_xere", T16, F32)
        xe_im = Tile(P, "s_xeim", T16, F32)
        c_re = Tile(P, "s_cre", T16, F32)
        c_im = Tile(P, "s_cim", T16, F32)
        s1 = Tile(P, "s_s1", T16, F32)
        s2 = Tile(P, "s_s2", T16, F32)
        ysk = Tile(P, "s_ysk", [128, 4, 128], F32)
        ygb = Tile(P, "s_ygb", [128, 4, 128], BF16)
        P.memset(c_re[:], 0.0)
        P.memset(c_im[:], 0.0)
        zbank = [(ps[3], ps[4]), (ps[5], ps[6])]

        def front1(u):
            c, g = divmod(u, 4)
            tsl = slice(c * 128, (c + 1) * 128)
            uT32, uTb = uT32s[c % 2], uTbs[c % 2]
            if g == 0:
                for i in range(4):
                    cols = slice(i * 128, (i + 1) * 128)
                    proj_fm(P, k, ps[0][:, i * 128:(i + 1) * 128], Wu, cols, browu, cols, tsl, 128)
                u_ps = v3(ps[0][:], "p (i t) -> p i t", i=4)
                P.copy(uT32[:], u_ps, e="act")
                P.copy(uTb[:], u_ps, e="act")
            js = slice(4 * g, 4 * g + 4)
            zr, zi = zbank[u % 2]
            P.mm(ps[1][:], uTb[:, g, :], v3(BTre[:, js, :], "p j q -> p (j q)"), start=True, stop=True)
            P.mm(ps[2][:], uTb[:, g, :], v3(BTim[:, js, :], "p j q -> p (j q)"), start=True, stop=True)
            P.copy(bure[:], v3(ps[1][:], "p (j q) -> p j q", j=4), e="act")
            P.copy(buim[:], v3(ps[2][:], "p (j q) -> p j q", j=4), e="act")

        def front2(u):
            c, g = divmod(u, 4)
            js = slice(4 * g, 4 * g + 4)
            zr, zi = zbank[u % 2]
            P.tt(t1[:], Epre[:, js, :], bure[:], ALU.mult)
            P.tt(t2[:], Epim[:, js, :], buim[:], ALU.mult)
            P.tt(Wre[:], t1[:], t2[:], ALU.subtract)
            P.tt(t3[:], Epre[:, js, :], buim[:], ALU.mult)
            P.tt(t4[:], Epim[:, js, :], bure[:], ALU.mult, e="pool")
            P.tt(Wim[:], t3[:], t4[:], ALU.add, e="pool")
            for jj in range(4):
                P.mm(zr[:, jj * 128:(jj + 1) * 128], Wre[:, jj, :], k.mask01b[:], start=True, stop=True)
            for jj in range(4):
                P.mm(zi[:, jj * 128:(jj + 1) * 128], Wim[:, jj, :], k.mask01b[:], start=True, stop=True)


        def front(u):
            front1(u)
            front2(u)

        def back(u):
            c, g = divmod(u, 4)
            tsl = slice(c * 128, (c + 1) * 128)
            uT32 = uT32s[c % 2]
            js = slice(4 * g, 4 * g + 4)
            zr, zi = zbank[u % 2]
            for jj in range(4):
                j = 4 * g + jj
                P.act(Zre[:, jj, :], zr[:, jj * 128:(jj + 1) * 128], AF.Identity, bias=c_re[:, j:j + 1])
            for jj in range(4):
                j = 4 * g + jj
                P.act(Zim[:, jj, :], zi[:, jj * 128:(jj + 1) * 128], AF.Identity, bias=c_im[:, j:j + 1])
            P.tt(t1[:], ETre[:, js, :], Zre[:], ALU.mult)
            P.tt(t2[:], ETim[:, js, :], Zim[:], ALU.mult)
            P.tt(Xre[:], t1[:], t2[:], ALU.subtract)
            P.tt(t3[:], ETim[:, js, :], Zre[:], ALU.mult)
            P.tt(t4[:], ETre[:, js, :], Zim[:], ALU.mult, e="pool")
            P.tt(Xim[:], t3[:], t4[:], ALU.add, e="pool")
            P.tt(xe_re[:, js], t1[:, :, 127], t2[:, :, 127], ALU.subtract)
            P.tt(xe_im[:, js], t3[:, :, 127], t4[:, :, 127], ALU.add, e="pool")
            yo = ps[7][:, g * 128:(g + 1) * 128]
            for jj in range(4):
                P.mm(yo, CTre[:, 4 * g + jj, :], Xre[:, jj, :], start=(jj == 0), stop=False)
                P.mm(yo, CTimn[:, 4 * g + jj, :], Xim[:, jj, :], start=False, stop=(jj == 3))
            if g != 3:
                return
            P.tt(s1[:], lam_re[:], xe_re[:], ALU.mult)
            P.tt(s2[:], lam_im[:], xe_im[:], ALU.mult)
            P.tt(c_re[:], s1[:], s2[:], ALU.subtract)
            P.tt(s1[:], lam_re[:], xe_im[:], ALU.mult)
            P.tt(s2[:], lam_im[:], xe_re[:], ALU.mult)
            P.tt(c_im[:], s1[:], s2[:], ALU.add)
            for kt in range(4):
                P.stt(ysk[:, kt, :], uT32[:, kt, :], dvec[:, kt:kt + 1], ps[7][:, kt * 128:(kt + 1) * 128], ALU.mult, ALU.add)
            P.tt(t3[:], ysk[:], ysk[:], ALU.mult, e="pool")
            P.ts(t3[:], t3[:], 0.044715, 1.0, ALU.mult, ALU.add, e="pool")
            P.tt(t3[:], t3[:], ysk[:], ALU.mult, e="pool")
            P.act(t4[:], t3[:], AF.Sigmoid, scale=2.0 * math.sqrt(2.0 / math.pi))
            P.tt(t3[:], ysk[:], t4[:], ALU.mult)
            P.copy(ygb[:], t3[:], e="act")
            for kp in range(4):
                for kt in range(4):
                    P.mm(ps[0][:, kp * 128:(kp + 1) * 128], Wglu[:, kt, kp * 128:(kp + 1) * 128], ygb[:, kt, :],
                         start=(kt == 0), stop=(kt == 3))
            for kp in range(4):
                P.act(t4[:, kp, :], ps[0][:, kp * 128:(kp + 1) * 128], AF.Sigmoid, bias=bglu[:, kp:kp + 1])
            P.tt(k.yT[1][:, :, tsl], t3[:], t4[:], ALU.mult)

        nu = 4 * NCHUNK
        front(0)
        for u in range(nu):
            if S5_ORDER == 1:
                if u + 1 < nu:
                    front1(u + 1)
                back(u)
                if u + 1 < nu:
                    front2(u + 1)
            else:
                if u + 1 < nu:
                    front(u + 1)
                back(u)


def layer_norm_tile(P, k, xv, g, b, wk):
    st, mv, rs, tmp = wk["st"], wk["mv"], wk["rs"], wk["tmp"]
    for hf in range(2):
        P.op("dve", lambda gg, hf=hf: gg.bn_stats(st[:, hf, :].ap, xv[:, hf * 512:(hf + 1) * 512].ap), reads=[xv], writes=[st[:]])
    P.op("dve", lambda gg: gg.bn_aggr(mv[:].ap, v3(st[:], "p a b -> p (a b)").ap), reads=[st[:]], writes=[mv[:]])
    rstd_from_var(P, rs[:], mv[:, 1:2], LN_EPS, tmp[:])
    P.ts(xv, xv, mv[:, 0:1], rs[:, 0:1], ALU.subtract, ALU.mult)
    P.tt(xv, xv, g[:], ALU.mult, e="pool")
    P.tt(xv, xv, b[:], ALU.add, e="pool")


def ln_work(P, pfx):
    return dict(st=Tile(P, pfx + "st", [128, 2, 6], F32), mv=Tile(P, pfx + "mv", [128, 2], F32),
                rs=Tile(P, pfx + "rs", [128, 1], F32), tmp=Tile(P, pfx + "tmp", [128, 1], F32))


def merge_phase(P, k, L, w, moe):
    ps = k.ps
    go = OFF['gate']
    with Scope(P):
        mixedT = Tile(P, "mixedT", [128, 8, SEQ], BF16)
        with Scope(P):
            wgt = [Tile(P, f"wgt{i}", [128, 8, 3, 128], BF16) for i in range(2)]
            wup = [Tile(P, f"wup{i}", [128, 3, 4, 128], BF16) for i in range(2)]
            bnat = Tile(P, "bg_nat", [24, 128], F32)
            bgate = Tile(P, "bgate", [128, 24], F32)
            sig = [Tile(P, f"mg_sig{i}", [128, 512], F32) for i in range(2)]
            acc = Tile(P, "mg_acc", [128, 512], F32)
            tmp = [Tile(P, "mg_tmp0", [128, 512], F32)] * 2
            P.dma("sp", bnat[:], D(w['b_in'][L, go:go + 3072].rearrange("(r p) -> r p", p=128)), bnat[:])
            P.tr(ps[7][:, 0:24], bnat[:], k.ident[0:24, 0:24])
            P.copy(bgate[:], ps[7][:, 0:24])
            it = 0
            for dti in range(8):
                slot = dti % 2
                for b in range(3):
                    c0 = go + b * 1024 + dti * 128
                    P.dma("pool", wgt[slot][:, :, b, :], D(w['w_in'][L, :, c0:c0 + 128].rearrange("(kt p) n -> p kt n", p=128)), wgt[slot][:])
                    P.dma("pool", wup[slot][:, b, :, :], D(w['w_up'][L, b, :, dti * 128:(dti + 1) * 128].rearrange("(kt p) n -> p kt n", p=128)), wup[slot][:])
                for tb in range(4):
                    tsl = slice(tb * 512, (tb + 1) * 512)
                    for b in range(3):
                        pg, pu = ps[2 * (it % 2)], ps[2 * (it % 2) + 1]
                        for kt in range(8):
                            P.mm(pg[:], wgt[slot][:, kt, b, :], k.xT[:, kt, tsl], start=(kt == 0), stop=(kt == 7))
                        for kt in range(4):
                            P.mm(pu[:], wup[slot][:, b, kt, :], k.yT[b][:, kt, tsl], start=(kt == 0), stop=(kt == 3))
                        sg = sig[it % 2]
                        P.act(sg[:], pg[:], AF.Sigmoid, bias=bgate[:, b * 8 + dti:b * 8 + dti + 1])
                        if b == 0:
                            P.tt(acc[:], sg[:], pu[:], ALU.mult)
                        elif b == 1:
                            P.tt(tmp[0][:], sg[:], pu[:], ALU.mult)
                            P.tt(acc[:], acc[:], tmp[0][:], ALU.add, e="pool")
                        else:
                            P.tt(tmp[1][:], sg[:], pu[:], ALU.mult)
                            P.tt(mixedT[:, dti, tsl], acc[:], tmp[1][:], ALU.add, e="pool")
                        it += 1
        if MERGE_STOP == 1:
            return
        with Scope(P):
            Wo = Tile(P, "Wo", [128, 8, 1024], BF16)
            P.dma("pool", Wo[:], D(w['w_o'][L].rearrange("(kt p) n -> p kt n", p=128)), Wo[:])
            for tt in range(NT):
                xv = k.xres.sub(tt)
                tsl = slice(tt * 128, (tt + 1) * 128)
                for hf in range(2):
                    po = ps[2 + (2 * tt + hf) % 4]
                    for dti in range(8):
                        P.mm(po[:], mixedT[:, dti, tsl], Wo[:, dti, hf * 512:(hf + 1) * 512], start=(dti == 0), stop=(dti == 7))
                    P.stt(xv[:, hf * 512:(hf + 1) * 512], xv[:, hf * 512:(hf + 1) * 512], DN_ALPHA, po[:], ALU.mult, ALU.add)
        with Scope(P):
            g1 = Tile(P, "ln1g", [128, 1024], F32)
            b1 = Tile(P, "ln1b", [128, 1024], F32)
            wks = [ln_work(P, "ln1a_"), ln_work(P, "ln1b_")]
            P.dma("sp", g1[:], D(w['ln1_g'][L].partition_broadcast(128)), g1[:])
            P.dma("sp", b1[:], D(w['ln1_b'][L].partition_broadcast(128)), b1[:])
            if moe:
                Wr = Tile(P, "Wr", [128, 8, 8], F32)
                br = Tile(P, "br", [1, 8], F32)
                xTf = Tile(P, "xTf", [128, 8, 128], F32)
                rt = Tile(P, "rt", [128, 6, 8], F32)
                P.dma("sp", Wr[:], D(w['moe_router'][0].rearrange("(kt p) n -> p kt n", p=128)), Wr[:])
                P.dma("sp", br[:], D(w['moe_router_b'][0:1, :]), br[:])
            for tt in range(NT):
                xv = k.xres.sub(tt)
                layer_norm_tile(P, k, xv, g1, b1, wks[tt % 2])
                make_xT(P, k, tt, xtf=(xTf if moe else None), banks=((0, 1) if tt % 2 == 0 else (2, 3)))
                if moe:
                    lg = ps[4][:, 0:8]
                    P.mm(lg, k.ones[0:1, 0:128], br[:], start=True, stop=False)
                    for kt in range(8):
                        P.mm(lg, xTf[:, kt, :], Wr[:, kt, :], start=False, stop=(kt == 7))
                    P.copy(rt[:, 0, :], lg)
                    P.op("dve", lambda g_: g_.max(rt[:, 1, :].ap, rt[:, 0, :].ap), reads=[rt[:]], writes=[rt[:]])
                    P.ts(rt[:, 2, :], rt[:, 0, :], rt[:, 1, 1:2], None, ALU.is_ge)
                    P.ts(rt[:, 5, 0:1], rt[:, 1, 0:1], -1.0, None, ALU.mult)
                    P.act(rt[:, 3, :], rt[:, 0, :], AF.Exp, bias=rt[:, 5, 0:1])
                    P.tt(rt[:, 4, :], rt[:, 3, :], rt[:, 2, :], ALU.mult)
                    P.reduce(rt[:, 5, 1:2], rt[:, 4, :], ALU.add)
                    P.recip(rt[:, 5, 2:3], rt[:, 5, 1:2])
                    P.ts(k.comb[:, tt, :], rt[:, 4, :], rt[:, 5, 2:3], None, ALU.mult)


def ffn_phase(P, k, L, w, moe, out_dram, last):
    ps = k.ps
    with Scope(P):
        with Scope(P):
            Wpg = Tile(P, "Wpg", [128, 8, 1024], BF16)
            Wpp = Tile(P, "Wpp", [128, 2, 1024], BF16)
            pt = [Tile(P, f"pt{i}", [128, 256], F32) for i in range(2)]
            pTb = Tile(P, "pTb", [128, 2, 128], BF16)
            sg = Tile(P, "ple_sg", [128, 1024], F32)
            P.dma("pool", Wpg[:], D(w['ple_w_gate'][L].rearrange("(kt p) n -> p kt n", p=128)), Wpg[:])
            P.dma("pool", Wpp[:], D(w['ple_w_proj'][L].rearrange("(kt p) n -> p kt n", p=128)), Wpp[:])
            for tt in range(NT):
                xv = k.xres.sub(tt)
                tsl = slice(tt * 128, (tt + 1) * 128)
                ptt = pt[tt % 2]
                P.dma("sp", ptt[:], D(w['p'][L, tt * 128:(tt + 1) * 128, :]), ptt[:])
                for q in range(2):
                    P.tr(ps[6][:, q * 128:(q + 1) * 128], ptt[:, q * 128:(q + 1) * 128], k.ident[:])
                P.copy(pTb[:], v3(ps[6][:, 0:256], "p (q t) -> p q t", q=2), e="act")
                for hf in range(2):
                    hs = slice(hf * 512, (hf + 1) * 512)
                    pg, pp = ps[2 + hf], ps[4 + hf]
                    for kt in range(8):
                        P.mm(pg[:], k.xT[:, kt, tsl], Wpg[:, kt, hs], start=(kt == 0), stop=(kt == 7))
                    for kt in range(2):
                        P.mm(pp[:], pTb[:, kt, :], Wpp[:, kt, hs], start=(kt == 0), stop=(kt == 1))
                    P.act(sg[:, hs], pg[:], AF.Sigmoid)
                    P.tt(sg[:, hs], sg[:, hs], pp[:], ALU.mult)
                    P.stt(xv[:, hs], xv[:, hs], DN_ALPHA, sg[:, hs], ALU.mult, ALU.add)
        if FFN_STOP == 1:
            return
        with Scope(P):
            wg = [Tile(P, f"f_wg{i}", [128, 8, 512], BF16) for i in range(2)]
            wu = [Tile(P, f"f_wu{i}", [128, 8, 512], BF16) for i in range(2)]
            wd = [Tile(P, f"f_wd{i}", [128, 4, 1024], BF16) for i in range(2)]
            actT = [Tile(P, f"f_act{i}", [128, 4, 512], BF16) for i in range(2)]
            sgl = [Tile(P, f"f_sg{i}", [128, 512], F32) for i in range(2)]
            if moe:
                experts = [(w['moe_wg'][0, e], w['moe_wu'][0, e], w['moe_wd'][0, e], 3584, e) for e in range(N_EXP)]
            else:
                experts = [(w['ffn_wg'][0], w['ffn_wu'][0], w['ffn_wd'][0], 2816, None)]
            cnt = 0
            it = 0
            ia = 0
            io = 0
            for (Wg, Wu_, Wd, dff, e) in experts:
                nft = dff // 128
                f0t = 0
                while f0t < nft:
                    nf = min(4, nft - f0t)
                    slot = cnt % 2
                    cs = slice(f0t * 128, (f0t + nf) * 128)
                    P.dma("pool", wg[slot][:, :, 0:nf * 128], D(Wg[:, cs].rearrange("(kt p) n -> p kt n", p=128)), wg[slot][:])
                    P.dma("pool", wu[slot][:, :, 0:nf * 128], D(Wu_[:, cs].rearrange("(kt p) n -> p kt n", p=128)), wu[slot][:])
                    P.dma("pool", wd[slot][:, 0:nf, :], D(Wd[cs, :].rearrange("(ft p) n -> p ft n", p=128)), wd[slot][:])
                    for tb in range(4):
                        tsl = slice(tb * 512, (tb + 1) * 512)
                        at = actT[ia % 2]
                        ia += 1
                        for ft in range(nf):
                            pg, pu = ps[it % 2], ps[2 + it % 2]
                            fs = slice(ft * 128, (ft + 1) * 128)
                            for kt in range(8):
                                P.mm(pg[:], wg[slot][:, kt, fs], k.xT[:, kt, tsl], start=(kt == 0), stop=(kt == 7))
                            for kt in range(8):
                                P.mm(pu[:], wu[slot][:, kt, fs], k.xT[:, kt, tsl], start=(kt == 0), stop=(kt == 7))
                            P.act(sgl[it % 2][:], pg[:], AF.Silu)
                            P.tt(at[:, ft, :], sgl[it % 2][:], pu[:], ALU.mult)
                            it += 1
                        for t4 in range(4):
                            tt = tb * 4 + t4
                            xv = k.xres.sub(tt)
                            for hf in range(2):
                                hs = slice(hf * 512, (hf + 1) * 512)
                                po = ps[4 + io % 4]
                                io += 1
                                for ft in range(nf):
                                    P.mm(po[:], at[:, ft, t4 * 128:(t4 + 1) * 128], wd[slot][:, ft, hs], start=(ft == 0), stop=(ft == nf - 1))
                                if e is None:
                                    P.tt(xv[:, hs], xv[:, hs], po[:], ALU.add)
                                else:
                                    P.stt(xv[:, hs], po[:], k.comb[:, tt, e:e + 1], xv[:, hs], ALU.mult, ALU.add)
                    f0t += nf
                    cnt += 1
        if FFN_STOP == 2:
            return
        with Scope(P):
            g2 = Tile(P, "ln2g", [128, 1024], F32)
            b2 = Tile(P, "ln2b", [128, 1024], F32)
            wks = [ln_work(P, "ln2a_"), ln_work(P, "ln2b_")]
            P.dma("sp", g2[:], D(w['ln2_g'][L].partition_broadcast(128)), g2[:])
            P.dma("sp", b2[:], D(w['ln2_b'][L].partition_broadcast(128)), b2[:])
            for tt in range(NT):
                xv = k.xres.sub(tt)
                layer_norm_tile(P, k, xv, g2, b2, wks[tt % 2])
                if last:
                    P.dma("sp", D(out_dram[tt * 128:(tt + 1) * 128, :]), xv, xv)
                else:
                    make_xT(P, k, tt, banks=((0, 1) if tt % 2 == 0 else (2, 3)))


N_EXP = int(os.environ.get('N_EXP', '8'))
MERGE_STOP = int(os.environ.get('MERGE_STOP', '0'))
FFN_STOP = int(os.environ.get('FFN_STOP', '0'))
N_LAYERS = int(os.environ.get('N_LAYERS', '2'))
DUMP = os.environ.get('DUMP', '')


def dump_xres(P, k, out_dram):
    for tt in range(NT):
        xv = k.xres.sub(tt)
        P.dma("sp", D(out_dram[tt * 128:(tt + 1) * 128, :]), xv, xv)


def build(n_layers=None, debug=None):
    n_layers = N_LAYERS if n_layers is None else n_layers
    nc = bass.Bass("TRN2", target_bir_lowering=False)
    w = {}
    for name, shp in INPUT_SHAPES.items():
        w[name] = nc.dram_tensor(name, list(shp), F32, kind="ExternalInput").ap()
    out = nc.dram_tensor("out", [SEQ, DM], F32, kind="ExternalOutput").ap()
    dbg = None
    if debug == "yT":
        dbg = nc.dram_tensor("dbg", [128, 3 * 4 * SEQ], BF16, kind="ExternalOutput").ap()
    with ExitStack() as es:
        P = Prog(nc, es)
        P.mute = MUTE
        k = K()
        setup_consts(P, k)
        k.xres = Tile(P, "xres", [128, NT, DM], F32, nreg=NT)
        k.xT = Tile(P, "xT", [128, 8, SEQ], BF16, nreg=NT)
        k.comb = Tile(P, "comb", [128, NT, 8], F32)
        load_x(P, k, w['x'])
        stop = False
        for L in range(n_layers):
            moe = (L % 2 == 1)
            with Scope(P):
                k.yT = {}
                k.yT[1] = Tile(P, "yT_s5", [128, 4, SEQ], BF16)
                if debug == 'yT':
                    P.memset(k.yT[1][:], 0.0)
                if 's5' in PHASES:
                    s5_phase(P, k, L, w)
                    MUTE[0] = False
                k.yT[2] = Tile(P, "yT_ml", [128, 4, SEQ], BF16)
                if debug == 'yT':
                    P.memset(k.yT[2][:], 0.0)
                if 'ml' in PHASES:
                    mlstm_phase(P, k, L, w)
                k.yT[0] = Tile(P, "yT_gla", [128, 4, SEQ], BF16)
                if debug == 'yT':
                    P.memset(k.yT[0][:], 0.0)
                if 'gla' in PHASES:
                    gla_phase(P, k, L, w)
                if debug == "yT":
                    for b in range(3):
                        P.dma("sp", D(dbg[:, b * 4 * SEQ:(b + 1) * 4 * SEQ]),
                              v3(k.yT[b][:], "p a t -> p (a t)"), k.yT[b][:])
                    stop = True
                else:
                    merge_phase(P, k, L, w, moe)
            if stop:
                break
            if DUMP == f"{L}:ln1":
                dump_xres(P, k, out)
                break
            ffn_phase(P, k, L, w, moe, out, last=(L == n_layers - 1))
            if DUMP == f"{L}:ln2" and L != n_layers - 1:
                dump_xres(P, k, out)
                break
        P.finish()
        print("ops", P.n_ops, "waits", P.n_waits, {e: P.cnt[e] for e in ENG}, "dsems", P.n_dsem)
    return nc


def kernel(**inputs):
    nc = build()
    shared = {k_: np.ascontiguousarray(np.asarray(v, dtype=np.float32)) for k_, v in inputs.items() if k_ not in ("x", "p")}
    xs = np.asarray(inputs["x"], dtype=np.float32)
    ps_ = np.asarray(inputs["p"], dtype=np.float32)
    n = xs.shape[0]
    maps = []
    for b in range(n):
        m = dict(shared)
        m["x"] = np.ascontiguousarray(xs[b])
        m["p"] = np.ascontiguousarray(ps_[:, b])
        maps.append(m)
    res = run_bass_kernel_spmd(nc, maps, core_ids=list(range(n)))
    return np.stack([np.asarray(res.results[b]["out"]) for b in range(n)]).astype(np.float32)
```

```python
import numpy as np
from contextlib import ExitStack
import concourse.bass as bass
import concourse.mybir as mybir
from concourse.bass_utils import run_bass_kernel_spmd

dt = mybir.dt
F32, BF16, I32 = dt.float32, dt.bfloat16, dt.int32
AF = mybir.ActivationFunctionType
ALU = mybir.AluOpType
AX = mybir.AxisListType

SPARSE_PE_INC = False
SAME_ENGINE_SYNC = True


class Region:
    __slots__ = ("name", "writer", "readers", "dma_sem", "dma_n", "dma_base", "uid", "excl", "dma_q")
    _next = [0]

    def __init__(self, name):
        self.name = name
        self.writer = None
        self.readers = {}
        self.dma_sem = None
        self.dma_n = 0
        self.dma_base = 0
        self.excl = False
        Region._next[0] += 1
        self.uid = Region._next[0]


class V:
    __slots__ = ("ap", "regs")

    def __init__(self, ap, regs):
        self.ap = ap
        self.regs = regs

    def __getitem__(self, idx):
        return V(self.ap[idx], self.regs)

    def bitcast(self, d):
        return V(self.ap.bitcast(d), self.regs)

    def bcast(self, shape):
        return V(self.ap.broadcast_to(shape), self.regs)


class Tile:
    _n = [0]

    def __init__(self, prog, name, shape, dtype, space="sbuf", nreg=1):
        Tile._n[0] += 1
        name = "%s_%d" % (name, Tile._n[0])
        self.name = name
        self.shape = shape
        if space == "sbuf":
            self.t = prog.es.enter_context(prog.nc.sbuf_tensor(name, shape, dtype))
        elif space == "psum":
            self.t = prog.es.enter_context(prog.nc.psum_tensor(name, shape, dtype))
        else:
            self.t = space
        self.regs = [Region(f"{name}.{i}") for i in range(nreg)]
        if space == "psum":
            for r in self.regs:
                r.excl = True
        prog.scope_regions[-1].extend(self.regs)

    def __getitem__(self, idx):
        return V(self.t[idx], self.regs)

    def sub(self, j):
        return V(self.t[:, j], [self.regs[j]])

    def reg(self, j, idx):
        return V(self.t[idx], [self.regs[j]])


ENG = ("pe", "act", "dve", "pool", "sp")


class Prog:
    def __init__(self, nc, es):
        self.nc = nc
        self.es = es
        self.eng = {"pe": nc.tensor, "act": nc.scalar, "dve": nc.vector, "pool": nc.gpsimd, "sp": nc.sync}
        self.sem = {e: es.enter_context(nc.semaphore("sem_" + e)) for e in ENG}
        self.cnt = {e: 0 for e in ENG}
        self.waited = {e: {f: 0 for f in ENG} for e in ENG}
        self.dma_waited = {e: {} for e in ENG}
        self.es0 = es
        self.scope_regions = [[]]
        self.sem_pool = {'pool': [], 'sp': [], 'act': []}
        self.n_dsem = 0
        self.dma_regions = []
        self.n_ops = 0
        self.n_waits = 0
        self.mute = None

    def _collect(self, e, reads, writes):
        deps = {}
        dmadeps = {}

        def add(w):
            if w is None:
                return
            if w[0] == "dma":
                r, n = w[1], w[2]
                k = r.uid
                if k not in dmadeps or dmadeps[k][1] < n:
                    dmadeps[k] = (r, n)
            else:
                f, i = w
                if f == e and (e == "pe" or not SAME_ENGINE_SYNC):
                    return
                if deps.get(f, 0) < i:
                    deps[f] = i

        for v in reads:
            for r in v.regs:
                add(r.writer)
                if r.excl:
                    for k, val in r.readers.items():
                        if not isinstance(k, tuple) and k != e:
                            add((k, val))
        for v in writes:
            for r in v.regs:
                add(r.writer)
                for k, val in r.readers.items():
                    if isinstance(k, tuple):
                        add(("dma", val[0], val[1]))
                    else:
                        add((k, val))
        return deps, dmadeps

    def _emit_waits(self, e, deps, dmadeps):
        eng = self.eng[e]
        for f, i in deps.items():
            if self.waited[e][f] >= i:
                continue
            if f == e and i < self.cnt[e]:
                pass
            eng.wait_ge(self.sem[f], i)
            self.n_waits += 1
            self.waited[e][f] = i
        for k, (r, n) in dmadeps.items():
            if self.dma_waited[e].get(k, 0) >= n:
                continue
            eng.wait_ge(r.dma_sem, r.dma_base + 16 * n)
            self.n_waits += 1
            self.dma_waited[e][k] = n

    def op(self, e, fn, reads=(), writes=(), inc=True):
        if self.mute is not None and self.mute[0]:
            return None
        deps, dmadeps = self._collect(e, reads, writes)
        self._emit_waits(e, deps, dmadeps)
        inst = fn(self.eng[e])
        if inc:
            self.cnt[e] += 1
            n = self.cnt[e]
            inst.then_inc(self.sem[e], 1)
        else:
            n = self.cnt[e] + 1
        for v in reads:
            for r in v.regs:
                if r.readers.get(e, 0) < n:
                    r.readers[e] = n
        for v in writes:
            for r in v.regs:
                r.writer = (e, n)
                r.readers = {}
        self.n_ops += 1
        return inst

    def dma(self, q, out, in_, sb, **kw):
        if self.mute is not None and self.mute[0]:
            return None
        deps, dmadeps = self._collect(q, [in_], [out])
        self._emit_waits(q, deps, dmadeps)
        r0 = sb.regs[0]
        if r0.dma_sem is None:
            r0.dma_q = q
            if self.sem_pool[q]:
                r0.dma_sem, r0.dma_base = self.sem_pool[q].pop()
            else:
                r0.dma_sem = self.es0.enter_context(self.nc.semaphore("dsem%d" % self.n_dsem))
                self.n_dsem += 1
                r0.dma_base = 0
            self.dma_regions.append(r0)
        assert r0.dma_q == q, (r0.name, r0.dma_q, q)
        inst = self.eng[q].dma_start(out=out.ap, in_=in_.ap, **kw)
        inst.then_inc(r0.dma_sem, 16)
        r0.dma_n += 1
        n = r0.dma_n
        for r in in_.regs:
            r.readers[("dma", r0.uid)] = (r0, n)
        for r in out.regs:
            r.writer = ("dma", r0, n)
            r.readers = {}
        return inst

    def push_scope(self):
        self.scope_regions.append([])

    def pop_scope(self):
        for r in self.scope_regions.pop():
            if r.dma_sem is not None:
                self.sem_pool[r.dma_q].append((r.dma_sem, r.dma_base + 16 * r.dma_n))
                self.dma_regions.remove(r)
                r.dma_sem = None

    def barrier(self):
        for e in ENG:
            deps = {f: self.cnt[f] for f in ENG if (f != e or e != 'pe') and self.cnt[f] > 0}
            dmadeps = {r.uid: (r, r.dma_n) for r in self.dma_regions}
            self._emit_waits(e, deps, dmadeps)

    def mm(self, out, lhsT, rhs, start=True, stop=True, **kw):
        return self.op("pe", lambda g: g.matmul(out.ap, lhsT.ap, rhs.ap, start=start, stop=stop, **kw),
                       reads=[lhsT, rhs] + ([] if start else [out]), writes=[out], inc=(stop or not SPARSE_PE_INC))

    def tr(self, out, in_, ident):
        return self.op("pe", lambda g: g.transpose(out.ap, in_.ap, ident.ap), reads=[in_, ident], writes=[out])

    def act(self, out, in_, func, bias=None, scale=None, accum=None, e="act"):
        kw = {}
        reads = [in_]
        if bias is not None:
            if isinstance(bias, V):
                kw["bias"] = bias.ap
                reads.append(bias)
            else:
                kw["bias"] = bias
        if scale is not None:
            if isinstance(scale, V):
                kw["scale"] = scale.ap
                reads.append(scale)
            else:
                kw["scale"] = scale
        writes = [out]
        if accum is not None:
            kw["accum_out"] = accum.ap
            writes.append(accum)
        return self.op(e, lambda g: g.activation(out.ap, in_.ap, func, **kw), reads=reads, writes=writes)

    def tt(self, out, a, b, op, e="dve"):
        return self.op(e, lambda g: g.tensor_tensor(out.ap, a.ap, b.ap, op), reads=[a, b], writes=[out])

    def ts(self, out, a, s1, s2, op0, op1=None, e="dve", accum=None):
        reads = [a]
        s1a, s2a = s1, s2
        if isinstance(s1, V):
            reads.append(s1)
            s1a = s1.ap
        if isinstance(s2, V):
            reads.append(s2)
            s2a = s2.ap
        kw = {}
        writes = [out]
        if accum is not None:
            kw["accum_out"] = accum.ap
            writes.append(accum)
        if op1 is None:
            return self.op(e, lambda g: g.tensor_scalar(out.ap, a.ap, s1a, s2a, op0, **kw), reads=reads, writes=writes)
        return self.op(e, lambda g: g.tensor_scalar(out.ap, a.ap, s1a, s2a, op0, op1, **kw), reads=reads, writes=writes)

    def stt(self, out, a, s, b, op0, op1, e="dve", accum=None):
        reads = [a, b]
        sa = s
        if isinstance(s, V):
            reads.append(s)
            sa = s.ap
        kw = {}
        writes = [out]
        if accum is not None:
            kw["accum_out"] = accum.ap
            writes.append(accum)
        return self.op(e, lambda g: g.scalar_tensor_tensor(out.ap, a.ap, sa, b.ap, op0, op1, **kw), reads=reads, writes=writes)

    def copy(self, out, in_, e="dve"):
        if e == "act":
            return self.op(e, lambda g: g.copy(out.ap, in_.ap), reads=[in_], writes=[out])
        return self.op(e, lambda g: g.tensor_copy(out.ap, in_.ap), reads=[in_], writes=[out])

    def memset(self, out, val, e="pool"):
        return self.op(e, lambda g: g.memset(out.ap, val), reads=[], writes=[out])

    def reduce(self, out, in_, op, axis=AX.X, e="dve", **kw):
        return self.op(e, lambda g: g.tensor_reduce(out.ap, in_.ap, axis, op, **kw), reads=[in_], writes=[out])

    def recip(self, out, in_, e="dve"):
        return self.op(e, lambda g: g.reciprocal(out.ap, in_.ap), reads=[in_], writes=[out])

    def finish(self):
        self.barrier()


SEQ = 2048
DM = 1024
NT = 16
DEPTH = 2
import os
NCHUNK = int(os.environ.get('NCHUNK', '16'))
S5_STOP = int(os.environ.get('S5_STOP', '99'))
GLA_STOP = int(os.environ.get('GLA_STOP', '0'))
S5_ORDER = int(os.environ.get('S5_ORDER', '0'))
GLA_VAR = int(os.environ.get('GLA_VAR', '0'))
PHASES = os.environ.get('PHASES', 'gla,s5,ml').split(',')
OFF = dict(gq=0, gk=256, gv=512, ga=1024, gg=1040, su=1552, mq=2064, mk=2320, mv=2576, mi=3088, mf=3092, mo=3096, gate=3608, end=6680)
DN_ALPHA = (2.0 * DEPTH) ** 0.25
LN_EPS = 1e-5

INPUT_SHAPES = {
    'x': (SEQ, DM), 'p': (2, SEQ, 256), 'w_in': (2, 1024, 6680), 'b_in': (2, 6680), 'gla_w_a2': (2, 16, 256),
    'gla_b_a2': (2, 256), 'gla_norm_g': (2, 512), 's5_a_re': (2, 32, 64), 's5_a_im': (2, 32, 64), 's5_log_dt': (2, 32),
    's5_b_re': (2, 32, 64, 16), 's5_b_im': (2, 32, 64, 16), 's5_c_re': (2, 32, 16, 64), 's5_c_im': (2, 32, 16, 64),
    's5_d': (2, 512), 's5_w_glu': (2, 512, 512), 's5_b_glu': (2, 512), 'ml_conv_w': (2, 4, 512), 'ml_conv_b': (2, 512),
    'ml_norm_g': (2, 512), 'w_up': (2, 3, 512, 1024), 'w_o': (2, 1024, 1024), 'ln1_g': (2, 1024), 'ln1_b': (2, 1024),
    'ffn_wg': (1, 1024, 2816), 'ffn_wu': (1, 1024, 2816), 'ffn_wd': (1, 2816, 1024), 'moe_router': (1, 1024, 8),
    'moe_router_b': (1, 8), 'moe_wg': (1, 8, 1024, 3584), 'moe_wu': (1, 8, 1024, 3584), 'moe_wd': (1, 8, 3584, 1024),
    'ple_w_gate': (2, 1024, 1024), 'ple_w_proj': (2, 256, 1024), 'ln2_g': (2, 1024), 'ln2_b': (2, 1024),
}


class Scope:
    def __init__(self, P):
        self.P = P

    def __enter__(self):
        self.es = ExitStack()
        self.es.__enter__()
        self.saved = self.P.es
        self.P.es = self.es
        self.P.push_scope()
        return self

    def __exit__(self, *a):
        self.P.barrier()
        self.P.pop_scope()
        self.P.es = self.saved
        return self.es.__exit__(*a)


def D(ap):
    return V(ap, [])


class K:
    pass


def setup_consts(P, k):
    k.ones = Tile(P, "ones", [128, 128], F32)
    k.ident = Tile(P, "ident", [128, 128], F32)
    k.identb = Tile(P, "identb", [128, 128], BF16)
    k.onesb = Tile(P, "onesb", [1, 512], BF16)
    k.mask01 = Tile(P, "mask01", [128, 128], F32)
    k.mask01b = Tile(P, "mask01b", [128, 128], BF16)
    k.triu_s = Tile(P, "triu_s", [128, 128], F32)
    k.tril_s = Tile(P, "tril_s", [128, 128], F32)
    P.memset(k.ones[:], 1.0)
    P.op("pool", lambda g: g.affine_select(k.ident[:].ap, k.ones[:, 0:128].ap, [[-1, 128]], ALU.is_equal, 0.0,
                                           base=0, channel_multiplier=1), reads=[k.ones[:]], writes=[k.ident[:]])
    P.copy(k.identb[:], k.ident[:], e="pool")
    P.memset(k.onesb[:], 1.0)
    P.op("pool", lambda g: g.affine_select(k.mask01[:].ap, k.ones[:, 0:128].ap, [[1, 128]], ALU.is_ge, 0.0,
                                           base=0, channel_multiplier=-1), reads=[k.ones[:]], writes=[k.mask01[:]])
    P.copy(k.mask01b[:], k.mask01[:], e="pool")
    P.ts(k.triu_s[:], k.mask01[:], -1.0 / 16, None, ALU.mult, e="pool")
    P.ts(k.tril_s[:], k.mask01[:], 1.0 / 16, -1.0 / 16, ALU.mult, ALU.add, e="pool")
    k.ps = [Tile(P, f"ps{i}", [128, 512], F32, space="psum") for i in range(8)]


def load_x(P, k, x_dram):
    for j in range(NT):
        P.dma("sp", k.xres.sub(j), D(x_dram[j * 128:(j + 1) * 128, :]), k.xres.sub(j))
        make_xT(P, k, j)


def make_xT(P, k, j, xtf=None, banks=(0, 1)):
    for half in range(2):
        pst = k.ps[banks[half]]
        for q in range(4):
            kt = half * 4 + q
            P.tr(pst[:, q * 128:(q + 1) * 128], k.xres.sub(j)[:, kt * 128:(kt + 1) * 128], k.ident[:])
        dst = k.xT.reg(j, (slice(None), slice(half * 4, half * 4 + 4), slice(j * 128, (j + 1) * 128)))
        src = pst[:].ap.rearrange("p (q t) -> p q t", q=4)
        srcv = V(src, pst.regs)
        P.copy(dst, srcv, e="act" if half == 0 else "dve")
        if xtf is not None:
            P.copy(xtf[:, half * 4:half * 4 + 4, :], srcv, e="dve" if half == 0 else "act")


def proj_fm(P, k, ps_out, W, wc, brow, bc, tsl, n):
    P.mm(ps_out, brow[0:1, bc], k.ones[0:1, 0:n], start=True, stop=False)
    for kt in range(8):
        P.mm(ps_out, W[:, kt, wc], k.xT[:, kt, tsl], start=False, stop=(kt == 7))


def proj_tm(P, k, ps_out, W, wc, brow, bc, tsl):
    P.mm(ps_out, k.ones[0:1, 0:128], brow[0:1, bc], start=True, stop=False)
    for kt in range(8):
        P.mm(ps_out, k.xT[:, kt, tsl], W[:, kt, wc], start=False, stop=(kt == 7))


def rstd_from_var(P, out, var, eps, tmp):
    P.ts(tmp, var, eps, None, ALU.add)
    P.act(tmp, tmp, AF.Ln)
    P.act(out, tmp, AF.Exp, scale=-0.5)


def gla_phase(P, k, L, w):
    ps = k.ps
    with Scope(P):
        WG = Tile(P, "WG", [128, 8, 1552], BF16)
        brow = Tile(P, "g_brow", [1, 1552], BF16)
        wa2 = Tile(P, "wa2", [16, 256], F32)
        ba2 = Tile(P, "ba2", [1, 256], F32)
        ng = Tile(P, "g_ng", [128, 512], F32)
        P.dma("pool", WG[:], D(w['w_in'][L, :, 0:1552].rearrange("(kt p) n -> p kt n", p=128)), WG[:])
        P.dma("pool", brow[:], D(w['b_in'][L:L + 1, 0:1552]), brow[:])
        P.dma("sp", wa2[:], D(w['gla_w_a2'][L]), wa2[:])
        P.dma("sp", ba2[:], D(w['gla_b_a2'][L:L + 1, :]), ba2[:])
        P.dma("sp", ng[:], D(w['gla_norm_g'][L].partition_broadcast(128)), ng[:])
        qk_raw = Tile(P, "g_qkraw", [128, 4, 512], BF16)
        alr_all = Tile(P, "g_alr", [16, 512], F32)
        sp = Tile(P, "g_sp", [128, 256], F32)
        eT = Tile(P, "g_eT", [128, 2, 128], F32)
        einvT = Tile(P, "g_einvT", [128, 2, 128], F32)
        erev = Tile(P, "g_erev", [128, 256], F32)
        dec = [Tile(P, f"g_dec{i}", [128, 2], F32) for i in range(2)]
        qdT = [Tile(P, f"g_qdT{i}", [128, 2, 128], BF16) for i in range(2)]
        kiT = [Tile(P, f"g_kiT{i}", [128, 2, 128], BF16) for i in range(2)]
        ktail = [Tile(P, f"g_ktail{i}", [128, 256], BF16) for i in range(2)]
        vbf = [Tile(P, f"g_vbf{i}", [128, 512], BF16) for i in range(2)]
        sg = [Tile(P, f"g_sg{i}", [128, 512], BF16) for i in range(2)]
        sgf = Tile(P, "g_sgf", [128, 512], F32)
        attT = Tile(P, "g_attT", [128, 4, 128], BF16)
        S32 = Tile(P, "g_S32", [128, 2, 128], F32)
        Sbf = Tile(P, "g_Sbf", [128, 2, 128], BF16)
        st = Tile(P, "g_st", [128, 4, 6], F32)
        mv = Tile(P, "g_mv", [128, 4, 2], F32)
        rstd = Tile(P, "g_rstd", [128, 4], F32)
        tmp4 = Tile(P, "g_tmp4", [128, 4], F32)
        yn = Tile(P, "g_yn", [128, 512], F32)
        ybf = Tile(P, "g_ybf", [128, 512], BF16)
        P.memset(S32[:], 0.0)
        P.memset(Sbf[:], 0.0)

        def bias_fm(ps_out, bc, n):
            P.mm(ps_out, brow[0:1, bc], k.onesb[0:1, 0:n], start=True, stop=False)

        def bias_tm(ps_out, bc):
            P.mm(ps_out, k.onesb[0:1, 0:128], brow[0:1, bc], start=True, stop=False)

        def stage_a(c):
            par = c % 2
            tsl = slice(c * 128, (c + 1) * 128)
            off = (c % 4) * 128
            osl = slice(off, off + 128)
            if c % 4 == 0:
                t4 = slice(c * 128, c * 128 + 512)
                for i in range(4):
                    cols = slice(i * 128, (i + 1) * 128)
                    bias_fm(ps[0][:], cols, 512)
                    for kt in range(8):
                        P.mm(ps[0][:], WG[:, kt, cols], k.xT[:, kt, t4], start=False, stop=(kt == 7))
                    P.copy(qk_raw[:, i, :], ps[0][:], e="act")
                ac = slice(OFF['ga'], OFF['ga'] + 16)
                bias_fm(ps[0][0:16, :], ac, 512)
                for kt in range(8):
                    P.mm(ps[0][0:16, :], WG[:, kt, ac], k.xT[:, kt, t4], start=False, stop=(kt == 7))
                P.copy(alr_all[:], ps[0][0:16, :], e="act")
            P.mm(ps[1][:, 0:256], k.ones[0:1, 0:128], ba2[:], start=True, stop=False)
            P.mm(ps[1][:, 0:256], alr_all[:, osl], wa2[:], start=False, stop=True)
            P.act(sp[:], ps[1][:, 0:256], AF.Exp, scale=-1.0)
            P.act(sp[:], sp[:], AF.Ln, bias=1.0)
            P.mm(ps[1][:, 256:512], k.tril_s[:], sp[:], start=True, stop=True)
            for i in range(2):
                P.mm(ps[2][:, i * 128:(i + 1) * 128], sp[:, i * 128:(i + 1) * 128], k.triu_s[:], start=True, stop=True)
            cumT = v3(ps[2][:, 0:256], "p (i t) -> p i t", i=2)
            P.act(eT[:], cumT, AF.Exp)
            P.act(einvT[:], cumT, AF.Exp, scale=-1.0)
            P.act(erev[:], ps[1][:, 256:512], AF.Exp)
            P.copy(dec[par][:], eT[:, :, 127], e="act")
            P.stt(qdT[par][:], qk_raw[:, 0:2, osl], 0.125, eT[:], ALU.mult, ALU.mult)
            P.tt(kiT[par][:], qk_raw[:, 2:4, osl], einvT[:], ALU.mult, e="pool")
            kc = slice(OFF['gk'], OFF['gk'] + 256)
            bias_tm(ps[2][:, 256:512], kc)
            for kt in range(8):
                P.mm(ps[2][:, 256:512], k.xT[:, kt, tsl], WG[:, kt, kc], start=False, stop=(kt == 7))
            vc = slice(OFF['gv'], OFF['gv'] + 512)
            bias_tm(ps[3][:], vc)
            for kt in range(8):
                P.mm(ps[3][:], k.xT[:, kt, tsl], WG[:, kt, vc], start=False, stop=(kt == 7))
            gc = slice(OFF['gg'], OFF['gg'] + 512)
            bias_tm(ps[4][:], gc)
            for kt in range(8):
                P.mm(ps[4][:], k.xT[:, kt, tsl], WG[:, kt, gc], start=False, stop=(kt == 7))
            P.tt(ktail[par][:], ps[2][:, 256:512], erev[:], ALU.mult)
            P.copy(vbf[par][:], ps[3][:], e="act")
            P.act(sgf[:], ps[4][:], AF.Silu)
            P.tt(sg[par][:], sgf[:], ng[:], ALU.mult, e="pool")

        def stage_b(c):
            par = c % 2
            tsl = slice(c * 128, (c + 1) * 128)
            abank = {0: (ps[5], 0), 2: (ps[5], 128), 1: (ps[6], 0), 3: (ps[6], 128)}
            for h in (0, 2, 1, 3):
                hp, ho = h // 2, (h % 2) * 64
                bk, co = abank[h]
                P.mm(bk[:, co:co + 128], kiT[par][ho:ho + 64, hp, :], qdT[par][ho:ho + 64, hp, :], start=True, stop=True)
            for h in range(4):
                bk, co = abank[h]
                P.tt(attT[:, h, :], bk[:, co:co + 128], k.mask01[:], ALU.mult)
            for h in range(4):
                hp, ho = h // 2, (h % 2) * 64
                P.mm(ps[7][:, h * 128:(h + 1) * 128], attT[:, h, :], vbf[par][:, h * 128:(h + 1) * 128], start=True, stop=False)
                P.mm(ps[7][:, h * 128:(h + 1) * 128], qdT[par][ho:ho + 64, hp, :], Sbf[ho:ho + 64, hp, :], start=False, stop=True)
            sbank = [ps[5], ps[6]]
            for hp in range(2):
                P.mm(sbank[hp][:, 256:512], ktail[par][:, hp * 128:(hp + 1) * 128], vbf[par][:, hp * 256:(hp + 1) * 256], start=True, stop=True)
            for hp in range(2):
                for hh in range(2):
                    ho = hh * 64
                    P.stt(S32[ho:ho + 64, hp, :], S32[ho:ho + 64, hp, :], dec[par][ho:ho + 64, hp:hp + 1],
                          sbank[hp][ho:ho + 64, 256 + hh * 128:256 + (hh + 1) * 128], ALU.mult, ALU.add)
            P.copy(Sbf[:], S32[:], e="pool")
            for h in range(4):
                P.op("dve", lambda g, h=h: g.bn_stats(st[:, h, :].ap, ps[7][:, h * 128:(h + 1) * 128].ap),
                     reads=[ps[7][:]], writes=[st[:]])
                P.op("dve", lambda g, h=h: g.bn_aggr(mv[:, h, :].ap, st[:, h, :].ap), reads=[st[:]], writes=[mv[:]])
            rstd_from_var(P, rstd[:], mv[:, :, 1], LN_EPS, tmp4[:])
            for h in range(4):
                P.ts(yn[:, h * 128:(h + 1) * 128], ps[7][:, h * 128:(h + 1) * 128], mv[:, h, 0:1], rstd[:, h:h + 1],
                     ALU.subtract, ALU.mult)
            P.tt(ybf[:], yn[:], sg[par][:], ALU.mult, e="pool")
            psb = V(ps[5][:].ap.bitcast(BF16), ps[5].regs)
            for q in range(4):
                P.tr(psb[:, q * 128:(q + 1) * 128], ybf[:, q * 128:(q + 1) * 128], k.identb[:])
            P.copy(k.yT[0][:, :, tsl],
                   V(psb[:, 0:512].ap.rearrange("p (q t) -> p q t", q=4), ps[5].regs), e="act")

        stage_a(0)
        for c in range(NCHUNK):
            if c + 1 < NCHUNK:
                stage_a(c + 1)
            stage_b(c)


def v3(v, pat, **kw):
    return V(v.ap.rearrange(pat, **kw), v.regs)


def mlstm_phase(P, k, L, w):
    ps = k.ps
    o0 = OFF['mq']
    with Scope(P):
        WM = Tile(P, "WM", [128, 8, 1544], BF16)
        brow = Tile(P, "m_brow", [1, 1544], BF16)
        gb8 = Tile(P, "m_gb8", [1, 8], F32)
        WIF = Tile(P, "WIF", [128, 8, 512], BF16)
        browIF = Tile(P, "m_browIF", [1, 512], F32)
        cw = Tile(P, "m_cw", [128, 4, 4], F32)
        cb = Tile(P, "m_cb", [128, 4], F32)
        ngm = Tile(P, "m_ng", [128, 512], F32)
        P.dma("sp", gb8[:], D(w['b_in'][L:L + 1, o0 + 1024:o0 + 1032]), gb8[:])
        P.dma("pool", WM[:], D(w['w_in'][L, :, o0:o0 + 1544].rearrange("(kt p) n -> p kt n", p=128)), WM[:])
        P.dma("pool", brow[:], D(w['b_in'][L:L + 1, o0:o0 + 1544]), brow[:])
        for tap in range(4):
            P.dma("sp", cw[:, tap, :], D(w['ml_conv_w'][L, tap].rearrange("(j p) -> p j", p=128)), cw[:], allow_slow_non_contiguous=True)
        P.dma("sp", cb[:], D(w['ml_conv_b'][L].rearrange("(j p) -> p j", p=128)), cb[:], allow_slow_non_contiguous=True)
        P.dma("sp", ngm[:], D(w['ml_norm_g'][L].partition_broadcast(128)), ngm[:])
        for gi in range(2):
            for h in range(4):
                col = 1024 + gi * 4 + h
                dst = slice(gi * 256 + h * 64, gi * 256 + (h + 1) * 64)
                P.copy(WIF[:, :, dst], WM[:, :, col:col + 1].bcast([128, 8, 64]), e="dve")
                P.copy(browIF[0:1, dst], gb8[0:1, gi * 4 + h:gi * 4 + h + 1].bcast([1, 64]), e="dve")
        bif_hi = Tile(P, "m_bifhi", [1, 512], BF16)
        bif_lo = Tile(P, "m_biflo", [1, 512], BF16)
        P.copy(bif_hi[:], browIF[:], e="dve")
        P.tt(browIF[:], browIF[:], bif_hi[:], ALU.subtract)
        P.copy(bif_lo[:], browIF[:], e="dve")
        raw = Tile(P, "m_raw4", [128, 4, 515], BF16)
        gif = Tile(P, "m_gif", [128, 4, 512], F32)
        acc = Tile(P, "m_acc", [128, 4, 128], F32)
        kf = Tile(P, "m_kf", [128, 2, 128], F32)
        spf = Tile(P, "m_spf", [128, 2, 128], F32)
        halo = Tile(P, "m_halo", [128, 4, 3], BF16)
        ncum = Tile(P, "m_ncum", [128, 2, 128], F32)
        aT = Tile(P, "m_aT", [128, 2, 128], F32)
        ksc = spf
        clT = Tile(P, "m_clT", [128, 2, 128], F32)
        gfacf = Tile(P, "m_gfacf", [128, 512], BF16)
        mst = Tile(P, "m_mst", [128, 2], F32)
        sm = Tile(P, "m_sm", [128, 16], F32)
        qfT = [Tile(P, f"m_qfT{i}", [128, 2, 128], BF16) for i in range(2)]
        kpT = [Tile(P, f"m_kpT{i}", [128, 2, 128], BF16) for i in range(2)]
        kptok = [Tile(P, f"m_kptok{i}", [128, 256], BF16) for i in range(2)]
        clampv = [Tile(P, f"m_clampv{i}", [128, 4], F32) for i in range(2)]
        vaug = [Tile(P, f"m_vaug{i}", [128, 4, 129], BF16) for i in range(2)]
        gfac = [Tile(P, f"m_gfac{i}", [128, 512], BF16) for i in range(2)]
        wdec = [Tile(P, f"m_wdec{i}", [128, 2], F32) for i in range(2)]
        C32 = Tile(P, "m_C32", [128, 2, 129], F32)
        Cbf = Tile(P, "m_Cbf", [128, 2, 129], BF16)
        attTb = Tile(P, "m_attTb", [128, 4, 128], BF16)
        st = Tile(P, "m_st", [128, 4, 6], F32)
        mv = Tile(P, "m_mv", [128, 4, 2], F32)
        sm2 = Tile(P, "m_sm2", [128, 6, 4], F32)
        yn = Tile(P, "m_yn", [128, 512], F32)
        ybf = Tile(P, "m_ybf", [128, 512], BF16)
        P.memset(raw[:], 0.0)
        P.memset(C32[:], 0.0)
        P.memset(mst[:], 0.0)
        for i in range(2):
            P.memset(vaug[i][:], 1.0)

        def stage_a(c):
            par = c % 2
            tsl = slice(c * 128, (c + 1) * 128)
            off = (c % 4) * 128
            osl = slice(off, off + 128)
            if c % 4 == 0:
                t4 = slice(c * 128, c * 128 + 512)
                P.copy(halo[:], raw[:, :, 512:515], e="pool")
                for i in range(8):
                    cols = slice((i % 4) * 128, (i % 4 + 1) * 128)
                    bank = ps[i % 2]
                    if i < 4:
                        P.mm(bank[:], brow[0:1, cols], k.onesb[0:1, 0:512], start=True, stop=False)
                        Wsrc = WM
                    else:
                        P.mm(bank[:], bif_hi[0:1, cols], k.onesb[0:1, 0:512], start=True, stop=False)
                        P.mm(bank[:], bif_lo[0:1, cols], k.onesb[0:1, 0:512], start=False, stop=False)
                        Wsrc = WIF
                    for kt in range(8):
                        P.mm(bank[:], Wsrc[:, kt, cols], k.xT[:, kt, t4], start=False, stop=(kt == 7))
                    if i < 4:
                        P.copy(raw[:, i, 3:515], bank[:], e="act")
                    else:
                        P.copy(gif[:, i - 4, :], bank[:], e="act")
                P.copy(raw[:, :, 0:3], halo[:], e="pool")
            for i in range(4):
                P.ts(acc[:, i, :], raw[:, i, off:off + 128], cw[:, 0, i:i + 1], cb[:, i:i + 1], ALU.mult, ALU.add)
                for tap in range(1, 4):
                    P.stt(acc[:, i, :], raw[:, i, off + tap:off + tap + 128], cw[:, tap, i:i + 1], acc[:, i, :], ALU.mult, ALU.add)
            P.act(qfT[par][:], acc[:, 0:2, :], AF.Silu)
            P.act(kf[:], acc[:, 2:4, :], AF.Silu)
            gi_ps = gif[:, 0:2, osl]
            gf_ps = gif[:, 2:4, osl]
            P.act(spf[:], gf_ps, AF.Exp, scale=-1.0)
            P.act(spf[:], spf[:], AF.Ln, bias=1.0)
            for i in range(2):
                P.op("dve", lambda g, i=i: g.tensor_tensor_scan(ncum[:, i, :].ap, k.ones[:, 0:128].ap, spf[:, i, :].ap, 0.0,
                                                                 ALU.mult, ALU.add),
                     reads=[k.ones[:], spf[:]], writes=[ncum[:]])
            P.tt(aT[:], gi_ps, ncum[:], ALU.add)
            P.reduce(sm[:, 0:2], aT[:], ALU.max)
            P.tt(sm[:, 2:4], sm[:, 0:2], mst[:], ALU.max)
            P.ts(sm[:, 4:6], sm[:, 2:4], -1.0, None, ALU.mult)
            P.tt(sm[:, 8:10], mst[:], sm[:, 2:4], ALU.subtract)
            P.act(wdec[par][:], sm[:, 8:10], AF.Exp)
            for i in range(2):
                P.act(ksc[:, i, :], aT[:, i, :], AF.Exp, bias=sm[:, 4 + i:5 + i])
                P.act(clT[:, i, :], ncum[:, i, :], AF.Exp, bias=sm[:, 4 + i:5 + i])
            P.stt(kpT[par][:], kf[:], 0.125, ksc[:], ALU.mult, ALU.mult)
            P.tt(mst[:], sm[:, 2:4], ncum[:, :, 127], ALU.subtract)
            for i in range(2):
                P.tr(ps[1][:, i * 128:(i + 1) * 128], clT[:, i, :], k.ident[:])
            psb1 = V(ps[1][:].ap.bitcast(BF16), ps[1].regs)
            for i in range(2):
                P.tr(psb1[:, 512 + i * 128:512 + (i + 1) * 128], kpT[par][:, i, :], k.identb[:])
            P.copy(clampv[par][:], v3(ps[1][:, 0:256], "p (h r) -> p h r", r=64)[:, :, 0], e="act")
            P.copy(kptok[par][:], psb1[:, 512:768], e="act")
            vc = slice(512, 1024)
            P.mm(ps[2][:], k.onesb[0:1, 0:128], brow[0:1, vc], start=True, stop=False)
            for kt in range(8):
                P.mm(ps[2][:], k.xT[:, kt, tsl], WM[:, kt, vc], start=False, stop=(kt == 7))
            oc = slice(1032, 1544)
            P.mm(ps[3][:], k.onesb[0:1, 0:128], brow[0:1, oc], start=True, stop=False)
            for kt in range(8):
                P.mm(ps[3][:], k.xT[:, kt, tsl], WM[:, kt, oc], start=False, stop=(kt == 7))
            P.copy(vaug[par][:, :, 0:128], v3(ps[2][:], "p (h e) -> p h e", h=4), e="act")
            P.act(gfacf[:], ps[3][:], AF.Sigmoid)
            P.tt(gfac[par][:], gfacf[:], ngm[:], ALU.mult, e="pool")

        def stage_b(c):
            par = c % 2
            tsl = slice(c * 128, (c + 1) * 128)
            for i in range(2):
                P.ts(C32[:, i, :], C32[:, i, :], wdec[par][:, i:i + 1], None, ALU.mult)
            P.copy(Cbf[:], C32[:], e="pool")
            abank = {0: (ps[4], 0), 2: (ps[4], 128), 1: (ps[5], 0), 3: (ps[5], 128)}
            for h in (0, 2, 1, 3):
                hp, ho = h // 2, (h % 2) * 64
                bk, co = abank[h]
                P.mm(bk[:, co:co + 128], kpT[par][ho:ho + 64, hp, :], qfT[par][ho:ho + 64, hp, :], start=True, stop=True)
            for h in range(4):
                bk, co = abank[h]
                P.tt(attTb[:, h, :], bk[:, co:co + 128], k.mask01[:], ALU.mult)
            oa = []
            for h in range(4):
                hp, ho = h // 2, (h % 2) * 64
                bank = ps[6] if h < 2 else ps[7]
                o_ = bank[:, (h % 2) * 129:(h % 2) * 129 + 129]
                oa.append(o_)
                P.mm(o_, attTb[:, h, :], vaug[par][:, h, :], start=True, stop=False)
                P.mm(o_, qfT[par][ho:ho + 64, hp, :], Cbf[ho:ho + 64, hp, :], start=False, stop=True)
            for hp in range(2):
                pss = ps[4 + hp]
                P.mm(pss[:, 0:258], kptok[par][:, hp * 128:(hp + 1) * 128],
                     v3(vaug[par][:, 2 * hp:2 * hp + 2, :], "p h e -> p (h e)"), start=True, stop=True)
            for hp in range(2):
                pss = ps[4 + hp]
                for hh in range(2):
                    ho = hh * 64
                    P.tt(C32[ho:ho + 64, hp, :], C32[ho:ho + 64, hp, :], pss[ho:ho + 64, hh * 129:(hh + 1) * 129], ALU.add)
            for h in range(4):
                P.act(sm2[:, 0, h:h + 1], oa[h][:, 128:129], AF.Abs)
            for h in range(4):
                P.op("dve", lambda g, h=h: g.bn_stats(st[:, h, :].ap, oa[h][:, 0:128].ap), reads=[oa[h]], writes=[st[:]])
                P.op("dve", lambda g, h=h: g.bn_aggr(mv[:, h, :].ap, st[:, h, :].ap), reads=[st[:]], writes=[mv[:]])
            P.tt(sm2[:, 0, :], sm2[:, 0, :], clampv[par][:], ALU.max)
            P.recip(sm2[:, 1, :], sm2[:, 0, :])
            P.tt(sm2[:, 2, :], sm2[:, 1, :], sm2[:, 1, :], ALU.mult)
            P.tt(sm2[:, 3, :], mv[:, :, 1], sm2[:, 2, :], ALU.mult)
            rstd_from_var(P, sm2[:, 4, :], sm2[:, 3, :], LN_EPS, sm2[:, 4, :])
            P.tt(sm2[:, 5, :], sm2[:, 4, :], sm2[:, 1, :], ALU.mult)
            for h in range(4):
                P.ts(yn[:, h * 128:(h + 1) * 128], oa[h][:, 0:128], mv[:, h, 0:1], sm2[:, 5, h:h + 1], ALU.subtract, ALU.mult)
            P.tt(ybf[:], yn[:], gfac[par][:], ALU.mult, e="pool")
            psb = V(ps[4][:].ap.bitcast(BF16), ps[4].regs)
            for q in range(4):
                P.tr(psb[:, q * 128:(q + 1) * 128], ybf[:, q * 128:(q + 1) * 128], k.identb[:])
            P.copy(k.yT[2][:, :, tsl],
                   V(psb[:, 0:512].ap.rearrange("p (q t) -> p q t", q=4), ps[4].regs), e="act")

        stage_a(0)
        for c in range(NCHUNK):
            if c + 1 < NCHUNK:
                stage_a(c + 1)
            stage_b(c)


import math
TWO_PI = 2.0 * math.pi


class StopBuild(Exception):
    pass


def stop_at(n):
    if S5_STOP <= n:
        MUTE[0] = True


MUTE = [False]


def s5_phase(P, k, L, w):
    ps = k.ps
    T16 = [128, 16]
    with Scope(P):
        ETre = Tile(P, "s_ETre", [128, 16, 128], BF16)
        ETim = Tile(P, "s_ETim", [128, 16, 128], BF16)
        Epre = Tile(P, "s_Epre", [128, 16, 128], BF16)
        Epim = Tile(P, "s_Epim", [128, 16, 128], BF16)
        lam_re = Tile(P, "s_lamre", T16, F32)
        lam_im = Tile(P, "s_lamim", T16, F32)
        with Scope(P):
            are = Tile(P, "s_are", T16, F32)
            aim = Tile(P, "s_aim", T16, F32)
            ldt = Tile(P, "s_ldt", T16, F32)
            P.dma("sp", are[:], D(w['s5_a_re'][L].rearrange("(j gl) p -> (gl p) j", gl=2)), are[:], allow_slow_non_contiguous=True)
            P.dma("sp", aim[:], D(w['s5_a_im'][L].rearrange("(j gl) p -> (gl p) j", gl=2)), aim[:], allow_slow_non_contiguous=True)
            ldt_src = w['s5_log_dt'][L].rearrange("(j gl) -> gl j", gl=2)
            for gl in range(2):
                P.dma("sp", ldt[gl * 64:(gl + 1) * 64, :], D(ldt_src[gl].partition_broadcast(64)), ldt[:], allow_slow_non_contiguous=True)
            sm = {n: Tile(P, "sq_" + n, T16, F32) for n in
                  ["dt", "re", "im", "mag", "magi", "t0", "t1", "sinv", "cosv", "li_re", "li_im", "cf_re", "cf_im", "pw_re", "pw_im", "q_re", "q_im"]}
            ni = Tile(P, "s_ni", T16, I32)
            e = "dve"
            P.act(sm["dt"][:], ldt[:], AF.Exp)
            P.tt(sm["re"][:], are[:], sm["dt"][:], ALU.mult)
            P.tt(sm["im"][:], aim[:], sm["dt"][:], ALU.mult)
            P.act(sm["mag"][:], sm["re"][:], AF.Exp)
            P.act(sm["magi"][:], sm["re"][:], AF.Exp, scale=-1.0)

            def wrap(t):
                P.ts(sm["t1"][:], t, math.pi, TWO_PI, ALU.is_gt, ALU.mult)
                P.tt(t, t, sm["t1"][:], ALU.subtract)
                P.ts(sm["t1"][:], t, -math.pi, TWO_PI, ALU.is_lt, ALU.mult)
                P.tt(t, t, sm["t1"][:], ALU.add)

            P.ts(sm["t0"][:], sm["im"][:], 1.0 / TWO_PI, None, ALU.mult)
            P.copy(ni[:], sm["t0"][:])
            P.copy(sm["t0"][:], ni[:])
            P.stt(sm["im"][:], sm["t0"][:], -TWO_PI, sm["im"][:], ALU.mult, ALU.add)
            wrap(sm["im"][:])
            P.act(sm["sinv"][:], sm["im"][:], AF.Sin)
            P.ts(sm["t0"][:], sm["im"][:], math.pi / 2, None, ALU.add)
            wrap(sm["t0"][:])
            P.act(sm["cosv"][:], sm["t0"][:], AF.Sin)
            P.tt(lam_re[:], sm["mag"][:], sm["cosv"][:], ALU.mult)
            P.tt(lam_im[:], sm["mag"][:], sm["sinv"][:], ALU.mult)
            P.tt(sm["li_re"][:], sm["magi"][:], sm["cosv"][:], ALU.mult)
            P.stt(sm["li_im"][:], sm["magi"][:], -1.0, sm["sinv"][:], ALU.mult, ALU.mult)
            P.ts(sm["q_re"][:], lam_re[:], -1.0, None, ALU.add)
            P.tt(sm["t0"][:], are[:], are[:], ALU.mult)
            P.tt(sm["t1"][:], aim[:], aim[:], ALU.mult)
            P.tt(sm["t0"][:], sm["t0"][:], sm["t1"][:], ALU.add)
            P.recip(sm["q_im"][:], sm["t0"][:])
            P.tt(sm["t0"][:], sm["q_re"][:], are[:], ALU.mult)
            P.tt(sm["t1"][:], lam_im[:], aim[:], ALU.mult)
            P.tt(sm["t0"][:], sm["t0"][:], sm["t1"][:], ALU.add)
            P.tt(sm["cf_re"][:], sm["t0"][:], sm["q_im"][:], ALU.mult)
            P.tt(sm["t0"][:], lam_im[:], are[:], ALU.mult)
            P.tt(sm["t1"][:], sm["q_re"][:], aim[:], ALU.mult)
            P.tt(sm["t0"][:], sm["t0"][:], sm["t1"][:], ALU.subtract)
            P.tt(sm["cf_im"][:], sm["t0"][:], sm["q_im"][:], ALU.mult)
            if S5_STOP <= 1:
                return
            Tre = Tile(P, "s_Tre", [128, 16, 128], F32)
            Tim = Tile(P, "s_Tim", [128, 16, 128], F32)
            tA = Tile(P, "s_tA", [128, 16, 64], F32)
            tB = Tile(P, "s_tB", [128, 16, 64], F32)

            def build_table(bre, bim):
                P.memset(Tre[:, :, 0:1], 1.0)
                P.memset(Tim[:, :, 0:1], 0.0)
                P.copy(Tre[:, :, 1], bre)
                P.copy(Tim[:, :, 1], bim)
                P.copy(sm["pw_re"][:], bre)
                P.copy(sm["pw_im"][:], bim)
                for kk in range(1, 7):
                    n = 1 << kk
                    P.tt(sm["t0"][:], sm["pw_re"][:], sm["pw_re"][:], ALU.mult)
                    P.tt(sm["t1"][:], sm["pw_im"][:], sm["pw_im"][:], ALU.mult)
                    P.tt(sm["q_re"][:], sm["pw_re"][:], sm["pw_im"][:], ALU.mult)
                    P.tt(sm["pw_re"][:], sm["t0"][:], sm["t1"][:], ALU.subtract)
                    P.ts(sm["pw_im"][:], sm["q_re"][:], 2.0, None, ALU.mult)
                    pr = v3(sm["pw_re"][:], "p (j o) -> p j o", o=1).bcast([128, 16, n])
                    pi_ = v3(sm["pw_im"][:], "p (j o) -> p j o", o=1).bcast([128, 16, n])
                    P.tt(tA[:, :, 0:n], Tre[:, :, 0:n], pr, ALU.mult)
                    P.tt(tB[:, :, 0:n], Tim[:, :, 0:n], pi_, ALU.mult, e="pool")
                    P.tt(Tre[:, :, n:2 * n], tA[:, :, 0:n], tB[:, :, 0:n], ALU.subtract)
                    P.tt(tA[:, :, 0:n], Tre[:, :, 0:n], pi_, ALU.mult)
                    P.tt(tB[:, :, 0:n], Tim[:, :, 0:n], pr, ALU.mult, e="pool")
                    P.tt(Tim[:, :, n:2 * n], tA[:, :, 0:n], tB[:, :, 0:n], ALU.add)

            build_table(lam_re[:], lam_im[:])
            P.copy(ETre[:], Tre[:], e="act")
            P.copy(ETim[:], Tim[:], e="act")
            build_table(sm["li_re"][:], sm["li_im"][:])
            cr = v3(sm["cf_re"][:], "p (j o) -> p j o", o=1)
            ci = v3(sm["cf_im"][:], "p (j o) -> p j o", o=1)
            for hh in range(2):
                sl = slice(hh * 64, (hh + 1) * 64)
                crb, cib = cr.bcast([128, 16, 64]), ci.bcast([128, 16, 64])
                P.tt(tA[:], Tre[:, :, sl], crb, ALU.mult)
                P.tt(tB[:], Tim[:, :, sl], cib, ALU.mult, e="pool")
                P.tt(tA[:], tA[:], tB[:], ALU.subtract)
                P.tt(tB[:], Tre[:, :, sl], cib, ALU.mult, e="pool")
                P.tt(Tim[:, :, sl], Tim[:, :, sl], crb, ALU.mult)
                P.tt(Tim[:, :, sl], Tim[:, :, sl], tB[:], ALU.add)
                P.copy(Tre[:, :, sl], tA[:])
            for (src, dst) in ((Tre, Epre), (Tim, Epim)):
                for g4 in range(4):
                    bank = ps[g4 % 2]
                    for jj in range(4):
                        P.tr(bank[:, jj * 128:(jj + 1) * 128], src[:, g4 * 4 + jj, :], k.ident[:])
                    P.copy(dst[:, g4 * 4:g4 * 4 + 4, :], v3(bank[:], "p (j q) -> p j q", j=4), e="act" if g4 % 2 == 0 else "dve")
        if S5_STOP <= 2:
            return
        BTre = Tile(P, "s_BTre", [128, 16, 128], BF16)
        BTim = Tile(P, "s_BTim", [128, 16, 128], BF16)
        CTre = Tile(P, "s_CTre", [128, 16, 128], BF16)
        CTimn = Tile(P, "s_CTimn", [128, 16, 128], BF16)
        Wu = Tile(P, "s_Wu", [128, 8, 512], BF16)
        Wglu = Tile(P, "s_Wglu", [128, 4, 512], BF16)
        browu = Tile(P, "s_browu", [1, 512], F32)
        bglu = Tile(P, "s_bglu", [128, 4], F32)
        dvec = Tile(P, "s_dvec", [128, 4], F32)
        o0 = OFF['su']
        P.dma("pool", Wu[:], D(w['w_in'][L, :, o0:o0 + 512].rearrange("(kt p) n -> p kt n", p=128)), Wu[:])
        P.dma("pool", Wglu[:], D(w['s5_w_glu'][L].rearrange("(kt p) n -> p kt n", p=128)), Wglu[:])
        P.dma("sp", browu[:], D(w['b_in'][L:L + 1, o0:o0 + 512]), browu[:])
        P.dma("sp", bglu[:], D(w['s5_b_glu'][L].rearrange("(j p) -> p j", p=128)), bglu[:], allow_slow_non_contiguous=True)
        P.dma("sp", dvec[:], D(w['s5_d'][L].rearrange("(j p) -> p j", p=128)), dvec[:], allow_slow_non_contiguous=True)
        with Scope(P):
            Bnat = Tile(P, "s_Bnat", [128, 16, 16], F32)
            Bpad = Tile(P, "s_Bpad", [128, 16, 128], F32)
            Cnat = Tile(P, "s_Cnat", [128, 4, 128], F32)
            Cfull = Tile(P, "s_Cfull", [128, 4, 128], F32)
            P.memset(Bpad[:], 0.0)
            P.memset(CTre[:], 0.0)
            P.memset(CTimn[:], 0.0)
            for (nm, BT) in (("s5_b_re", BTre), ("s5_b_im", BTim)):
                P.dma("sp", Bnat[:], D(w[nm][L].rearrange("g p n -> (g p) n").rearrange("(j q) n -> q j n", q=128)), Bnat[:])
                Bp4 = v3(Bpad[:], "q (kt jj) c -> q kt jj c", jj=4)
                Bn4 = v3(Bnat[:], "q (kt jj) n -> q kt jj n", jj=4)
                for jj in range(4):
                    for half in range(2):
                        c0 = (2 * jj + half) * 16
                        P.copy(Bp4[half * 64:(half + 1) * 64, :, jj, c0:c0 + 16], Bn4[half * 64:(half + 1) * 64, :, jj, :])
                for g4 in range(4):
                    bank = ps[g4 % 2]
                    for jj in range(4):
                        P.tr(bank[:, jj * 128:(jj + 1) * 128], Bpad[:, g4 * 4 + jj, :], k.ident[:])
                    P.copy(BT[:, g4 * 4:g4 * 4 + 4, :], v3(bank[:], "p (j q) -> p j q", j=4), e="act" if g4 % 2 == 0 else "dve")
            for (nm, CT, sgn) in (("s5_c_re", CTre, 1.0), ("s5_c_im", CTimn, -1.0)):
                csrc = w[nm][L].rearrange("(kt g8) n p -> (g8 n) kt p", g8=8)
                P.dma("sp", Cnat[:, :, 0:64], D(csrc), Cnat[:])
                P.dma("sp", Cnat[:, :, 64:128], D(csrc), Cnat[:])
                for kt in range(4):
                    P.tr(ps[2][:, kt * 128:(kt + 1) * 128], Cnat[:, kt, :], k.ident[:])
                P.copy(Cfull[:], v3(ps[2][:], "p (kt c) -> p kt c", kt=4), e="act")
                C4 = v3(CT[:], "q (kt jj) c -> q kt jj c", jj=4)
                for jj in range(4):
                    for half in range(2):
                        c0 = (2 * jj + half) * 16
                        P.ts(C4[half * 64:(half + 1) * 64, :, jj, c0:c0 + 16], Cfull[half * 64:(half + 1) * 64, :, c0:c0 + 16],
                             sgn, None, ALU.mult)
        if S5_STOP <= 3:
            return
        uT32s = [Tile(P, f"s_uT32{i}", [128, 4, 128], F32) for i in range(2)]
        uTbs = [Tile(P, f"s_uTb{i}", [128, 4, 128], BF16) for i in range(2)]
        bure = Tile(P, "s_bure", [128, 4, 128], F32)
        buim = Tile(P, "s_buim", [128, 4, 128], F32)
        t1 = Tile(P, "s_t1", [128, 4, 128], F32)
        t2 = Tile(P, "s_t2", [128, 4, 128], F32)
        t3 = Tile(P, "s_t3", [128, 4, 128], F32)
        t4 = Tile(P, "s_t4", [128, 4, 128], F32)
        Wre = Tile(P, "s_Wre", [128, 4, 128], BF16)
        Wim = Tile(P, "s_Wim", [128, 4, 128], BF16)
        Zre = Tile(P, "s_Zre", [128, 4, 128], F32)
        Zim = Tile(P, "s_Zim", [128, 4, 128], F32)
        Xre = Tile(P, "s_Xre", [128, 4, 128], BF16)
        Xim = Tile(P, "s_Xim", [128, 4, 128], BF16)
        xe_re = Tile(P, "s_xere", T16, F32)
        xe_im = Tile(P, "s_xeim", T16, F32)
        c_re = Tile(P, "s_cre", T16, F32)
        c_im = Tile(P, "s_cim", T16, F32)
        s1 = Tile(P, "s_s1", T16, F32)
        s2 = Tile(P, "s_s2", T16, F32)
        ysk = Tile(P, "s_ysk", [128, 4, 128], F32)
        ygb = Tile(P, "s_ygb", [128, 4, 128], BF16)
        P.memset(c_re[:], 0.0)
        P.memset(c_im[:], 0.0)
        zbank = [(ps[3], ps[4]), (ps[5], ps[6])]

        def front1(u):
            c, g = divmod(u, 4)
            tsl = slice(c * 128, (c + 1) * 128)
            uT32, uTb = uT32s[c % 2], uTbs[c % 2]
            if g == 0:
                for i in range(4):
                    cols = slice(i * 128, (i + 1) * 128)
                    proj_fm(P, k, ps[0][:, i * 128:(i + 1) * 128], Wu, cols, browu, cols, tsl, 128)
                u_ps = v3(ps[0][:], "p (i t) -> p i t", i=4)
                P.copy(uT32[:], u_ps, e="act")
                P.copy(uTb[:], u_ps, e="act")
            js = slice(4 * g, 4 * g + 4)
            zr, zi = zbank[u % 2]
            P.mm(ps[1][:], uTb[:, g, :], v3(BTre[:, js, :], "p j q -> p (j q)"), start=True, stop=True)
            P.mm(ps[2][:], uTb[:, g, :], v3(BTim[:, js, :], "p j q -> p (j q)"), start=True, stop=True)
            P.copy(bure[:], v3(ps[1][:], "p (j q) -> p j q", j=4), e="act")
            P.copy(buim[:], v3(ps[2][:], "p (j q) -> p j q", j=4), e="act")

        def front2(u):
            c, g = divmod(u, 4)
            js = slice(4 * g, 4 * g + 4)
            zr, zi = zbank[u % 2]
            P.tt(t1[:], Epre[:, js, :], bure[:], ALU.mult)
            P.tt(t2[:], Epim[:, js, :], buim[:], ALU.mult)
            P.tt(Wre[:], t1[:], t2[:], ALU.subtract)
            P.tt(t3[:], Epre[:, js, :], buim[:], ALU.mult)
            P.tt(t4[:], Epim[:, js, :], bure[:], ALU.mult, e="pool")
            P.tt(Wim[:], t3[:], t4[:], ALU.add, e="pool")
            for jj in range(4):
                P.mm(zr[:, jj * 128:(jj + 1) * 128], Wre[:, jj, :], k.mask01b[:], start=True, stop=True)
            for jj in range(4):
                P.mm(zi[:, jj * 128:(jj + 1) * 128], Wim[:, jj, :], k.mask01b[:], start=True, stop=True)


        def front(u):
            front1(u)
            front2(u)

        def back(u):
            c, g = divmod(u, 4)
            tsl = slice(c * 128, (c + 1) * 128)
            uT32 = uT32s[c % 2]
            js = slice(4 * g, 4 * g + 4)
            zr, zi = zbank[u % 2]
            for jj in range(4):
                j = 4 * g + jj
                P.act(Zre[:, jj, :], zr[:, jj * 128:(jj + 1) * 128], AF.Identity, bias=c_re[:, j:j + 1])
            for jj in range(4):
                j = 4 * g + jj
                P.act(Zim[:, jj, :], zi[:, jj * 128:(jj + 1) * 128], AF.Identity, bias=c_im[:, j:j + 1])
            P.tt(t1[:], ETre[:, js, :], Zre[:], ALU.mult)
            P.tt(t2[:], ETim[:, js, :], Zim[:], ALU.mult)
            P.tt(Xre[:], t1[:], t2[:], ALU.subtract)
            P.tt(t3[:], ETim[:, js, :], Zre[:], ALU.mult)
            P.tt(t4[:], ETre[:, js, :], Zim[:], ALU.mult, e="pool")
            P.tt(Xim[:], t3[:], t4[:], ALU.add, e="pool")
            P.tt(xe_re[:, js], t1[:, :, 127], t2[:, :, 127], ALU.subtract)
            P.tt(xe_im[:, js], t3[:, :, 127], t4[:, :, 127], ALU.add, e="pool")
            yo = ps[7][:, g * 128:(g + 1) * 128]
            for jj in range(4):
                P.mm(yo, CTre[:, 4 * g + jj, :], Xre[:, jj, :], start=(jj == 0), stop=False)
                P.mm(yo, CTimn[:, 4 * g + jj, :], Xim[:, jj, :], start=False, stop=(jj == 3))
            if g != 3:
                return
            P.tt(s1[:], lam_re[:], xe_re[:], ALU.mult)
            P.tt(s2[:], lam_im[:], xe_im[:], ALU.mult)
            P.tt(c_re[:], s1[:], s2[:], ALU.subtract)
            P.tt(s1[:], lam_re[:], xe_im[:], ALU.mult)
            P.tt(s2[:], lam_im[:], xe_re[:], ALU.mult)
            P.tt(c_im[:], s1[:], s2[:], ALU.add)
            for kt in range(4):
                P.stt(ysk[:, kt, :], uT32[:, kt, :], dvec[:, kt:kt + 1], ps[7][:, kt * 128:(kt + 1) * 128], ALU.mult, ALU.add)
            P.tt(t3[:], ysk[:], ysk[:], ALU.mult, e="pool")
            P.ts(t3[:], t3[:], 0.044715, 1.0, ALU.mult, ALU.add, e="pool")
            P.tt(t3[:], t3[:], ysk[:], ALU.mult, e="pool")
            P.act(t4[:], t3[:], AF.Sigmoid, scale=2.0 * math.sqrt(2.0 / math.pi))
            P.tt(t3[:], ysk[:], t4[:], ALU.mult)
            P.copy(ygb[:], t3[:], e="act")
            for kp in range(4):
                for kt in range(4):
                    P.mm(ps[0][:, kp * 128:(kp + 1) * 128], Wglu[:, kt, kp * 128:(kp + 1) * 128], ygb[:, kt, :],
                         start=(kt == 0), stop=(kt == 3))
            for kp in range(4):
                P.act(t4[:, kp, :], ps[0][:, kp * 128:(kp + 1) * 128], AF.Sigmoid, bias=bglu[:, kp:kp + 1])
            P.tt(k.yT[1][:, :, tsl], t3[:], t4[:], ALU.mult)

        nu = 4 * NCHUNK
        front(0)
        for u in range(nu):
            if S5_ORDER == 1:
                if u + 1 < nu:
                    front1(u + 1)
                back(u)
                if u + 1 < nu:
                    front2(u + 1)
            else:
                if u + 1 < nu:
                    front(u + 1)
                back(u)


def layer_norm_tile(P, k, xv, g, b, wk):
    st, mv, rs, tmp = wk["st"], wk["mv"], wk["rs"], wk["tmp"]
    for hf in range(2):
        P.op("dve", lambda gg, hf=hf: gg.bn_stats(st[:, hf, :].ap, xv[:, hf * 512:(hf + 1) * 512].ap), reads=[xv], writes=[st[:]])
    P.op("dve", lambda gg: gg.bn_aggr(mv[:].ap, v3(st[:], "p a b -> p (a b)").ap), reads=[st[:]], writes=[mv[:]])
    rstd_from_var(P, rs[:], mv[:, 1:2], LN_EPS, tmp[:])
    P.ts(xv, xv, mv[:, 0:1], rs[:, 0:1], ALU.subtract, ALU.mult)
    P.tt(xv, xv, g[:], ALU.mult, e="pool")
    P.tt(xv, xv, b[:], ALU.add, e="pool")


def ln_work(P, pfx):
    return dict(st=Tile(P, pfx + "st", [128, 2, 6], F32), mv=Tile(P, pfx + "mv", [128, 2], F32),
                rs=Tile(P, pfx + "rs", [128, 1], F32), tmp=Tile(P, pfx + "tmp", [128, 1], F32))


def merge_phase(P, k, L, w, moe):
    ps = k.ps
    go = OFF['gate']
    with Scope(P):
        mixedT = Tile(P, "mixedT", [128, 8, SEQ], BF16)
        with Scope(P):
            wgt = [Tile(P, f"wgt{i}", [128, 8, 3, 128], BF16) for i in range(2)]
            wup = [Tile(P, f"wup{i}", [128, 3, 4, 128], BF16) for i in range(2)]
            bnat = Tile(P, "bg_nat", [24, 128], F32)
            bgate = Tile(P, "bgate", [128, 24], F32)
            sig = [Tile(P, f"mg_sig{i}", [128, 512], F32) for i in range(2)]
            acc = Tile(P, "mg_acc", [128, 512], F32)
            tmp = [Tile(P, "mg_tmp0", [128, 512], F32)] * 2
            P.dma("sp", bnat[:], D(w['b_in'][L, go:go + 3072].rearrange("(r p) -> r p", p=128)), bnat[:])
            P.tr(ps[7][:, 0:24], bnat[:], k.ident[0:24, 0:24])
            P.copy(bgate[:], ps[7][:, 0:24])
            it = 0
            for dti in range(8):
                slot = dti % 2
                for b in range(3):
                    c0 = go + b * 1024 + dti * 128
                    P.dma("pool", wgt[slot][:, :, b, :], D(w['w_in'][L, :, c0:c0 + 128].rearrange("(kt p) n -> p kt n", p=128)), wgt[slot][:])
                    P.dma("pool", wup[slot][:, b, :, :], D(w['w_up'][L, b, :, dti * 128:(dti + 1) * 128].rearrange("(kt p) n -> p kt n", p=128)), wup[slot][:])
                for tb in range(4):
                    tsl = slice(tb * 512, (tb + 1) * 512)
                    for b in range(3):
                        pg, pu = ps[2 * (it % 2)], ps[2 * (it % 2) + 1]
                        for kt in range(8):
                            P.mm(pg[:], wgt[slot][:, kt, b, :], k.xT[:, kt, tsl], start=(kt == 0), stop=(kt == 7))
                        for kt in range(4):
                            P.mm(pu[:], wup[slot][:, b, kt, :], k.yT[b][:, kt, tsl], start=(kt == 0), stop=(kt == 3))
                        sg = sig[it % 2]
                        P.act(sg[:], pg[:], AF.Sigmoid, bias=bgate[:, b * 8 + dti:b * 8 + dti + 1])
                        if b == 0:
                            P.tt(acc[:], sg[:], pu[:], ALU.mult)
                        elif b == 1:
                            P.tt(tmp[0][:], sg[:], pu[:], ALU.mult)
                            P.tt(acc[:], acc[:], tmp[0][:], ALU.add, e="pool")
                        else:
                            P.tt(tmp[1][:], sg[:], pu[:], ALU.mult)
                            P.tt(mixedT[:, dti, tsl], acc[:], tmp[1][:], ALU.add, e="pool")
                        it += 1
        if MERGE_STOP == 1:
            return
        with Scope(P):
            Wo = Tile(P, "Wo", [128, 8, 1024], BF16)
            P.dma("pool", Wo[:], D(w['w_o'][L].rearrange("(kt p) n -> p kt n", p=128)), Wo[:])
            for tt in range(NT):
                xv = k.xres.sub(tt)
                tsl = slice(tt * 128, (tt + 1) * 128)
                for hf in range(2):
                    po = ps[2 + (2 * tt + hf) % 4]
                    for dti in range(8):
                        P.mm(po[:], mixedT[:, dti, tsl], Wo[:, dti, hf * 512:(hf + 1) * 512], start=(dti == 0), stop=(dti == 7))
                    P.stt(xv[:, hf * 512:(hf + 1) * 512], xv[:, hf * 512:(hf + 1) * 512], DN_ALPHA, po[:], ALU.mult, ALU.add)
        with Scope(P):
            g1 = Tile(P, "ln1g", [128, 1024], F32)
            b1 = Tile(P, "ln1b", [128, 1024], F32)
            wks = [ln_work(P, "ln1a_"), ln_work(P, "ln1b_")]
            P.dma("sp", g1[:], D(w['ln1_g'][L].partition_broadcast(128)), g1[:])
            P.dma("sp", b1[:], D(w['ln1_b'][L].partition_broadcast(128)), b1[:])
            if moe:
                Wr = Tile(P, "Wr", [128, 8, 8], F32)
                br = Tile(P, "br", [1, 8], F32)
                xTf = Tile(P, "xTf", [128, 8, 128], F32)
                rt = Tile(P, "rt", [128, 6, 8], F32)
                P.dma("sp", Wr[:], D(w['moe_router'][0].rearrange("(kt p) n -> p kt n", p=128)), Wr[:])
                P.dma("sp", br[:], D(w['moe_router_b'][0:1, :]), br[:])
            for tt in range(NT):
                xv = k.xres.sub(tt)
                layer_norm_tile(P, k, xv, g1, b1, wks[tt % 2])
                make_xT(P, k, tt, xtf=(xTf if moe else None), banks=((0, 1) if tt % 2 == 0 else (2, 3)))
                if moe:
                    lg = ps[4][:, 0:8]
                    P.mm(lg, k.ones[0:1, 0:128], br[:], start=True, stop=False)
                    for kt in range(8):
                        P.mm(lg, xTf[:, kt, :], Wr[:, kt, :], start=False, stop=(kt == 7))
                    P.copy(rt[:, 0, :], lg)
                    P.op("dve", lambda g_: g_.max(rt[:, 1, :].ap, rt[:, 0, :].ap), reads=[rt[:]], writes=[rt[:]])
                    P.ts(rt[:, 2, :], rt[:, 0, :], rt[:, 1, 1:2], None, ALU.is_ge)
                    P.ts(rt[:, 5, 0:1], rt[:, 1, 0:1], -1.0, None, ALU.mult)
                    P.act(rt[:, 3, :], rt[:, 0, :], AF.Exp, bias=rt[:, 5, 0:1])
                    P.tt(rt[:, 4, :], rt[:, 3, :], rt[:, 2, :], ALU.mult)
                    P.reduce(rt[:, 5, 1:2], rt[:, 4, :], ALU.add)
                    P.recip(rt[:, 5, 2:3], rt[:, 5, 1:2])
                    P.ts(k.comb[:, tt, :], rt[:, 4, :], rt[:, 5, 2:3], None, ALU.mult)


def ffn_phase(P, k, L, w, moe, out_dram, last):
    ps = k.ps
    with Scope(P):
        with Scope(P):
            Wpg = Tile(P, "Wpg", [128, 8, 1024], BF16)
            Wpp = Tile(P, "Wpp", [128, 2, 1024], BF16)
            pt = [Tile(P, f"pt{i}", [128, 256], F32) for i in range(2)]
            pTb = Tile(P, "pTb", [128, 2, 128], BF16)
            sg = Tile(P, "ple_sg", [128, 1024], F32)
            P.dma("pool", Wpg[:], D(w['ple_w_gate'][L].rearrange("(kt p) n -> p kt n", p=128)), Wpg[:])
            P.dma("pool", Wpp[:], D(w['ple_w_proj'][L].rearrange("(kt p) n -> p kt n", p=128)), Wpp[:])
            for tt in range(NT):
                xv = k.xres.sub(tt)
                tsl = slice(tt * 128, (tt + 1) * 128)
                ptt = pt[tt % 2]
                P.dma("sp", ptt[:], D(w['p'][L, tt * 128:(tt + 1) * 128, :]), ptt[:])
                for q in range(2):
                    P.tr(ps[6][:, q * 128:(q + 1) * 128], ptt[:, q * 128:(q + 1) * 128], k.ident[:])
                P.copy(pTb[:], v3(ps[6][:, 0:256], "p (q t) -> p q t", q=2), e="act")
                for hf in range(2):
                    hs = slice(hf * 512, (hf + 1) * 512)
                    pg, pp = ps[2 + hf], ps[4 + hf]
                    for kt in range(8):
                        P.mm(pg[:], k.xT[:, kt, tsl], Wpg[:, kt, hs], start=(kt == 0), stop=(kt == 7))
                    for kt in range(2):
                        P.mm(pp[:], pTb[:, kt, :], Wpp[:, kt, hs], start=(kt == 0), stop=(kt == 1))
                    P.act(sg[:, hs], pg[:], AF.Sigmoid)
                    P.tt(sg[:, hs], sg[:, hs], pp[:], ALU.mult)
                    P.stt(xv[:, hs], xv[:, hs], DN_ALPHA, sg[:, hs], ALU.mult, ALU.add)
        if FFN_STOP == 1:
            return
        with Scope(P):
            wg = [Tile(P, f"f_wg{i}", [128, 8, 512], BF16) for i in range(2)]
            wu = [Tile(P, f"f_wu{i}", [128, 8, 512], BF16) for i in range(2)]
            wd = [Tile(P, f"f_wd{i}", [128, 4, 1024], BF16) for i in range(2)]
            actT = [Tile(P, f"f_act{i}", [128, 4, 512], BF16) for i in range(2)]
            sgl = [Tile(P, f"f_sg{i}", [128, 512], F32) for i in range(2)]
            if moe:
                experts = [(w['moe_wg'][0, e], w['moe_wu'][0, e], w['moe_wd'][0, e], 3584, e) for e in range(N_EXP)]
            else:
                experts = [(w['ffn_wg'][0], w['ffn_wu'][0], w['ffn_wd'][0], 2816, None)]
            cnt = 0
            it = 0
            ia = 0
            io = 0
            for (Wg, Wu_, Wd, dff, e) in experts:
                nft = dff // 128
                f0t = 0
                while f0t < nft:
                    nf = min(4, nft - f0t)
                    slot = cnt % 2
                    cs = slice(f0t * 128, (f0t + nf) * 128)
                    P.dma("pool", wg[slot][:, :, 0:nf * 128], D(Wg[:, cs].rearrange("(kt p) n -> p kt n", p=128)), wg[slot][:])
                    P.dma("pool", wu[slot][:, :, 0:nf * 128], D(Wu_[:, cs].rearrange("(kt p) n -> p kt n", p=128)), wu[slot][:])
                    P.dma("pool", wd[slot][:, 0:nf, :], D(Wd[cs, :].rearrange("(ft p) n -> p ft n", p=128)), wd[slot][:])
                    for tb in range(4):
                        tsl = slice(tb * 512, (tb + 1) * 512)
                        at = actT[ia % 2]
                        ia += 1
                        for ft in range(nf):
                            pg, pu = ps[it % 2], ps[2 + it % 2]
                            fs = slice(ft * 128, (ft + 1) * 128)
                            for kt in range(8):
                                P.mm(pg[:], wg[slot][:, kt, fs], k.xT[:, kt, tsl], start=(kt == 0), stop=(kt == 7))
                            for kt in range(8):
                                P.mm(pu[:], wu[slot][:, kt, fs], k.xT[:, kt, tsl], start=(kt == 0), stop=(kt == 7))
                            P.act(sgl[it % 2][:], pg[:], AF.Silu)
                            P.tt(at[:, ft, :], sgl[it % 2][:], pu[:], ALU.mult)
                            it += 1
                        for t4 in range(4):
                            tt = tb * 4 + t4
                            xv = k.xres.sub(tt)
                            for hf in range(2):
                                hs = slice(hf * 512, (hf + 1) * 512)
                                po = ps[4 + io % 4]
                                io += 1
                                for ft in range(nf):
                                    P.mm(po[:], at[:, ft, t4 * 128:(t4 + 1) * 128], wd[slot][:, ft, hs], start=(ft == 0), stop=(ft == nf - 1))
                                if e is None:
                                    P.tt(xv[:, hs], xv[:, hs], po[:], ALU.add)
                                else:
                                    P.stt(xv[:, hs], po[:], k.comb[:, tt, e:e + 1], xv[:, hs], ALU.mult, ALU.add)
                    f0t += nf
                    cnt += 1
        if FFN_STOP == 2:
            return
        with Scope(P):
            g2 = Tile(P, "ln2g", [128, 1024], F32)
            b2 = Tile(P, "ln2b", [128, 1024], F32)
            wks = [ln_work(P, "ln2a_"), ln_work(P, "ln2b_")]
            P.dma("sp", g2[:], D(w['ln2_g'][L].partition_broadcast(128)), g2[:])
            P.dma("sp", b2[:], D(w['ln2_b'][L].partition_broadcast(128)), b2[:])
            for tt in range(NT):
                xv = k.xres.sub(tt)
                layer_norm_tile(P, k, xv, g2, b2, wks[tt % 2])
                if last:
                    P.dma("sp", D(out_dram[tt * 128:(tt + 1) * 128, :]), xv, xv)
                else:
                    make_xT(P, k, tt, banks=((0, 1) if tt % 2 == 0 else (2, 3)))


N_EXP = int(os.environ.get('N_EXP', '8'))
MERGE_STOP = int(os.environ.get('MERGE_STOP', '0'))
FFN_STOP = int(os.environ.get('FFN_STOP', '0'))
N_LAYERS = int(os.environ.get('N_LAYERS', '2'))
DUMP = os.environ.get('DUMP', '')


def dump_xres(P, k, out_dram):
    for tt in range(NT):
        xv = k.xres.sub(tt)
        P.dma("sp", D(out_dram[tt * 128:(tt + 1) * 128, :]), xv, xv)


def build(n_layers=None, debug=None):
    n_layers = N_LAYERS if n_layers is None else n_layers
    nc = bass.Bass("TRN2", target_bir_lowering=False)
    w = {}
    for name, shp in INPUT_SHAPES.items():
        w[name] = nc.dram_tensor(name, list(shp), F32, kind="ExternalInput").ap()
    out = nc.dram_tensor("out", [SEQ, DM], F32, kind="ExternalOutput").ap()
    dbg = None
    if debug == "yT":
        dbg = nc.dram_tensor("dbg", [128, 3 * 4 * SEQ], BF16, kind="ExternalOutput").ap()
    with ExitStack() as es:
        P = Prog(nc, es)
        P.mute = MUTE
        k = K()
        setup_consts(P, k)
        k.xres = Tile(P, "xres", [128, NT, DM], F32, nreg=NT)
        k.xT = Tile(P, "xT", [128, 8, SEQ], BF16, nreg=NT)
        k.comb = Tile(P, "comb", [128, NT, 8], F32)
        load_x(P, k, w['x'])
        stop = False
        for L in range(n_layers):
            moe = (L % 2 == 1)
            with Scope(P):
                k.yT = {}
                k.yT[1] = Tile(P, "yT_s5", [128, 4, SEQ], BF16)
                if debug == 'yT':
                    P.memset(k.yT[1][:], 0.0)
                if 's5' in PHASES:
                    s5_phase(P, k, L, w)
                    MUTE[0] = False
                k.yT[2] = Tile(P, "yT_ml", [128, 4, SEQ], BF16)
                if debug == 'yT':
                    P.memset(k.yT[2][:], 0.0)
                if 'ml' in PHASES:
                    mlstm_phase(P, k, L, w)
                k.yT[0] = Tile(P, "yT_gla", [128, 4, SEQ], BF16)
                if debug == 'yT':
                    P.memset(k.yT[0][:], 0.0)
                if 'gla' in PHASES:
                    gla_phase(P, k, L, w)
                if debug == "yT":
                    for b in range(3):
                        P.dma("sp", D(dbg[:, b * 4 * SEQ:(b + 1) * 4 * SEQ]),
                              v3(k.yT[b][:], "p a t -> p (a t)"), k.yT[b][:])
                    stop = True
                else:
                    merge_phase(P, k, L, w, moe)
            if stop:
                break
            if DUMP == f"{L}:ln1":
                dump_xres(P, k, out)
                break
            ffn_phase(P, k, L, w, moe, out, last=(L == n_layers - 1))
            if DUMP == f"{L}:ln2" and L != n_layers - 1:
                dump_xres(P, k, out)
                break
        P.finish()
        print("ops", P.n_ops, "waits", P.n_waits, {e: P.cnt[e] for e in ENG}, "dsems", P.n_dsem)
    return nc


NCHUNK = 16
PHASES = ['gla', 's5', 'ml']
S5_STOP = 99
GLA_STOP = 0
GLA_VAR = 0
S5_ORDER = 0
N_EXP = 8
MERGE_STOP = 0
FFN_STOP = 0
N_LAYERS = 2
DUMP = ''

def kernel(**inputs):
    nc = build()
    shared = {k_: np.ascontiguousarray(np.asarray(v, dtype=np.float32)) for k_, v in inputs.items() if k_ not in ("x", "p")}
    xs = np.asarray(inputs["x"], dtype=np.float32)
    ps_ = np.asarray(inputs["p"], dtype=np.float32)
    n = xs.shape[0]
    maps = []
    for b in range(n):
        m = dict(shared)
        m["x"] = np.ascontiguousarray(xs[b])
        m["p"] = np.ascontiguousarray(ps_[:, b])
        maps.append(m)
    res = run_bass_kernel_spmd(nc, maps, core_ids=list(range(n)))
    return np.stack([np.asarray(res.results[b]["out"]) for b in range(n)]).astype(np.float32)
```

```python
import numpy as np
from contextlib import ExitStack
import concourse.bass as bass
import concourse.mybir as mybir
from concourse.bass_utils import run_bass_kernel_spmd

dt = mybir.dt
F32, BF16, I32 = dt.float32, dt.bfloat16, dt.int32
AF = mybir.ActivationFunctionType
ALU = mybir.AluOpType
AX = mybir.AxisListType

SPARSE_PE_INC = False
SAME_ENGINE_SYNC = True


class Region:
    __slots__ = ("name", "writer", "readers", "dma_sem", "dma_n", "dma_base", "uid", "excl", "dma_q")
    _next = [0]

    def __init__(self, name):
        self.name = name
        self.writer = None
        self.readers = {}
        self.dma_sem = None
        self.dma_n = 0
        self.dma_base = 0
        self.excl = False
        Region._next[0] += 1
        self.uid = Region._next[0]


class V:
    __slots__ = ("ap", "regs")

    def __init__(self, ap, regs):
        self.ap = ap
        self.regs = regs

    def __getitem__(self, idx):
        return V(self.ap[idx], self.regs)

    def bitcast(self, d):
        return V(self.ap.bitcast(d), self.regs)

    def bcast(self, shape):
        return V(self.ap.broadcast_to(shape), self.regs)


class Tile:
    _n = [0]

    def __init__(self, prog, name, shape, dtype, space="sbuf", nreg=1):
        Tile._n[0] += 1
        name = "%s_%d" % (name, Tile._n[0])
        self.name = name
        self.shape = shape
        if space == "sbuf":
            self.t = prog.es.enter_context(prog.nc.sbuf_tensor(name, shape, dtype))
        elif space == "psum":
            self.t = prog.es.enter_context(prog.nc.psum_tensor(name, shape, dtype))
        else:
            self.t = space
        self.regs = [Region(f"{name}.{i}") for i in range(nreg)]
        if space == "psum":
            for r in self.regs:
                r.excl = True
        prog.scope_regions[-1].extend(self.regs)

    def __getitem__(self, idx):
        return V(self.t[idx], self.regs)

    def sub(self, j):
        return V(self.t[:, j], [self.regs[j]])

    def reg(self, j, idx):
        return V(self.t[idx], [self.regs[j]])


ENG = ("pe", "act", "dve", "pool", "sp")


class Prog:
    def __init__(self, nc, es):
        self.nc = nc
        self.es = es
        self.eng = {"pe": nc.tensor, "act": nc.scalar, "dve": nc.vector, "pool": nc.gpsimd, "sp": nc.sync}
        self.sem = {e: es.enter_context(nc.semaphore("sem_" + e)) for e in ENG}
        self.cnt = {e: 0 for e in ENG}
        self.waited = {e: {f: 0 for f in ENG} for e in ENG}
        self.dma_waited = {e: {} for e in ENG}
        self.es0 = es
        self.scope_regions = [[]]
        self.sem_pool = {'pool': [], 'sp': [], 'act': []}
        self.n_dsem = 0
        self.dma_regions = []
        self.n_ops = 0
        self.n_waits = 0
        self.mute = None

    def _collect(self, e, reads, writes):
        deps = {}
        dmadeps = {}

        def add(w):
            if w is None:
                return
            if w[0] == "dma":
                r, n = w[1], w[2]
                k = r.uid
                if k not in dmadeps or dmadeps[k][1] < n:
                    dmadeps[k] = (r, n)
            else:
                f, i = w
                if f == e and (e == "pe" or not SAME_ENGINE_SYNC):
                    return
                if deps.get(f, 0) < i:
                    deps[f] = i

        for v in reads:
            for r in v.regs:
                add(r.writer)
                if r.excl:
                    for k, val in r.readers.items():
                        if not isinstance(k, tuple) and k != e:
                            add((k, val))
        for v in writes:
            for r in v.regs:
                add(r.writer)
                for k, val in r.readers.items():
                    if isinstance(k, tuple):
                        add(("dma", val[0], val[1]))
                    else:
                        add((k, val))
        return deps, dmadeps

    def _emit_waits(self, e, deps, dmadeps):
        eng = self.eng[e]
        for f, i in deps.items():
            if self.waited[e][f] >= i:
                continue
            if f == e and i < self.cnt[e]:
                pass
            eng.wait_ge(self.sem[f], i)
            self.n_waits += 1
            self.waited[e][f] = i
        for k, (r, n) in dmadeps.items():
            if self.dma_waited[e].get(k, 0) >= n:
                continue
            eng.wait_ge(r.dma_sem, r.dma_base + 16 * n)
            self.n_waits += 1
            self.dma_waited[e][k] = n

    def op(self, e, fn, reads=(), writes=(), inc=True):
        if self.mute is not None and self.mute[0]:
            return None
        deps, dmadeps = self._collect(e, reads, writes)
        self._emit_waits(e, deps, dmadeps)
        inst = fn(self.eng[e])
        if inc:
            self.cnt[e] += 1
            n = self.cnt[e]
            inst.then_inc(self.sem[e], 1)
        else:
            n = self.cnt[e] + 1
        for v in reads:
            for r in v.regs:
                if r.readers.get(e, 0) < n:
                    r.readers[e] = n
        for v in writes:
            for r in v.regs:
                r.writer = (e, n)
                r.readers = {}
        self.n_ops += 1
        return inst

    def dma(self, q, out, in_, sb, **kw):
        if self.mute is not None and self.mute[0]:
            return None
        deps, dmadeps = self._collect(q, [in_], [out])
        self._emit_waits(q, deps, dmadeps)
        r0 = sb.regs[0]
        if r0.dma_sem is None:
            r0.dma_q = q
            if self.sem_pool[q]:
                r0.dma_sem, r0.dma_base = self.sem_pool[q].pop()
            else:
                r0.dma_sem = self.es0.enter_context(self.nc.semaphore("dsem%d" % self.n_dsem))
                self.n_dsem += 1
                r0.dma_base = 0
            self.dma_regions.append(r0)
        assert r0.dma_q == q, (r0.name, r0.dma_q, q)
        inst = self.eng[q].dma_start(out=out.ap, in_=in_.ap, **kw)
        inst.then_inc(r0.dma_sem, 16)
        r0.dma_n += 1
        n = r0.dma_n
        for r in in_.regs:
            r.readers[("dma", r0.uid)] = (r0, n)
        for r in out.regs:
            r.writer = ("dma", r0, n)
            r.readers = {}
        return inst

    def push_scope(self):
        self.scope_regions.append([])

    def pop_scope(self):
        for r in self.scope_regions.pop():
            if r.dma_sem is not None:
                self.sem_pool[r.dma_q].append((r.dma_sem, r.dma_base + 16 * r.dma_n))
                self.dma_regions.remove(r)
                r.dma_sem = None

    def barrier(self):
        for e in ENG:
            deps = {f: self.cnt[f] for f in ENG if (f != e or e != 'pe') and self.cnt[f] > 0}
            dmadeps = {r.uid: (r, r.dma_n) for r in self.dma_regions}
            self._emit_waits(e, deps, dmadeps)

    def mm(self, out, lhsT, rhs, start=True, stop=True, **kw):
        return self.op("pe", lambda g: g.matmul(out.ap, lhsT.ap, rhs.ap, start=start, stop=stop, **kw),
                       reads=[lhsT, rhs] + ([] if start else [out]), writes=[out], inc=(stop or not SPARSE_PE_INC))

    def tr(self, out, in_, ident):
        return self.op("pe", lambda g: g.transpose(out.ap, in_.ap, ident.ap), reads=[in_, ident], writes=[out])

    def act(self, out, in_, func, bias=None, scale=None, accum=None, e="act"):
        kw = {}
        reads = [in_]
        if bias is not None:
            if isinstance(bias, V):
                kw["bias"] = bias.ap
                reads.append(bias)
            else:
                kw["bias"] = bias
        if scale is not None:
            if isinstance(scale, V):
                kw["scale"] = scale.ap
                reads.append(scale)
            else:
                kw["scale"] = scale
        writes = [out]
        if accum is not None:
            kw["accum_out"] = accum.ap
            writes.append(accum)
        return self.op(e, lambda g: g.activation(out.ap, in_.ap, func, **kw), reads=reads, writes=writes)

    def tt(self, out, a, b, op, e="dve"):
        return self.op(e, lambda g: g.tensor_tensor(out.ap, a.ap, b.ap, op), reads=[a, b], writes=[out])

    def ts(self, out, a, s1, s2, op0, op1=None, e="dve", accum=None):
        reads = [a]
        s1a, s2a = s1, s2
        if isinstance(s1, V):
            reads.append(s1)
            s1a = s1.ap
        if isinstance(s2, V):
            reads.append(s2)
            s2a = s2.ap
        kw = {}
        writes = [out]
        if accum is not None:
            kw["accum_out"] = accum.ap
            writes.append(accum)
        if op1 is None:
            return self.op(e, lambda g: g.tensor_scalar(out.ap, a.ap, s1a, s2a, op0, **kw), reads=reads, writes=writes)
        return self.op(e, lambda g: g.tensor_scalar(out.ap, a.ap, s1a, s2a, op0, op1, **kw), reads=reads, writes=writes)

    def stt(self, out, a, s, b, op0, op1, e="dve", accum=None):
        reads = [a, b]
        sa = s
        if isinstance(s, V):
            reads.append(s)
            sa = s.ap
        kw = {}
        writes = [out]
        if accum is not None:
            kw["accum_out"] = accum.ap
            writes.append(accum)
        return self.op(e, lambda g: g.scalar_tensor_tensor(out.ap, a.ap, sa, b.ap, op0, op1, **kw), reads=reads, writes=writes)

    def copy(self, out, in_, e="dve"):
        if e == "act":
            return self.op(e, lambda g: g.copy(out.ap, in_.ap), reads=[in_], writes=[out])
        return self.op(e, lambda g: g.tensor_copy(out.ap, in_.ap), reads=[in_], writes=[out])

    def memset(self, out, val, e="pool"):
        return self.op(e, lambda g: g.memset(out.ap, val), reads=[], writes=[out])

    def reduce(self, out, in_, op, axis=AX.X, e="dve", **kw):
        return self.op(e, lambda g: g.tensor_reduce(out.ap, in_.ap, axis, op, **kw), reads=[in_], writes=[out])

    def recip(self, out, in_, e="dve"):
        return self.op(e, lambda g: g.reciprocal(out.ap, in_.ap), reads=[in_], writes=[out])

    def finish(self):
        self.barrier()


SEQ = 2048
DM = 1024
NT = 16
DEPTH = 2
import os
NCHUNK = int(os.environ.get('NCHUNK', '16'))
S5_STOP = int(os.environ.get('S5_STOP', '99'))
GLA_STOP = int(os.environ.get('GLA_STOP', '0'))
S5_ORDER = int(os.environ.get('S5_ORDER', '0'))
GLA_VAR = int(os.environ.get('GLA_VAR', '0'))
PHASES = os.environ.get('PHASES', 'gla,s5,ml').split(',')
OFF = dict(gq=0, gk=256, gv=512, ga=1024, gg=1040, su=1552, mq=2064, mk=2320, mv=2576, mi=3088, mf=3092, mo=3096, gate=3608, end=6680)
DN_ALPHA = (2.0 * DEPTH) ** 0.25
LN_EPS = 1e-5

INPUT_SHAPES = {
    'x': (SEQ, DM), 'p': (2, SEQ, 256), 'w_in': (2, 1024, 6680), 'b_in': (2, 6680), 'gla_w_a2': (2, 16, 256),
    'gla_b_a2': (2, 256), 'gla_norm_g': (2, 512), 's5_a_re': (2, 32, 64), 's5_a_im': (2, 32, 64), 's5_log_dt': (2, 32),
    's5_b_re': (2, 32, 64, 16), 's5_b_im': (2, 32, 64, 16), 's5_c_re': (2, 32, 16, 64), 's5_c_im': (2, 32, 16, 64),
    's5_d': (2, 512), 's5_w_glu': (2, 512, 512), 's5_b_glu': (2, 512), 'ml_conv_w': (2, 4, 512), 'ml_conv_b': (2, 512),
    'ml_norm_g': (2, 512), 'w_up': (2, 3, 512, 1024), 'w_o': (2, 1024, 1024), 'ln1_g': (2, 1024), 'ln1_b': (2, 1024),
    'ffn_wg': (1, 1024, 2816), 'ffn_wu': (1, 1024, 2816), 'ffn_wd': (1, 2816, 1024), 'moe_router': (1, 1024, 8),
    'moe_router_b': (1, 8), 'moe_wg': (1, 8, 1024, 3584), 'moe_wu': (1, 8, 1024, 3584), 'moe_wd': (1, 8, 3584, 1024),
    'ple_w_gate': (2, 1024, 1024), 'ple_w_proj': (2, 256, 1024), 'ln2_g': (2, 1024), 'ln2_b': (2, 1024),
}


class Scope:
    def __init__(self, P):
        self.P = P

    def __enter__(self):
        self.es = ExitStack()
        self.es.__enter__()
        self.saved = self.P.es
        self.P.es = self.es
        self.P.push_scope()
        return self

    def __exit__(self, *a):
        self.P.barrier()
        self.P.pop_scope()
        self.P.es = self.saved
        return self.es.__exit__(*a)


def D(ap):
    return V(ap, [])


class K:
    pass


def setup_consts(P, k):
    k.ones = Tile(P, "ones", [128, 128], F32)
    k.ident = Tile(P, "ident", [128, 128], F32)
    k.identb = Tile(P, "identb", [128, 128], BF16)
    k.onesb = Tile(P, "onesb", [1, 512], BF16)
    k.mask01 = Tile(P, "mask01", [128, 128], F32)
    k.mask01b = Tile(P, "mask01b", [128, 128], BF16)
    k.triu_s = Tile(P, "triu_s", [128, 128], F32)
    k.tril_s = Tile(P, "tril_s", [128, 128], F32)
    P.memset(k.ones[:], 1.0)
    P.op("pool", lambda g: g.affine_select(k.ident[:].ap, k.ones[:, 0:128].ap, [[-1, 128]], ALU.is_equal, 0.0,
                                           base=0, channel_multiplier=1), reads=[k.ones[:]], writes=[k.ident[:]])
    P.copy(k.identb[:], k.ident[:], e="pool")
    P.memset(k.onesb[:], 1.0)
    P.op("pool", lambda g: g.affine_select(k.mask01[:].ap, k.ones[:, 0:128].ap, [[1, 128]], ALU.is_ge, 0.0,
                                           base=0, channel_multiplier=-1), reads=[k.ones[:]], writes=[k.mask01[:]])
    P.copy(k.mask01b[:], k.mask01[:], e="pool")
    P.ts(k.triu_s[:], k.mask01[:], -1.0 / 16, None, ALU.mult, e="pool")
    P.ts(k.tril_s[:], k.mask01[:], 1.0 / 16, -1.0 / 16, ALU.mult, ALU.add, e="pool")
    k.ps = [Tile(P, f"ps{i}", [128, 512], F32, space="psum") for i in range(8)]


def load_x(P, k, x_dram):
    for j in range(NT):
        P.dma("sp", k.xres.sub(j), D(x_dram[j * 128:(j + 1) * 128, :]), k.xres.sub(j))
        make_xT(P, k, j)


def make_xT(P, k, j, xtf=None, banks=(0, 1)):
    for half in range(2):
        pst = k.ps[banks[half]]
        for q in range(4):
            kt = half * 4 + q
            P.tr(pst[:, q * 128:(q + 1) * 128], k.xres.sub(j)[:, kt * 128:(kt + 1) * 128], k.ident[:])
        dst = k.xT.reg(j, (slice(None), slice(half * 4, half * 4 + 4), slice(j * 128, (j + 1) * 128)))
        src = pst[:].ap.rearrange("p (q t) -> p q t", q=4)
        srcv = V(src, pst.regs)
        P.copy(dst, srcv, e="act" if half == 0 else "dve")
        if xtf is not None:
            P.copy(xtf[:, half * 4:half * 4 + 4, :], srcv, e="dve" if half == 0 else "act")


def proj_fm(P, k, ps_out, W, wc, brow, bc, tsl, n):
    P.mm(ps_out, brow[0:1, bc], k.ones[0:1, 0:n], start=True, stop=False)
    for kt in range(8):
        P.mm(ps_out, W[:, kt, wc], k.xT[:, kt, tsl], start=False, stop=(kt == 7))


def proj_tm(P, k, ps_out, W, wc, brow, bc, tsl):
    P.mm(ps_out, k.ones[0:1, 0:128], brow[0:1, bc], start=True, stop=False)
    for kt in range(8):
        P.mm(ps_out, k.xT[:, kt, tsl], W[:, kt, wc], start=False, stop=(kt == 7))


def rstd_from_var(P, out, var, eps, tmp):
    P.ts(tmp, var, eps, None, ALU.add)
    P.act(tmp, tmp, AF.Ln)
    P.act(out, tmp, AF.Exp, scale=-0.5)


def gla_phase(P, k, L, w):
    ps = k.ps
    with Scope(P):
        WG = Tile(P, "WG", [128, 8, 1552], BF16)
        brow = Tile(P, "g_brow", [1, 1552], BF16)
        wa2 = Tile(P, "wa2", [16, 256], F32)
        ba2 = Tile(P, "ba2", [1, 256], F32)
        ng = Tile(P, "g_ng", [128, 512], F32)
        P.dma("pool", WG[:], D(w['w_in'][L, :, 0:1552].rearrange("(kt p) n -> p kt n", p=128)), WG[:])
        P.dma("pool", brow[:], D(w['b_in'][L:L + 1, 0:1552]), brow[:])
        P.dma("sp", wa2[:], D(w['gla_w_a2'][L]), wa2[:])
        P.dma("sp", ba2[:], D(w['gla_b_a2'][L:L + 1, :]), ba2[:])
        P.dma("sp", ng[:], D(w['gla_norm_g'][L].partition_broadcast(128)), ng[:])
        qk_raw = Tile(P, "g_qkraw", [128, 4, 512], BF16)
        alr_all = Tile(P, "g_alr", [16, 512], F32)
        sp = Tile(P, "g_sp", [128, 256], F32)
        eT = Tile(P, "g_eT", [128, 2, 128], F32)
        einvT = Tile(P, "g_einvT", [128, 2, 128], F32)
        erev = Tile(P, "g_erev", [128, 256], F32)
        dec = [Tile(P, f"g_dec{i}", [128, 2], F32) for i in range(2)]
        qdT = [Tile(P, f"g_qdT{i}", [128, 2, 128], BF16) for i in range(2)]
        kiT = [Tile(P, f"g_kiT{i}", [128, 2, 128], BF16) for i in range(2)]
        ktail = [Tile(P, f"g_ktail{i}", [128, 256], BF16) for i in range(2)]
        vbf = [Tile(P, f"g_vbf{i}", [128, 512], BF16) for i in range(2)]
        sg = [Tile(P, f"g_sg{i}", [128, 512], BF16) for i in range(2)]
        sgf = Tile(P, "g_sgf", [128, 512], F32)
        attT = Tile(P, "g_attT", [128, 4, 128], BF16)
        S32 = Tile(P, "g_S32", [128, 2, 128], F32)
        Sbf = Tile(P, "g_Sbf", [128, 2, 128], BF16)
        st = Tile(P, "g_st", [128, 4, 6], F32)
        mv = Tile(P, "g_mv", [128, 4, 2], F32)
        rstd = Tile(P, "g_rstd", [128, 4], F32)
        tmp4 = Tile(P, "g_tmp4", [128, 4], F32)
        yn = Tile(P, "g_yn", [128, 512], F32)
        ybf = Tile(P, "g_ybf", [128, 512], BF16)
        P.memset(S32[:], 0.0)
        P.memset(Sbf[:], 0.0)

        def bias_fm(ps_out, bc, n):
            P.mm(ps_out, brow[0:1, bc], k.onesb[0:1, 0:n], start=True, stop=False)

        def bias_tm(ps_out, bc):
            P.mm(ps_out, k.onesb[0:1, 0:128], brow[0:1, bc], start=True, stop=False)

        def stage_a(c):
            par = c % 2
            tsl = slice(c * 128, (c + 1) * 128)
            off = (c % 4) * 128
            osl = slice(off, off + 128)
            if c % 4 == 0:
                t4 = slice(c * 128, c * 128 + 512)
                for i in range(4):
                    cols = slice(i * 128, (i + 1) * 128)
                    bias_fm(ps[0][:], cols, 512)
                    for kt in range(8):
                        P.mm(ps[0][:], WG[:, kt, cols], k.xT[:, kt, t4], start=False, stop=(kt == 7))
                    P.copy(qk_raw[:, i, :], ps[0][:], e="act")
                ac = slice(OFF['ga'], OFF['ga'] + 16)
                bias_fm(ps[0][0:16, :], ac, 512)
                for kt in range(8):
                    P.mm(ps[0][0:16, :], WG[:, kt, ac], k.xT[:, kt, t4], start=False, stop=(kt == 7))
                P.copy(alr_all[:], ps[0][0:16, :], e="act")
            P.mm(ps[1][:, 0:256], k.ones[0:1, 0:128], ba2[:], start=True, stop=False)
            P.mm(ps[1][:, 0:256], alr_all[:, osl], wa2[:], start=False, stop=True)
            P.act(sp[:], ps[1][:, 0:256], AF.Exp, scale=-1.0)
            P.act(sp[:], sp[:], AF.Ln, bias=1.0)
            P.mm(ps[1][:, 256:512], k.tril_s[:], sp[:], start=True, stop=True)
            for i in range(2):
                P.mm(ps[2][:, i * 128:(i + 1) * 128], sp[:, i * 128:(i + 1) * 128], k.triu_s[:], start=True, stop=True)
            cumT = v3(ps[2][:, 0:256], "p (i t) -> p i t", i=2)
            P.act(eT[:], cumT, AF.Exp)
            P.act(einvT[:], cumT, AF.Exp, scale=-1.0)
            P.act(erev[:], ps[1][:, 256:512], AF.Exp)
            P.copy(dec[par][:], eT[:, :, 127], e="act")
            P.stt(qdT[par][:], qk_raw[:, 0:2, osl], 0.125, eT[:], ALU.mult, ALU.mult)
            P.tt(kiT[par][:], qk_raw[:, 2:4, osl], einvT[:], ALU.mult, e="pool")
            kc = slice(OFF['gk'], OFF['gk'] + 256)
            bias_tm(ps[2][:, 256:512], kc)
            for kt in range(8):
                P.mm(ps[2][:, 256:512], k.xT[:, kt, tsl], WG[:, kt, kc], start=False, stop=(kt == 7))
            vc = slice(OFF['gv'], OFF['gv'] + 512)
            bias_tm(ps[3][:], vc)
            for kt in range(8):
                P.mm(ps[3][:], k.xT[:, kt, tsl], WG[:, kt, vc], start=False, stop=(kt == 7))
            gc = slice(OFF['gg'], OFF['gg'] + 512)
            bias_tm(ps[4][:], gc)
            for kt in range(8):
                P.mm(ps[4][:], k.xT[:, kt, tsl], WG[:, kt, gc], start=False, stop=(kt == 7))
            P.tt(ktail[par][:], ps[2][:, 256:512], erev[:], ALU.mult)
            P.copy(vbf[par][:], ps[3][:], e="act")
            P.act(sgf[:], ps[4][:], AF.Silu)
            P.tt(sg[par][:], sgf[:], ng[:], ALU.mult, e="pool")

        def stage_b(c):
            par = c % 2
            tsl = slice(c * 128, (c + 1) * 128)
            abank = {0: (ps[5], 0), 2: (ps[5], 128), 1: (ps[6], 0), 3: (ps[6], 128)}
            for h in (0, 2, 1, 3):
                hp, ho = h // 2, (h % 2) * 64
                bk, co = abank[h]
                P.mm(bk[:, co:co + 128], kiT[par][ho:ho + 64, hp, :], qdT[par][ho:ho + 64, hp, :], start=True, stop=True)
            for h in range(4):
                bk, co = abank[h]
                P.tt(attT[:, h, :], bk[:, co:co + 128], k.mask01[:], ALU.mult)
            for h in range(4):
                hp, ho = h // 2, (h % 2) * 64
                P.mm(ps[7][:, h * 128:(h + 1) * 128], attT[:, h, :], vbf[par][:, h * 128:(h + 1) * 128], start=True, stop=False)
                P.mm(ps[7][:, h * 128:(h + 1) * 128], qdT[par][ho:ho + 64, hp, :], Sbf[ho:ho + 64, hp, :], start=False, stop=True)
            sbank = [ps[5], ps[6]]
            for hp in range(2):
                P.mm(sbank[hp][:, 256:512], ktail[par][:, hp * 128:(hp + 1) * 128], vbf[par][:, hp * 256:(hp + 1) * 256], start=True, stop=True)
            for hp in range(2):
                for hh in range(2):
                    ho = hh * 64
                    P.stt(S32[ho:ho + 64, hp, :], S32[ho:ho + 64, hp, :], dec[par][ho:ho + 64, hp:hp + 1],
                          sbank[hp][ho:ho + 64, 256 + hh * 128:256 + (hh + 1) * 128], ALU.mult, ALU.add)
            P.copy(Sbf[:], S32[:], e="pool")
            for h in range(4):
                P.op("dve", lambda g, h=h: g.bn_stats(st[:, h, :].ap, ps[7][:, h * 128:(h + 1) * 128].ap),
                     reads=[ps[7][:]], writes=[st[:]])
                P.op("dve", lambda g, h=h: g.bn_aggr(mv[:, h, :].ap, st[:, h, :].ap), reads=[st[:]], writes=[mv[:]])
            rstd_from_var(P, rstd[:], mv[:, :, 1], LN_EPS, tmp4[:])
            for h in range(4):
                P.ts(yn[:, h * 128:(h + 1) * 128], ps[7][:, h * 128:(h + 1) * 128], mv[:, h, 0:1], rstd[:, h:h + 1],
                     ALU.subtract, ALU.mult)
            P.tt(ybf[:], yn[:], sg[par][:], ALU.mult, e="pool")
            psb = V(ps[5][:].ap.bitcast(BF16), ps[5].regs)
            for q in range(4):
                P.tr(psb[:, q * 128:(q + 1) * 128], ybf[:, q * 128:(q + 1) * 128], k.identb[:])
            P.copy(k.yT[0][:, :, tsl],
                   V(psb[:, 0:512].ap.rearrange("p (q t) -> p q t", q=4), ps[5].regs), e="act")

        stage_a(0)
        for c in range(NCHUNK):
            if c + 1 < NCHUNK:
                stage_a(c + 1)
            stage_b(c)


def v3(v, pat, **kw):
    return V(v.ap.rearrange(pat, **kw), v.regs)


def mlstm_phase(P, k, L, w):
    ps = k.ps
    o0 = OFF['mq']
    with Scope(P):
        WM = Tile(P, "WM", [128, 8, 1544], BF16)
        brow = Tile(P, "m_brow", [1, 1544], BF16)
        gb8 = Tile(P, "m_gb8", [1, 8], F32)
        WIF = Tile(P, "WIF", [128, 8, 512], BF16)
        browIF = Tile(P, "m_browIF", [1, 512], F32)
        cw = Tile(P, "m_cw", [128, 4, 4], F32)
        cb = Tile(P, "m_cb", [128, 4], F32)
        ngm = Tile(P, "m_ng", [128, 512], F32)
        P.dma("sp", gb8[:], D(w['b_in'][L:L + 1, o0 + 1024:o0 + 1032]), gb8[:])
        P.dma("pool", WM[:], D(w['w_in'][L, :, o0:o0 + 1544].rearrange("(kt p) n -> p kt n", p=128)), WM[:])
        P.dma("pool", brow[:], D(w['b_in'][L:L + 1, o0:o0 + 1544]), brow[:])
        for tap in range(4):
            P.dma("sp", cw[:, tap, :], D(w['ml_conv_w'][L, tap].rearrange("(j p) -> p j", p=128)), cw[:], allow_slow_non_contiguous=True)
        P.dma("sp", cb[:], D(w['ml_conv_b'][L].rearrange("(j p) -> p j", p=128)), cb[:], allow_slow_non_contiguous=True)
        P.dma("sp", ngm[:], D(w['ml_norm_g'][L].partition_broadcast(128)), ngm[:])
        for gi in range(2):
            for h in range(4):
                col = 1024 + gi * 4 + h
                dst = slice(gi * 256 + h * 64, gi * 256 + (h + 1) * 64)
                P.copy(WIF[:, :, dst], WM[:, :, col:col + 1].bcast([128, 8, 64]), e="dve")
                P.copy(browIF[0:1, dst], gb8[0:1, gi * 4 + h:gi * 4 + h + 1].bcast([1, 64]), e="dve")
        bif_hi = Tile(P, "m_bifhi", [1, 512], BF16)
        bif_lo = Tile(P, "m_biflo", [1, 512], BF16)
        P.copy(bif_hi[:], browIF[:], e="dve")
        P.tt(browIF[:], browIF[:], bif_hi[:], ALU.subtract)
        P.copy(bif_lo[:], browIF[:], e="dve")
        raw = Tile(P, "m_raw4", [128, 4, 515], BF16)
        gif = Tile(P, "m_gif", [128, 4, 512], F32)
        acc = Tile(P, "m_acc", [128, 4, 128], F32)
        kf = Tile(P, "m_kf", [128, 2, 128], F32)
        spf = Tile(P, "m_spf", [128, 2, 128], F32)
        halo = Tile(P, "m_halo", [128, 4, 3], BF16)
        ncum = Tile(P, "m_ncum", [128, 2, 128], F32)
        aT = Tile(P, "m_aT", [128, 2, 128], F32)
        ksc = spf
        clT = Tile(P, "m_clT", [128, 2, 128], F32)
        gfacf = Tile(P, "m_gfacf", [128, 512], BF16)
        mst = Tile(P, "m_mst", [128, 2], F32)
        sm = Tile(P, "m_sm", [128, 16], F32)
        qfT = [Tile(P, f"m_qfT{i}", [128, 2, 128], BF16) for i in range(2)]
        kpT = [Tile(P, f"m_kpT{i}", [128, 2, 128], BF16) for i in range(2)]
        kptok = [Tile(P, f"m_kptok{i}", [128, 256], BF16) for i in range(2)]
        clampv = [Tile(P, f"m_clampv{i}", [128, 4], F32) for i in range(2)]
        vaug = [Tile(P, f"m_vaug{i}", [128, 4, 129], BF16) for i in range(2)]
        gfac = [Tile(P, f"m_gfac{i}", [128, 512], BF16) for i in range(2)]
        wdec = [Tile(P, f"m_wdec{i}", [128, 2], F32) for i in range(2)]
        C32 = Tile(P, "m_C32", [128, 2, 129], F32)
        Cbf = Tile(P, "m_Cbf", [128, 2, 129], BF16)
        attTb = Tile(P, "m_attTb", [128, 4, 128], BF16)
        st = Tile(P, "m_st", [128, 4, 6], F32)
        mv = Tile(P, "m_mv", [128, 4, 2], F32)
        sm2 = Tile(P, "m_sm2", [128, 6, 4], F32)
        yn = Tile(P, "m_yn", [128, 512], F32)
        ybf = Tile(P, "m_ybf", [128, 512], BF16)
        P.memset(raw[:], 0.0)
        P.memset(C32[:], 0.0)
        P.memset(mst[:], 0.0)
        for i in range(2):
            P.memset(vaug[i][:], 1.0)

        def stage_a(c):
            par = c % 2
            tsl = slice(c * 128, (c + 1) * 128)
            off = (c % 4) * 128
            osl = slice(off, off + 128)
            if c % 4 == 0:
                t4 = slice(c * 128, c * 128 + 512)
                P.copy(halo[:], raw[:, :, 512:515], e="pool")
                for i in range(8):
                    cols = slice((i % 4) * 128, (i % 4 + 1) * 128)
                    bank = ps[i % 2]
                    if i < 4:
                        P.mm(bank[:], brow[0:1, cols], k.onesb[0:1, 0:512], start=True, stop=False)
                        Wsrc = WM
                    else:
                        P.mm(bank[:], bif_hi[0:1, cols], k.onesb[0:1, 0:512], start=True, stop=False)
                        P.mm(bank[:], bif_lo[0:1, cols], k.onesb[0:1, 0:512], start=False, stop=False)
                        Wsrc = WIF
                    for kt in range(8):
                        P.mm(bank[:], Wsrc[:, kt, cols], k.xT[:, kt, t4], start=False, stop=(kt == 7))
                    if i < 4:
                        P.copy(raw[:, i, 3:515], bank[:], e="act")
                    else:
                        P.copy(gif[:, i - 4, :], bank[:], e="act")
                P.copy(raw[:, :, 0:3], halo[:], e="pool")
            for i in range(4):
                P.ts(acc[:, i, :], raw[:, i, off:off + 128], cw[:, 0, i:i + 1], cb[:, i:i + 1], ALU.mult, ALU.add)
                for tap in range(1, 4):
                    P.stt(acc[:, i, :], raw[:, i, off + tap:off + tap + 128], cw[:, tap, i:i + 1], acc[:, i, :], ALU.mult, ALU.add)
            P.act(qfT[par][:], acc[:, 0:2, :], AF.Silu)
            P.act(kf[:], acc[:, 2:4, :], AF.Silu)
            gi_ps = gif[:, 0:2, osl]
            gf_ps = gif[:, 2:4, osl]
            P.act(spf[:], gf_ps, AF.Exp, scale=-1.0)
            P.act(spf[:], spf[:], AF.Ln, bias=1.0)
            for i in range(2):
                P.op("dve", lambda g, i=i: g.tensor_tensor_scan(ncum[:, i, :].ap, k.ones[:, 0:128].ap, spf[:, i, :].ap, 0.0,
                                                                 ALU.mult, ALU.add),
                     reads=[k.ones[:], spf[:]], writes=[ncum[:]])
            P.tt(aT[:], gi_ps, ncum[:], ALU.add)
            P.reduce(sm[:, 0:2], aT[:], ALU.max)
            P.tt(sm[:, 2:4], sm[:, 0:2], mst[:], ALU.max)
            P.ts(sm[:, 4:6], sm[:, 2:4], -1.0, None, ALU.mult)
            P.tt(sm[:, 8:10], mst[:], sm[:, 2:4], ALU.subtract)
            P.act(wdec[par][:], sm[:, 8:10], AF.Exp)
            for i in range(2):
                P.act(ksc[:, i, :], aT[:, i, :], AF.Exp, bias=sm[:, 4 + i:5 + i])
                P.act(clT[:, i, :], ncum[:, i, :], AF.Exp, bias=sm[:, 4 + i:5 + i])
            P.stt(kpT[par][:], kf[:], 0.125, ksc[:], ALU.mult, ALU.mult)
            P.tt(mst[:], sm[:, 2:4], ncum[:, :, 127], ALU.subtract)
            for i in range(2):
                P.tr(ps[1][:, i * 128:(i + 1) * 128], clT[:, i, :], k.ident[:])
            psb1 = V(ps[1][:].ap.bitcast(BF16), ps[1].regs)
            for i in range(2):
                P.tr(psb1[:, 512 + i * 128:512 + (i + 1) * 128], kpT[par][:, i, :], k.identb[:])
            P.copy(clampv[par][:], v3(ps[1][:, 0:256], "p (h r) -> p h r", r=64)[:, :, 0], e="act")
            P.copy(kptok[par][:], psb1[:, 512:768], e="act")
            vc = slice(512, 1024)
            P.mm(ps[2][:], k.onesb[0:1, 0:128], brow[0:1, vc], start=True, stop=False)
            for kt in range(8):
                P.mm(ps[2][:], k.xT[:, kt, tsl], WM[:, kt, vc], start=False, stop=(kt == 7))
            oc = slice(1032, 1544)
            P.mm(ps[3][:], k.onesb[0:1, 0:128], brow[0:1, oc], start=True, stop=False)
            for kt in range(8):
                P.mm(ps[3][:], k.xT[:, kt, tsl], WM[:, kt, oc], start=False, stop=(kt == 7))
            P.copy(vaug[par][:, :, 0:128], v3(ps[2][:], "p (h e) -> p h e", h=4), e="act")
            P.act(gfacf[:], ps[3][:], AF.Sigmoid)
            P.tt(gfac[par][:], gfacf[:], ngm[:], ALU.mult, e="pool")

        def stage_b(c):
            par = c % 2
            tsl = slice(c * 128, (c + 1) * 128)
            for i in range(2):
                P.ts(C32[:, i, :], C32[:, i, :], wdec[par][:, i:i + 1], None, ALU.mult)
            P.copy(Cbf[:], C32[:], e="pool")
            abank = {0: (ps[4], 0), 2: (ps[4], 128), 1: (ps[5], 0), 3: (ps[5], 128)}
            for h in (0, 2, 1, 3):
                hp, ho = h // 2, (h % 2) * 64
                bk, co = abank[h]
                P.mm(bk[:, co:co + 128], kpT[par][ho:ho + 64, hp, :], qfT[par][ho:ho + 64, hp, :], start=True, stop=True)
            for h in range(4):
                bk, co = abank[h]
                P.tt(attTb[:, h, :], bk[:, co:co + 128], k.mask01[:], ALU.mult)
            oa = []
            for h in range(4):
                hp, ho = h // 2, (h % 2) * 64
                bank = ps[6] if h < 2 else ps[7]
                o_ = bank[:, (h % 2) * 129:(h % 2) * 129 + 129]
                oa.append(o_)
                P.mm(o_, attTb[:, h, :], vaug[par][:, h, :], start=True, stop=False)
                P.mm(o_, qfT[par][ho:ho + 64, hp, :], Cbf[ho:ho + 64, hp, :], start=False, stop=True)
            for hp in range(2):
                pss = ps[4 + hp]
                P.mm(pss[:, 0:258], kptok[par][:, hp * 128:(hp + 1) * 128],
                     v3(vaug[par][:, 2 * hp:2 * hp + 2, :], "p h e -> p (h e)"), start=True, stop=True)
            for hp in range(2):
                pss = ps[4 + hp]
                for hh in range(2):
                    ho = hh * 64
                    P.tt(C32[ho:ho + 64, hp, :], C32[ho:ho + 64, hp, :], pss[ho:ho + 64, hh * 129:(hh + 1) * 129], ALU.add)
            for h in range(4):
                P.act(sm2[:, 0, h:h + 1], oa[h][:, 128:129], AF.Abs)
            for h in range(4):
                P.op("dve", lambda g, h=h: g.bn_stats(st[:, h, :].ap, oa[h][:, 0:128].ap), reads=[oa[h]], writes=[st[:]])
                P.op("dve", lambda g, h=h: g.bn_aggr(mv[:, h, :].ap, st[:, h, :].ap), reads=[st[:]], writes=[mv[:]])
            P.tt(sm2[:, 0, :], sm2[:, 0, :], clampv[par][:], ALU.max)
            P.recip(sm2[:, 1, :], sm2[:, 0, :])
            P.tt(sm2[:, 2, :], sm2[:, 1, :], sm2[:, 1, :], ALU.mult)
            P.tt(sm2[:, 3, :], mv[:, :, 1], sm2[:, 2, :], ALU.mult)
            rstd_from_var(P, sm2[:, 4, :], sm2[:, 3, :], LN_EPS, sm2[:, 4, :])
            P.tt(sm2[:, 5, :], sm2[:, 4, :], sm2[:, 1, :], ALU.mult)
            for h in range(4):
                P.ts(yn[:, h * 128:(h + 1) * 128], oa[h][:, 0:128], mv[:, h, 0:1], sm2[:, 5, h:h + 1], ALU.subtract, ALU.mult)
            P.tt(ybf[:], yn[:], gfac[par][:], ALU.mult, e="pool")
            psb = V(ps[4][:].ap.bitcast(BF16), ps[4].regs)
            for q in range(4):
                P.tr(psb[:, q * 128:(q + 1) * 128], ybf[:, q * 128:(q + 1) * 128], k.identb[:])
            P.copy(k.yT[2][:, :, tsl],
                   V(psb[:, 0:512].ap.rearrange("p (q t) -> p q t", q=4), ps[4].regs), e="act")

        stage_a(0)
        for c in range(NCHUNK):
            if c + 1 < NCHUNK:
                stage_a(c + 1)
            stage_b(c)


import math
TWO_PI = 2.0 * math.pi


class StopBuild(Exception):
    pass


def stop_at(n):
    if S5_STOP <= n:
        MUTE[0] = True


MUTE = [False]


def s5_phase(P, k, L, w):
    ps = k.ps
    T16 = [128, 16]
    with Scope(P):
        ETre = Tile(P, "s_ETre", [128, 16, 128], BF16)
        ETim = Tile(P, "s_ETim", [128, 16, 128], BF16)
        Epre = Tile(P, "s_Epre", [128, 16, 128], BF16)
        Epim = Tile(P, "s_Epim", [128, 16, 128], BF16)
        lam_re = Tile(P, "s_lamre", T16, F32)
        lam_im = Tile(P, "s_lamim", T16, F32)
        with Scope(P):
            are = Tile(P, "s_are", T16, F32)
            aim = Tile(P, "s_aim", T16, F32)
            ldt = Tile(P, "s_ldt", T16, F32)
            P.dma("sp", are[:], D(w['s5_a_re'][L].rearrange("(j gl) p -> (gl p) j", gl=2)), are[:], allow_slow_non_contiguous=True)
            P.dma("sp", aim[:], D(w['s5_a_im'][L].rearrange("(j gl) p -> (gl p) j", gl=2)), aim[:], allow_slow_non_contiguous=True)
            ldt_src = w['s5_log_dt'][L].rearrange("(j gl) -> gl j", gl=2)
            for gl in range(2):
                P.dma("sp", ldt[gl * 64:(gl + 1) * 64, :], D(ldt_src[gl].partition_broadcast(64)), ldt[:], allow_slow_non_contiguous=True)
            sm = {n: Tile(P, "sq_" + n, T16, F32) for n in
                  ["dt", "re", "im", "mag", "magi", "t0", "t1", "sinv", "cosv", "li_re", "li_im", "cf_re", "cf_im", "pw_re", "pw_im", "q_re", "q_im"]}
            ni = Tile(P, "s_ni", T16, I32)
            e = "dve"
            P.act(sm["dt"][:], ldt[:], AF.Exp)
            P.tt(sm["re"][:], are[:], sm["dt"][:], ALU.mult)
            P.tt(sm["im"][:], aim[:], sm["dt"][:], ALU.mult)
            P.act(sm["mag"][:], sm["re"][:], AF.Exp)
            P.act(sm["magi"][:], sm["re"][:], AF.Exp, scale=-1.0)

            def wrap(t):
                P.ts(sm["t1"][:], t, math.pi, TWO_PI, ALU.is_gt, ALU.mult)
                P.tt(t, t, sm["t1"][:], ALU.subtract)
                P.ts(sm["t1"][:], t, -math.pi, TWO_PI, ALU.is_lt, ALU.mult)
                P.tt(t, t, sm["t1"][:], ALU.add)

            P.ts(sm["t0"][:], sm["im"][:], 1.0 / TWO_PI, None, ALU.mult)
            P.copy(ni[:], sm["t0"][:])
            P.copy(sm["t0"][:], ni[:])
            P.stt(sm["im"][:], sm["t0"][:], -TWO_PI, sm["im"][:], ALU.mult, ALU.add)
            wrap(sm["im"][:])
            P.act(sm["sinv"][:], sm["im"][:], AF.Sin)
            P.ts(sm["t0"][:], sm["im"][:], math.pi / 2, None, ALU.add)
            wrap(sm["t0"][:])
            P.act(sm["cosv"][:], sm["t0"][:], AF.Sin)
            P.tt(lam_re[:], sm["mag"][:], sm["cosv"][:], ALU.mult)
            P.tt(lam_im[:], sm["mag"][:], sm["sinv"][:], ALU.mult)
            P.tt(sm["li_re"][:], sm["magi"][:], sm["cosv"][:], ALU.mult)
            P.stt(sm["li_im"][:], sm["magi"][:], -1.0, sm["sinv"][:], ALU.mult, ALU.mult)
            P.ts(sm["q_re"][:], lam_re[:], -1.0, None, ALU.add)
            P.tt(sm["t0"][:], are[:], are[:], ALU.mult)
            P.tt(sm["t1"][:], aim[:], aim[:], ALU.mult)
            P.tt(sm["t0"][:], sm["t0"][:], sm["t1"][:], ALU.add)
            P.recip(sm["q_im"][:], sm["t0"][:])
            P.tt(sm["t0"][:], sm["q_re"][:], are[:], ALU.mult)
            P.tt(sm["t1"][:], lam_im[:], aim[:], ALU.mult)
            P.tt(sm["t0"][:], sm["t0"][:], sm["t1"][:], ALU.add)
            P.tt(sm["cf_re"][:], sm["t0"][:], sm["q_im"][:], ALU.mult)
            P.tt(sm["t0"][:], lam_im[:], are[:], ALU.mult)
            P.tt(sm["t1"][:], sm["q_re"][:], aim[:], ALU.mult)
            P.tt(sm["t0"][:], sm["t0"][:], sm["t1"][:], ALU.subtract)
            P.tt(sm["cf_im"][:], sm["t0"][:], sm["q_im"][:], ALU.mult)
            if S5_STOP <= 1:
                return
            Tre = Tile(P, "s_Tre", [128, 16, 128], F32)
            Tim = Tile(P, "s_Tim", [128, 16, 128], F32)
            tA = Tile(P, "s_tA", [128, 16, 64], F32)
            tB = Tile(P, "s_tB", [128, 16, 64], F32)

            def build_table(bre, bim):
                P.memset(Tre[:, :, 0:1], 1.0)
                P.memset(Tim[:, :, 0:1], 0.0)
                P.copy(Tre[:, :, 1], bre)
                P.copy(Tim[:, :, 1], bim)
                P.copy(sm["pw_re"][:], bre)
                P.copy(sm["pw_im"][:], bim)
                for kk in range(1, 7):
                    n = 1 << kk
                    P.tt(sm["t0"][:], sm["pw_re"][:], sm["pw_re"][:], ALU.mult)
                    P.tt(sm["t1"][:], sm["pw_im"][:], sm["pw_im"][:], ALU.mult)
                    P.tt(sm["q_re"][:], sm["pw_re"][:], sm["pw_im"][:], ALU.mult)
                    P.tt(sm["pw_re"][:], sm["t0"][:], sm["t1"][:], ALU.subtract)
                    P.ts(sm["pw_im"][:], sm["q_re"][:], 2.0, None, ALU.mult)
                    pr = v3(sm["pw_re"][:], "p (j o) -> p j o", o=1).bcast([128, 16, n])
                    pi_ = v3(sm["pw_im"][:], "p (j o) -> p j o", o=1).bcast([128, 16, n])
                    P.tt(tA[:, :, 0:n], Tre[:, :, 0:n], pr, ALU.mult)
                    P.tt(tB[:, :, 0:n], Tim[:, :, 0:n], pi_, ALU.mult, e="pool")
                    P.tt(Tre[:, :, n:2 * n], tA[:, :, 0:n], tB[:, :, 0:n], ALU.subtract)
                    P.tt(tA[:, :, 0:n], Tre[:, :, 0:n], pi_, ALU.mult)
                    P.tt(tB[:, :, 0:n], Tim[:, :, 0:n], pr, ALU.mult, e="pool")
                    P.tt(Tim[:, :, n:2 * n], tA[:, :, 0:n], tB[:, :, 0:n], ALU.add)

            build_table(lam_re[:], lam_im[:])
            P.copy(ETre[:], Tre[:], e="act")
            P.copy(ETim[:], Tim[:], e="act")
            build_table(sm["li_re"][:], sm["li_im"][:])
            cr = v3(sm["cf_re"][:], "p (j o) -> p j o", o=1)
            ci = v3(sm["cf_im"][:], "p (j o) -> p j o", o=1)
            for hh in range(2):
                sl = slice(hh * 64, (hh + 1) * 64)
                crb, cib = cr.bcast([128, 16, 64]), ci.bcast([128, 16, 64])
                P.tt(tA[:], Tre[:, :, sl], crb, ALU.mult)
                P.tt(tB[:], Tim[:, :, sl], cib, ALU.mult, e="pool")
                P.tt(tA[:], tA[:], tB[:], ALU.subtract)
                P.tt(tB[:], Tre[:, :, sl], cib, ALU.mult, e="pool")
                P.tt(Tim[:, :, sl], Tim[:, :, sl], crb, ALU.mult)
                P.tt(Tim[:, :, sl], Tim[:, :, sl], tB[:], ALU.add)
                P.copy(Tre[:, :, sl], tA[:])
            for (src, dst) in ((Tre, Epre), (Tim, Epim)):
                for g4 in range(4):
                    bank = ps[g4 % 2]
                    for jj in range(4):
                        P.tr(bank[:, jj * 128:(jj + 1) * 128], src[:, g4 * 4 + jj, :], k.ident[:])
                    P.copy(dst[:, g4 * 4:g4 * 4 + 4, :], v3(bank[:], "p (j q) -> p j q", j=4), e="act" if g4 % 2 == 0 else "dve")
        if S5_STOP <= 2:
            return
        BTre = Tile(P, "s_BTre", [128, 16, 128], BF16)
        BTim = Tile(P, "s_BTim", [128, 16, 128], BF16)
        CTre = Tile(P, "s_CTre", [128, 16, 128], BF16)
        CTimn = Tile(P, "s_CTimn", [128, 16, 128], BF16)
        Wu = Tile(P, "s_Wu", [128, 8, 512], BF16)
        Wglu = Tile(P, "s_Wglu", [128, 4, 512], BF16)
        browu = Tile(P, "s_browu", [1, 512], F32)
        bglu = Tile(P, "s_bglu", [128, 4], F32)
        dvec = Tile(P, "s_dvec", [128, 4], F32)
        o0 = OFF['su']
        P.dma("pool", Wu[:], D(w['w_in'][L, :, o0:o0 + 512].rearrange("(kt p) n -> p kt n", p=128)), Wu[:])
        P.dma("pool", Wglu[:], D(w['s5_w_glu'][L].rearrange("(kt p) n -> p kt n", p=128)), Wglu[:])
        P.dma("sp", browu[:], D(w['b_in'][L:L + 1, o0:o0 + 512]), browu[:])
        P.dma("sp", bglu[:], D(w['s5_b_glu'][L].rearrange("(j p) -> p j", p=128)), bglu[:], allow_slow_non_contiguous=True)
        P.dma("sp", dvec[:], D(w['s5_d'][L].rearrange("(j p) -> p j", p=128)), dvec[:], allow_slow_non_contiguous=True)
        with Scope(P):
            Bnat = Tile(P, "s_Bnat", [128, 16, 16], F32)
            Bpad = Tile(P, "s_Bpad", [128, 16, 128], F32)
            Cnat = Tile(P, "s_Cnat", [128, 4, 128], F32)
            Cfull = Tile(P, "s_Cfull", [128, 4, 128], F32)
            P.memset(Bpad[:], 0.0)
            P.memset(CTre[:], 0.0)
            P.memset(CTimn[:], 0.0)
            for (nm, BT) in (("s5_b_re", BTre), ("s5_b_im", BTim)):
                P.dma("sp", Bnat[:], D(w[nm][L].rearrange("g p n -> (g p) n").rearrange("(j q) n -> q j n", q=128)), Bnat[:])
                Bp4 = v3(Bpad[:], "q (kt jj) c -> q kt jj c", jj=4)
                Bn4 = v3(Bnat[:], "q (kt jj) n -> q kt jj n", jj=4)
                for jj in range(4):
                    for half in range(2):
                        c0 = (2 * jj + half) * 16
                        P.copy(Bp4[half * 64:(half + 1) * 64, :, jj, c0:c0 + 16], Bn4[half * 64:(half + 1) * 64, :, jj, :])
                for g4 in range(4):
                    bank = ps[g4 % 2]
                    for jj in range(4):
                        P.tr(bank[:, jj * 128:(jj + 1) * 128], Bpad[:, g4 * 4 + jj, :], k.ident[:])
                    P.copy(BT[:, g4 * 4:g4 * 4 + 4, :], v3(bank[:], "p (j q) -> p j q", j=4), e="act" if g4 % 2 == 0 else "dve")
            for (nm, CT, sgn) in (("s5_c_re", CTre, 1.0), ("s5_c_im", CTimn, -1.0)):
                csrc = w[nm][L].rearrange("(kt g8) n p -> (g8 n) kt p", g8=8)
                P.dma("sp", Cnat[:, :, 0:64], D(csrc), Cnat[:])
                P.dma("sp", Cnat[:, :, 64:128], D(csrc), Cnat[:])
                for kt in range(4):
                    P.tr(ps[2][:, kt * 128:(kt + 1) * 128], Cnat[:, kt, :], k.ident[:])
                P.copy(Cfull[:], v3(ps[2][:], "p (kt c) -> p kt c", kt=4), e="act")
                C4 = v3(CT[:], "q (kt jj) c -> q kt jj c", jj=4)
                for jj in range(4):
                    for half in range(2):
                        c0 = (2 * jj + half) * 16
                        P.ts(C4[half * 64:(half + 1) * 64, :, jj, c0:c0 + 16], Cfull[half * 64:(half + 1) * 64, :, c0:c0 + 16],
                             sgn, None, ALU.mult)
        if S5_STOP <= 3:
            return
        uT32s = [Tile(P, f"s_uT32{i}", [128, 4, 128], F32) for i in range(2)]
        uTbs = [Tile(P, f"s_uTb{i}", [128, 4, 128], BF16) for i in range(2)]
        bure = Tile(P, "s_bure", [128, 4, 128], F32)
        buim = Tile(P, "s_buim", [128, 4, 128], F32)
        t1 = Tile(P, "s_t1", [128, 4, 128], F32)
        t2 = Tile(P, "s_t2", [128, 4, 128], F32)
        t3 = Tile(P, "s_t3", [128, 4, 128], F32)
        t4 = Tile(P, "s_t4", [128, 4, 128], F32)
        Wre = Tile(P, "s_Wre", [128, 4, 128], BF16)
        Wim = Tile(P, "s_Wim", [128, 4, 128], BF16)
        Zre = Tile(P, "s_Zre", [128, 4, 128], F32)
        Zim = Tile(P, "s_Zim", [128, 4, 128], F32)
        Xre = Tile(P, "s_Xre", [128, 4, 128], BF16)
        Xim = Tile(P, "s_Xim", [128, 4, 128], BF16)
        xe_re = Tile(P, "s_xere", T16, F32)
        xe_im = Tile(P, "s_xeim", T16, F32)
        c_re = Tile(P, "s_cre", T16, F32)
        c_im = Tile(P, "s_cim", T16, F32)
        s1 = Tile(P, "s_s1", T16, F32)
        s2 = Tile(P, "s_s2", T16, F32)
        ysk = Tile(P, "s_ysk", [128, 4, 128], F32)
        ygb = Tile(P, "s_ygb", [128, 4, 128], BF16)
        P.memset(c_re[:], 0.0)
        P.memset(c_im[:], 0.0)
        zbank = [(ps[3], ps[4]), (ps[5], ps[6])]

        def front1(u):
            c, g = divmod(u, 4)
            tsl = slice(c * 128, (c + 1) * 128)
            uT32, uTb = uT32s[c % 2], uTbs[c % 2]
            if g == 0:
                for i in range(4):
                    cols = slice(i * 128, (i + 1) * 128)
                    proj_fm(P, k, ps[0][:, i * 128:(i + 1) * 128], Wu, cols, browu, cols, tsl, 128)
                u_ps = v3(ps[0][:], "p (i t) -> p i t", i=4)
                P.copy(uT32[:], u_ps, e="act")
                P.copy(uTb[:], u_ps, e="act")
            js = slice(4 * g, 4 * g + 4)
            zr, zi = zbank[u % 2]
            P.mm(ps[1][:], uTb[:, g, :], v3(BTre[:, js, :], "p j q -> p (j q)"), start=True, stop=True)
            P.mm(ps[2][:], uTb[:, g, :], v3(BTim[:, js, :], "p j q -> p (j q)"), start=True, stop=True)
            P.copy(bure[:], v3(ps[1][:], "p (j q) -> p j q", j=4), e="act")
            P.copy(buim[:], v3(ps[2][:], "p (j q) -> p j q", j=4), e="act")

        def front2(u):
            c, g = divmod(u, 4)
            js = slice(4 * g, 4 * g + 4)
            zr, zi = zbank[u % 2]
            P.tt(t1[:], Epre[:, js, :], bure[:], ALU.mult)
            P.tt(t2[:], Epim[:, js, :], buim[:], ALU.mult)
            P.tt(Wre[:], t1[:], t2[:], ALU.subtract)
            P.tt(t3[:], Epre[:, js, :], buim[:], ALU.mult)
            P.tt(t4[:], Epim[:, js, :], bure[:], ALU.mult, e="pool")
            P.tt(Wim[:], t3[:], t4[:], ALU.add, e="pool")
            for jj in range(4):
                P.mm(zr[:, jj * 128:(jj + 1) * 128], Wre[:, jj, :], k.mask01b[:], start=True, stop=True)
            for jj in range(4):
                P.mm(zi[:, jj * 128:(jj + 1) * 128], Wim[:, jj, :], k.mask01b[:], start=True, stop=True)


        def front(u):
            front1(u)
            front2(u)

        def back(u):
            c, g = divmod(u, 4)
            tsl = slice(c * 128, (c + 1) * 128)
            uT32 = uT32s[c % 2]
            js = slice(4 * g, 4 * g + 4)
            zr, zi = zbank[u % 2]
            for jj in range(4):
                j = 4 * g + jj
                P.act(Zre[:, jj, :], zr[:, jj * 128:(jj + 1) * 128], AF.Identity, bias=c_re[:, j:j + 1])
            for jj in range(4):
                j = 4 * g + jj
                P.act(Zim[:, jj, :], zi[:, jj * 128:(jj + 1) * 128], AF.Identity, bias=c_im[:, j:j + 1])
            P.tt(t1[:], ETre[:, js, :], Zre[:], ALU.mult)
            P.tt(t2[:], ETim[:, js, :], Zim[:], ALU.mult)
            P.tt(Xre[:], t1[:], t2[:], ALU.subtract)
            P.tt(t3[:], ETim[:, js, :], Zre[:], ALU.mult)
            P.tt(t4[:], ETre[:, js, :], Zim[:], ALU.mult, e="pool")
            P.tt(Xim[:], t3[:], t4[:], ALU.add, e="pool")
            P.tt(xe_re[:, js], t1[:, :, 127], t2[:, :, 127], ALU.subtract)
            P.tt(xe_im[:, js], t3[:, :, 127], t4[:, :, 127], ALU.add, e="pool")
            yo = ps[7][:, g * 128:(g + 1) * 128]
            for jj in range(4):
                P.mm(yo, CTre[:, 4 * g + jj, :], Xre[:, jj, :], start=(jj == 0), stop=False)
                P.mm(yo, CTimn[:, 4 * g + jj, :], Xim[:, jj, :], start=False, stop=(jj == 3))
            if g != 3:
                return
            P.tt(s1[:], lam_re[:], xe_re[:], ALU.mult)
            P.tt(s2[:], lam_im[:], xe_im[:], ALU.mult)
            P.tt(c_re[:], s1[:], s2[:], ALU.subtract)
            P.tt(s1[:], lam_re[:], xe_im[:], ALU.mult)
            P.tt(s2[:], lam_im[:], xe_re[:], ALU.mult)
            P.tt(c_im[:], s1[:], s2[:], ALU.add)
            for kt in range(4):
                P.stt(ysk[:, kt, :], uT32[:, kt, :], dvec[:, kt:kt + 1], ps[7][:, kt * 128:(kt + 1) * 128], ALU.mult, ALU.add)
            P.tt(t3[:], ysk[:], ysk[:], ALU.mult, e="pool")
            P.ts(t3[:], t3[:], 0.044715, 1.0, ALU.mult, ALU.add, e="pool")
            P.tt(t3[:], t3[:], ysk[:], ALU.mult, e="pool")
            P.act(t4[:], t3[:], AF.Sigmoid, scale=2.0 * math.sqrt(2.0 / math.pi))
            P.tt(t3[:], ysk[:], t4[:], ALU.mult)
            P.copy(ygb[:], t3[:], e="act")
            for kp in range(4):
                for kt in range(4):
                    P.mm(ps[0][:, kp * 128:(kp + 1) * 128], Wglu[:, kt, kp * 128:(kp + 1) * 128], ygb[:, kt, :],
                         start=(kt == 0), stop=(kt == 3))
            for kp in range(4):
                P.act(t4[:, kp, :], ps[0][:, kp * 128:(kp + 1) * 128], AF.Sigmoid, bias=bglu[:, kp:kp + 1])
            P.tt(k.yT[1][:, :, tsl], t3[:], t4[:], ALU.mult)

        nu = 4 * NCHUNK
        front(0)
        for u in range(nu):
            if S5_ORDER == 1:
                if u + 1 < nu:
                    front1(u + 1)
                back(u)
                if u + 1 < nu:
                    front2(u + 1)
            else:
                if u + 1 < nu:
                    front(u + 1)
                back(u)


def layer_norm_tile(P, k, xv, g, b, wk):
    st, mv, rs, tmp = wk["st"], wk["mv"], wk["rs"], wk["tmp"]
    for hf in range(2):
        P.op("dve", lambda gg, hf=hf: gg.bn_stats(st[:, hf, :].ap, xv[:, hf * 512:(hf + 1) * 512].ap), reads=[xv], writes=[st[:]])
    P.op("dve", lambda gg: gg.bn_aggr(mv[:].ap, v3(st[:], "p a b -> p (a b)").ap), reads=[st[:]], writes=[mv[:]])
    rstd_from_var(P, rs[:], mv[:, 1:2], LN_EPS, tmp[:])
    P.ts(xv, xv, mv[:, 0:1], rs[:, 0:1], ALU.subtract, ALU.mult)
    P.tt(xv, xv, g[:], ALU.mult, e="pool")
    P.tt(xv, xv, b[:], ALU.add, e="pool")


def ln_work(P, pfx):
    return dict(st=Tile(P, pfx + "st", [128, 2, 6], F32), mv=Tile(P, pfx + "mv", [128, 2], F32),
                rs=Tile(P, pfx + "rs", [128, 1], F32), tmp=Tile(P, pfx + "tmp", [128, 1], F32))


def merge_phase(P, k, L, w, moe):
    ps = k.ps
    go = OFF['gate']
    with Scope(P):
        mixedT = Tile(P, "mixedT", [128, 8, SEQ], BF16)
        with Scope(P):
            wgt = [Tile(P, f"wgt{i}", [128, 8, 3, 128], BF16) for i in range(2)]
            wup = [Tile(P, f"wup{i}", [128, 3, 4, 128], BF16) for i in range(2)]
            bnat = Tile(P, "bg_nat", [24, 128], F32)
            bgate = Tile(P, "bgate", [128, 24], F32)
            sig = [Tile(P, f"mg_sig{i}", [128, 512], F32) for i in range(2)]
            acc = Tile(P, "mg_acc", [128, 512], F32)
            tmp = [Tile(P, "mg_tmp0", [128, 512], F32)] * 2
            P.dma("sp", bnat[:], D(w['b_in'][L, go:go + 3072].rearrange("(r p) -> r p", p=128)), bnat[:])
            P.tr(ps[7][:, 0:24], bnat[:], k.ident[0:24, 0:24])
            P.copy(bgate[:], ps[7][:, 0:24])
            it = 0
            for dti in range(8):
                slot = dti % 2
                for b in range(3):
                    c0 = go + b * 1024 + dti * 128
                    P.dma("pool", wgt[slot][:, :, b, :], D(w['w_in'][L, :, c0:c0 + 128].rearrange("(kt p) n -> p kt n", p=128)), wgt[slot][:])
                    P.dma("pool", wup[slot][:, b, :, :], D(w['w_up'][L, b, :, dti * 128:(dti + 1) * 128].rearrange("(kt p) n -> p kt n", p=128)), wup[slot][:])
                for tb in range(4):
                    tsl = slice(tb * 512, (tb + 1) * 512)
                    for b in range(3):
                        pg, pu = ps[2 * (it % 2)], ps[2 * (it % 2) + 1]
                        for kt in range(8):
                            P.mm(pg[:], wgt[slot][:, kt, b, :], k.xT[:, kt, tsl], start=(kt == 0), stop=(kt == 7))
                        for kt in range(4):
                            P.mm(pu[:], wup[slot][:, b, kt, :], k.yT[b][:, kt, tsl], start=(kt == 0), stop=(kt == 3))
                        sg = sig[it % 2]
                        P.act(sg[:], pg[:], AF.Sigmoid, bias=bgate[:, b * 8 + dti:b * 8 + dti + 1])
                        if b == 0:
                            P.tt(acc[:], sg[:], pu[:], ALU.mult)
                        elif b == 1:
                            P.tt(tmp[0][:], sg[:], pu[:], ALU.mult)
                            P.tt(acc[:], acc[:], tmp[0][:], ALU.add, e="pool")
                        else:
                            P.tt(tmp[1][:], sg[:], pu[:], ALU.mult)
                            P.tt(mixedT[:, dti, tsl], acc[:], tmp[1][:], ALU.add, e="pool")
                        it += 1
        if MERGE_STOP == 1:
            return
        with Scope(P):
            Wo = Tile(P, "Wo", [128, 8, 1024], BF16)
            P.dma("pool", Wo[:], D(w['w_o'][L].rearrange("(kt p) n -> p kt n", p=128)), Wo[:])
            for tt in range(NT):
                xv = k.xres.sub(tt)
                tsl = slice(tt * 128, (tt + 1) * 128)
                for hf in range(2):
                    po = ps[2 + (2 * tt + hf) % 4]
                    for dti in range(8):
                        P.mm(po[:], mixedT[:, dti, tsl], Wo[:, dti, hf * 512:(hf + 1) * 512], start=(dti == 0), stop=(dti == 7))
                    P.stt(xv[:, hf * 512:(hf + 1) * 512], xv[:, hf * 512:(hf + 1) * 512], DN_ALPHA, po[:], ALU.mult, ALU.add)
        with Scope(P):
            g1 = Tile(P, "ln1g", [128, 1024], F32)
            b1 = Tile(P, "ln1b", [128, 1024], F32)
            wks = [ln_work(P, "ln1a_"), ln_work(P, "ln1b_")]
            P.dma("sp", g1[:], D(w['ln1_g'][L].partition_broadcast(128)), g1[:])
            P.dma("sp", b1[:], D(w['ln1_b'][L].partition_broadcast(128)), b1[:])
            if moe:
                Wr = Tile(P, "Wr", [128, 8, 8], F32)
                br = Tile(P, "br", [1, 8], F32)
                xTf = Tile(P, "xTf", [128, 8, 128], F32)
                rt = Tile(P, "rt", [128, 6, 8], F32)
                P.dma("sp", Wr[:], D(w['moe_router'][0].rearrange("(kt p) n -> p kt n", p=128)), Wr[:])
                P.dma("sp", br[:], D(w['moe_router_b'][0:1, :]), br[:])
            for tt in range(NT):
                xv = k.xres.sub(tt)
                layer_norm_tile(P, k, xv, g1, b1, wks[tt % 2])
                make_xT(P, k, tt, xtf=(xTf if moe else None), banks=((0, 1) if tt % 2 == 0 else (2, 3)))
                if moe:
                    lg = ps[4][:, 0:8]
                    P.mm(lg, k.ones[0:1, 0:128], br[:], start=True, stop=False)
                    for kt in range(8):
                        P.mm(lg, xTf[:, kt, :], Wr[:, kt, :], start=False, stop=(kt == 7))
                    P.copy(rt[:, 0, :], lg)
                    P.op("dve", lambda g_: g_.max(rt[:, 1, :].ap, rt[:, 0, :].ap), reads=[rt[:]], writes=[rt[:]])
                    P.ts(rt[:, 2, :], rt[:, 0, :], rt[:, 1, 1:2], None, ALU.is_ge)
                    P.ts(rt[:, 5, 0:1], rt[:, 1, 0:1], -1.0, None, ALU.mult)
                    P.act(rt[:, 3, :], rt[:, 0, :], AF.Exp, bias=rt[:, 5, 0:1])
                    P.tt(rt[:, 4, :], rt[:, 3, :], rt[:, 2, :], ALU.mult)
                    P.reduce(rt[:, 5, 1:2], rt[:, 4, :], ALU.add)
                    P.recip(rt[:, 5, 2:3], rt[:, 5, 1:2])
                    P.ts(k.comb[:, tt, :], rt[:, 4, :], rt[:, 5, 2:3], None, ALU.mult)


def ffn_phase(P, k, L, w, moe, out_dram, last):
    ps = k.ps
    with Scope(P):
        wg = [Tile(P, f"f_wg{i}", [128, 8, 512], BF16) for i in range(2)]
        wu = [Tile(P, f"f_wu{i}", [128, 8, 512], BF16) for i in range(2)]
        wd = [Tile(P, f"f_wd{i}", [128, 4, 1024], BF16) for i in range(2)]
        if moe:
            experts = [(w['moe_wg'][0, e], w['moe_wu'][0, e], w['moe_wd'][0, e], 3584, e) for e in range(N_EXP)]
        else:
            experts = [(w['ffn_wg'][0], w['ffn_wu'][0], w['ffn_wd'][0], 2816, None)]

        def issue_block(slot, Wg, Wu_, Wd, f0t, nf):
            cs = slice(f0t * 128, (f0t + nf) * 128)
            P.dma("pool", wg[slot][:, :, 0:nf * 128], D(Wg[:, cs].rearrange("(kt p) n -> p kt n", p=128)), wg[slot][:])
            P.dma("pool", wu[slot][:, :, 0:nf * 128], D(Wu_[:, cs].rearrange("(kt p) n -> p kt n", p=128)), wu[slot][:])
            P.dma("pool", wd[slot][:, 0:nf, :], D(Wd[cs, :].rearrange("(ft p) n -> p ft n", p=128)), wd[slot][:])

        with Scope(P):
            Wpg = Tile(P, "Wpg", [128, 8, 1024], BF16)
            Wpp = Tile(P, "Wpp", [128, 2, 1024], BF16)
            pt = [Tile(P, f"pt{i}", [128, 256], F32) for i in range(2)]
            pTb = Tile(P, "pTb", [128, 2, 128], BF16)
            sg = Tile(P, "ple_sg", [128, 1024], F32)
            P.dma("pool", Wpg[:], D(w['ple_w_gate'][L].rearrange("(kt p) n -> p kt n", p=128)), Wpg[:])
            P.dma("pool", Wpp[:], D(w['ple_w_proj'][L].rearrange("(kt p) n -> p kt n", p=128)), Wpp[:])
            issue_block(0, experts[0][0], experts[0][1], experts[0][2], 0, min(4, experts[0][3] // 128))
            for tt in range(NT):
                xv = k.xres.sub(tt)
                tsl = slice(tt * 128, (tt + 1) * 128)
                ptt = pt[tt % 2]
                P.dma("sp", ptt[:], D(w['p'][L, tt * 128:(tt + 1) * 128, :]), ptt[:])
                for q in range(2):
                    P.tr(ps[6][:, q * 128:(q + 1) * 128], ptt[:, q * 128:(q + 1) * 128], k.ident[:])
                P.copy(pTb[:], v3(ps[6][:, 0:256], "p (q t) -> p q t", q=2), e="act")
                for hf in range(2):
                    hs = slice(hf * 512, (hf + 1) * 512)
                    pg, pp = ps[2 + hf], ps[4 + hf]
                    for kt in range(8):
                        P.mm(pg[:], k.xT[:, kt, tsl], Wpg[:, kt, hs], start=(kt == 0), stop=(kt == 7))
                    for kt in range(2):
                        P.mm(pp[:], pTb[:, kt, :], Wpp[:, kt, hs], start=(kt == 0), stop=(kt == 1))
                    P.act(sg[:, hs], pg[:], AF.Sigmoid)
                    P.tt(sg[:, hs], sg[:, hs], pp[:], ALU.mult)
                    P.stt(xv[:, hs], xv[:, hs], DN_ALPHA, sg[:, hs], ALU.mult, ALU.add)
        if FFN_STOP == 1:
            return
        with Scope(P):
            actT = [Tile(P, f"f_act{i}", [128, 4, 512], BF16) for i in range(2)]
            sgl = [Tile(P, f"f_sg{i}", [128, 512], F32) for i in range(2)]
            cnt = 0
            it = 0
            ia = 0
            io = 0
            for (Wg, Wu_, Wd, dff, e) in experts:
                nft = dff // 128
                f0t = 0
                while f0t < nft:
                    nf = min(4, nft - f0t)
                    slot = cnt % 2
                    if cnt > 0:
                        issue_block(slot, Wg, Wu_, Wd, f0t, nf)
                    for tb in range(4):
                        tsl = slice(tb * 512, (tb + 1) * 512)
                        at = actT[ia % 2]
                        ia += 1
                        for ft in range(nf):
                            pg, pu = ps[it % 2], ps[2 + it % 2]
                            fs = slice(ft * 128, (ft + 1) * 128)
                            for kt in range(8):
                                P.mm(pg[:], wg[slot][:, kt, fs], k.xT[:, kt, tsl], start=(kt == 0), stop=(kt == 7))
                            for kt in range(8):
                                P.mm(pu[:], wu[slot][:, kt, fs], k.xT[:, kt, tsl], start=(kt == 0), stop=(kt == 7))
                            P.act(sgl[it % 2][:], pg[:], AF.Silu)
                            P.tt(at[:, ft, :], sgl[it % 2][:], pu[:], ALU.mult)
                            it += 1
                        for t4 in range(4):
                            tt = tb * 4 + t4
                            xv = k.xres.sub(tt)
                            for hf in range(2):
                                hs = slice(hf * 512, (hf + 1) * 512)
                                po = ps[4 + io % 4]
                                io += 1
                                for ft in range(nf):
                                    P.mm(po[:], at[:, ft, t4 * 128:(t4 + 1) * 128], wd[slot][:, ft, hs], start=(ft == 0), stop=(ft == nf - 1))
                                if e is None:
                                    P.tt(xv[:, hs], xv[:, hs], po[:], ALU.add)
                                else:
                                    P.stt(xv[:, hs], po[:], k.comb[:, tt, e:e + 1], xv[:, hs], ALU.mult, ALU.add)
                    f0t += nf
                    cnt += 1
        if FFN_STOP == 2:
            return
        with Scope(P):
            g2 = Tile(P, "ln2g", [128, 1024], F32)
            b2 = Tile(P, "ln2b", [128, 1024], F32)
            wks = [ln_work(P, "ln2a_"), ln_work(P, "ln2b_")]
            P.dma("sp", g2[:], D(w['ln2_g'][L].partition_broadcast(128)), g2[:])
            P.dma("sp", b2[:], D(w['ln2_b'][L].partition_broadcast(128)), b2[:])
            for tt in range(NT):
                xv = k.xres.sub(tt)
                layer_norm_tile(P, k, xv, g2, b2, wks[tt % 2])
                if last:
                    P.dma("sp", D(out_dram[tt * 128:(tt + 1) * 128, :]), xv, xv)
                else:
                    make_xT(P, k, tt, banks=((0, 1) if tt % 2 == 0 else (2, 3)))


N_EXP = int(os.environ.get('N_EXP', '8'))
MERGE_STOP = int(os.environ.get('MERGE_STOP', '0'))
FFN_STOP = int(os.environ.get('FFN_STOP', '0'))
N_LAYERS = int(os.environ.get('N_LAYERS', '2'))
DUMP = os.environ.get('DUMP', '')


def dump_xres(P, k, out_dram):
    for tt in range(NT):
        xv = k.xres.sub(tt)
        P.dma("sp", D(out_dram[tt * 128:(tt + 1) * 128, :]), xv, xv)


def build(n_layers=None, debug=None):
    n_layers = N_LAYERS if n_layers is None else n_layers
    nc = bass.Bass("TRN2", target_bir_lowering=False)
    w = {}
    for name, shp in INPUT_SHAPES.items():
        w[name] = nc.dram_tensor(name, list(shp), F32, kind="ExternalInput").ap()
    out = nc.dram_tensor("out", [SEQ, DM], F32, kind="ExternalOutput").ap()
    dbg = None
    if debug == "yT":
        dbg = nc.dram_tensor("dbg", [128, 3 * 4 * SEQ], BF16, kind="ExternalOutput").ap()
    with ExitStack() as es:
        P = Prog(nc, es)
        P.mute = MUTE
        k = K()
        setup_consts(P, k)
        k.xres = Tile(P, "xres", [128, NT, DM], F32, nreg=NT)
        k.xT = Tile(P, "xT", [128, 8, SEQ], BF16, nreg=NT)
        k.comb = Tile(P, "comb", [128, NT, 8], F32)
        load_x(P, k, w['x'])
        stop = False
        for L in range(n_layers):
            moe = (L % 2 == 1)
            with Scope(P):
                k.yT = {}
                k.yT[1] = Tile(P, "yT_s5", [128, 4, SEQ], BF16)
                if debug == 'yT':
                    P.memset(k.yT[1][:], 0.0)
                if 's5' in PHASES:
                    s5_phase(P, k, L, w)
                    MUTE[0] = False
                k.yT[2] = Tile(P, "yT_ml", [128, 4, SEQ], BF16)
                if debug == 'yT':
                    P.memset(k.yT[2][:], 0.0)
                if 'ml' in PHASES:
                    mlstm_phase(P, k, L, w)
                k.yT[0] = Tile(P, "yT_gla", [128, 4, SEQ], BF16)
                if debug == 'yT':
                    P.memset(k.yT[0][:], 0.0)
                if 'gla' in PHASES:
                    gla_phase(P, k, L, w)
                if debug == "yT":
                    for b in range(3):
                        P.dma("sp", D(dbg[:, b * 4 * SEQ:(b + 1) * 4 * SEQ]),
                              v3(k.yT[b][:], "p a t -> p (a t)"), k.yT[b][:])
                    stop = True
                else:
                    merge_phase(P, k, L, w, moe)
            if stop:
                break
            if DUMP == f"{L}:ln1":
                dump_xres(P, k, out)
                break
            ffn_phase(P, k, L, w, moe, out, last=(L == n_layers - 1))
            if DUMP == f"{L}:ln2" and L != n_layers - 1:
                dump_xres(P, k, out)
                break
        P.finish()
        print("ops", P.n_ops, "waits", P.n_waits, {e: P.cnt[e] for e in ENG}, "dsems", P.n_dsem)
    return nc


NCHUNK = 16
PHASES = ['gla', 's5', 'ml']
S5_STOP = 99
GLA_STOP = 0
GLA_VAR = 0
S5_ORDER = 0
N_EXP = 8
MERGE_STOP = 0
FFN_STOP = 0
N_LAYERS = 2
DUMP = ''

def kernel(**inputs):
    nc = build()
    shared = {k_: np.ascontiguousarray(np.asarray(v, dtype=np.float32)) for k_, v in inputs.items() if k_ not in ("x", "p")}
    xs = np.asarray(inputs["x"], dtype=np.float32)
    ps_ = np.asarray(inputs["p"], dtype=np.float32)
    n = xs.shape[0]
    maps = []
    for b in range(n):
        m = dict(shared)
        m["x"] = np.ascontiguousarray(xs[b])
        m["p"] = np.ascontiguousarray(ps_[:, b])
        maps.append(m)
    res = run_bass_kernel_spmd(nc, maps, core_ids=list(range(n)))
    return np.stack([np.asarray(res.results[b]["out"]) for b in range(n)]).astype(np.float32)
```

```python
import numpy as np
from contextlib import ExitStack
import concourse.bass as bass
import concourse.mybir as mybir
from concourse.bass_utils import run_bass_kernel_spmd

dt = mybir.dt
F32, BF16, I32 = dt.float32, dt.bfloat16, dt.int32
AF = mybir.ActivationFunctionType
ALU = mybir.AluOpType
AX = mybir.AxisListType

SPARSE_PE_INC = False
SAME_ENGINE_SYNC = True


class Region:
    __slots__ = ("name", "writer", "readers", "dma_sem", "dma_n", "dma_base", "uid", "excl", "dma_q")
    _next = [0]

    def __init__(self, name):
        self.name = name
        self.writer = None
        self.readers = {}
        self.dma_sem = None
        self.dma_n = 0
        self.dma_base = 0
        self.excl = False
        Region._next[0] += 1
        self.uid = Region._next[0]


class V:
    __slots__ = ("ap", "regs")

    def __init__(self, ap, regs):
        self.ap = ap
        self.regs = regs

    def __getitem__(self, idx):
        return V(self.ap[idx], self.regs)

    def bitcast(self, d):
        return V(self.ap.bitcast(d), self.regs)

    def bcast(self, shape):
        return V(self.ap.broadcast_to(shape), self.regs)


class Tile:
    _n = [0]

    def __init__(self, prog, name, shape, dtype, space="sbuf", nreg=1):
        Tile._n[0] += 1
        name = "%s_%d" % (name, Tile._n[0])
        self.name = name
        self.shape = shape
        if space == "sbuf":
            self.t = prog.es.enter_context(prog.nc.sbuf_tensor(name, shape, dtype))
        elif space == "psum":
            self.t = prog.es.enter_context(prog.nc.psum_tensor(name, shape, dtype))
        else:
            self.t = space
        self.regs = [Region(f"{name}.{i}") for i in range(nreg)]
        if space == "psum":
            for r in self.regs:
                r.excl = True
        prog.scope_regions[-1].extend(self.regs)

    def __getitem__(self, idx):
        return V(self.t[idx], self.regs)

    def sub(self, j):
        return V(self.t[:, j], [self.regs[j]])

    def reg(self, j, idx):
        return V(self.t[idx], [self.regs[j]])


ENG = ("pe", "act", "dve", "pool", "sp")


class Prog:
    def __init__(self, nc, es):
        self.nc = nc
        self.es = es
        self.eng = {"pe": nc.tensor, "act": nc.scalar, "dve": nc.vector, "pool": nc.gpsimd, "sp": nc.sync}
        self.sem = {e: es.enter_context(nc.semaphore("sem_" + e)) for e in ENG}
        self.cnt = {e: 0 for e in ENG}
        self.waited = {e: {f: 0 for f in ENG} for e in ENG}
        self.dma_waited = {e: {} for e in ENG}
        self.es0 = es
        self.scope_regions = [[]]
        self.sem_pool = {'pool': [], 'sp': [], 'act': []}
        self.n_dsem = 0
        self.dma_regions = []
        self.n_ops = 0
        self.n_waits = 0
        self.mute = None

    def _collect(self, e, reads, writes):
        deps = {}
        dmadeps = {}

        def add(w):
            if w is None:
                return
            if w[0] == "dma":
                r, n = w[1], w[2]
                k = r.uid
                if k not in dmadeps or dmadeps[k][1] < n:
                    dmadeps[k] = (r, n)
            else:
                f, i = w
                if f == e and (e == "pe" or not SAME_ENGINE_SYNC):
                    return
                if deps.get(f, 0) < i:
                    deps[f] = i

        for v in reads:
            for r in v.regs:
                add(r.writer)
                if r.excl:
                    for k, val in r.readers.items():
                        if not isinstance(k, tuple) and k != e:
                            add((k, val))
        for v in writes:
            for r in v.regs:
                add(r.writer)
                for k, val in r.readers.items():
                    if isinstance(k, tuple):
                        add(("dma", val[0], val[1]))
                    else:
                        add((k, val))
        return deps, dmadeps

    def _emit_waits(self, e, deps, dmadeps):
        eng = self.eng[e]
        for f, i in deps.items():
            if self.waited[e][f] >= i:
                continue
            if f == e and i < self.cnt[e]:
                pass
            eng.wait_ge(self.sem[f], i)
            self.n_waits += 1
            self.waited[e][f] = i
        for k, (r, n) in dmadeps.items():
            if self.dma_waited[e].get(k, 0) >= n:
                continue
            eng.wait_ge(r.dma_sem, r.dma_base + 16 * n)
            self.n_waits += 1
            self.dma_waited[e][k] = n

    def op(self, e, fn, reads=(), writes=(), inc=True):
        if self.mute is not None and self.mute[0]:
            return None
        deps, dmadeps = self._collect(e, reads, writes)
        self._emit_waits(e, deps, dmadeps)
        inst = fn(self.eng[e])
        if inc:
            self.cnt[e] += 1
            n = self.cnt[e]
            inst.then_inc(self.sem[e], 1)
        else:
            n = self.cnt[e] + 1
        for v in reads:
            for r in v.regs:
                if r.readers.get(e, 0) < n:
                    r.readers[e] = n
        for v in writes:
            for r in v.regs:
                r.writer = (e, n)
                r.readers = {}
        self.n_ops += 1
        return inst

    def dma(self, q, out, in_, sb, **kw):
        if self.mute is not None and self.mute[0]:
            return None
        deps, dmadeps = self._collect(q, [in_], [out])
        self._emit_waits(q, deps, dmadeps)
        r0 = sb.regs[0]
        if r0.dma_sem is None:
            r0.dma_q = q
            if self.sem_pool[q]:
                r0.dma_sem, r0.dma_base = self.sem_pool[q].pop()
            else:
                r0.dma_sem = self.es0.enter_context(self.nc.semaphore("dsem%d" % self.n_dsem))
                self.n_dsem += 1
                r0.dma_base = 0
            self.dma_regions.append(r0)
        assert r0.dma_q == q, (r0.name, r0.dma_q, q)
        inst = self.eng[q].dma_start(out=out.ap, in_=in_.ap, **kw)
        inst.then_inc(r0.dma_sem, 16)
        r0.dma_n += 1
        n = r0.dma_n
        for r in in_.regs:
            r.readers[("dma", r0.uid)] = (r0, n)
        for r in out.regs:
            r.writer = ("dma", r0, n)
            r.readers = {}
        return inst

    def push_scope(self):
        self.scope_regions.append([])

    def pop_scope(self):
        for r in self.scope_regions.pop():
            if r.dma_sem is not None:
                self.sem_pool[r.dma_q].append((r.dma_sem, r.dma_base + 16 * r.dma_n))
                self.dma_regions.remove(r)
                r.dma_sem = None

    def barrier(self):
        for e in ENG:
            deps = {f: self.cnt[f] for f in ENG if (f != e or e != 'pe') and self.cnt[f] > 0}
            dmadeps = {r.uid: (r, r.dma_n) for r in self.dma_regions}
            self._emit_waits(e, deps, dmadeps)

    def mm(self, out, lhsT, rhs, start=True, stop=True, **kw):
        return self.op("pe", lambda g: g.matmul(out.ap, lhsT.ap, rhs.ap, start=start, stop=stop, **kw),
                       reads=[lhsT, rhs] + ([] if start else [out]), writes=[out], inc=(stop or not SPARSE_PE_INC))

    def tr(self, out, in_, ident):
        return self.op("pe", lambda g: g.transpose(out.ap, in_.ap, ident.ap), reads=[in_, ident], writes=[out])

    def act(self, out, in_, func, bias=None, scale=None, accum=None, e="act"):
        kw = {}
        reads = [in_]
        if bias is not None:
            if isinstance(bias, V):
                kw["bias"] = bias.ap
                reads.append(bias)
            else:
                kw["bias"] = bias
        if scale is not None:
            if isinstance(scale, V):
                kw["scale"] = scale.ap
                reads.append(scale)
            else:
                kw["scale"] = scale
        writes = [out]
        if accum is not None:
            kw["accum_out"] = accum.ap
            writes.append(accum)
        return self.op(e, lambda g: g.activation(out.ap, in_.ap, func, **kw), reads=reads, writes=writes)

    def tt(self, out, a, b, op, e="dve"):
        return self.op(e, lambda g: g.tensor_tensor(out.ap, a.ap, b.ap, op), reads=[a, b], writes=[out])

    def ts(self, out, a, s1, s2, op0, op1=None, e="dve", accum=None):
        reads = [a]
        s1a, s2a = s1, s2
        if isinstance(s1, V):
            reads.append(s1)
            s1a = s1.ap
        if isinstance(s2, V):
            reads.append(s2)
            s2a = s2.ap
        kw = {}
        writes = [out]
        if accum is not None:
            kw["accum_out"] = accum.ap
            writes.append(accum)
        if op1 is None:
            return self.op(e, lambda g: g.tensor_scalar(out.ap, a.ap, s1a, s2a, op0, **kw), reads=reads, writes=writes)
        return self.op(e, lambda g: g.tensor_scalar(out.ap, a.ap, s1a, s2a, op0, op1, **kw), reads=reads, writes=writes)

    def stt(self, out, a, s, b, op0, op1, e="dve", accum=None):
        reads = [a, b]
        sa = s
        if isinstance(s, V):
            reads.append(s)
            sa = s.ap
        kw = {}
        writes = [out]
        if accum is not None:
            kw["accum_out"] = accum.ap
            writes.append(accum)
        return self.op(e, lambda g: g.scalar_tensor_tensor(out.ap, a.ap, sa, b.ap, op0, op1, **kw), reads=reads, writes=writes)

    def copy(self, out, in_, e="dve"):
        if e == "act":
            return self.op(e, lambda g: g.copy(out.ap, in_.ap), reads=[in_], writes=[out])
        return self.op(e, lambda g: g.tensor_copy(out.ap, in_.ap), reads=[in_], writes=[out])

    def memset(self, out, val, e="pool"):
        return self.op(e, lambda g: g.memset(out.ap, val), reads=[], writes=[out])

    def reduce(self, out, in_, op, axis=AX.X, e="dve", **kw):
        return self.op(e, lambda g: g.tensor_reduce(out.ap, in_.ap, axis, op, **kw), reads=[in_], writes=[out])

    def recip(self, out, in_, e="dve"):
        return self.op(e, lambda g: g.reciprocal(out.ap, in_.ap), reads=[in_], writes=[out])

    def finish(self):
        self.barrier()


SEQ = 2048
DM = 1024
NT = 16
DEPTH = 2
import os
NCHUNK = int(os.environ.get('NCHUNK', '16'))
S5_STOP = int(os.environ.get('S5_STOP', '99'))
GLA_STOP = int(os.environ.get('GLA_STOP', '0'))
S5_ORDER = int(os.environ.get('S5_ORDER', '0'))
GLA_VAR = int(os.environ.get('GLA_VAR', '0'))
PHASES = os.environ.get('PHASES', 'gla,s5,ml').split(',')
OFF = dict(gq=0, gk=256, gv=512, ga=1024, gg=1040, su=1552, mq=2064, mk=2320, mv=2576, mi=3088, mf=3092, mo=3096, gate=3608, end=6680)
DN_ALPHA = (2.0 * DEPTH) ** 0.25
LN_EPS = 1e-5

INPUT_SHAPES = {
    'x': (SEQ, DM), 'p': (2, SEQ, 256), 'w_in': (2, 1024, 6680), 'b_in': (2, 6680), 'gla_w_a2': (2, 16, 256),
    'gla_b_a2': (2, 256), 'gla_norm_g': (2, 512), 's5_a_re': (2, 32, 64), 's5_a_im': (2, 32, 64), 's5_log_dt': (2, 32),
    's5_b_re': (2, 32, 64, 16), 's5_b_im': (2, 32, 64, 16), 's5_c_re': (2, 32, 16, 64), 's5_c_im': (2, 32, 16, 64),
    's5_d': (2, 512), 's5_w_glu': (2, 512, 512), 's5_b_glu': (2, 512), 'ml_conv_w': (2, 4, 512), 'ml_conv_b': (2, 512),
    'ml_norm_g': (2, 512), 'w_up': (2, 3, 512, 1024), 'w_o': (2, 1024, 1024), 'ln1_g': (2, 1024), 'ln1_b': (2, 1024),
    'ffn_wg': (1, 1024, 2816), 'ffn_wu': (1, 1024, 2816), 'ffn_wd': (1, 2816, 1024), 'moe_router': (1, 1024, 8),
    'moe_router_b': (1, 8), 'moe_wg': (1, 8, 1024, 3584), 'moe_wu': (1, 8, 1024, 3584), 'moe_wd': (1, 8, 3584, 1024),
    'ple_w_gate': (2, 1024, 1024), 'ple_w_proj': (2, 256, 1024), 'ln2_g': (2, 1024), 'ln2_b': (2, 1024),
}


class Scope:
    def __init__(self, P):
        self.P = P

    def __enter__(self):
        self.es = ExitStack()
        self.es.__enter__()
        self.saved = self.P.es
        self.P.es = self.es
        self.P.push_scope()
        return self

    def __exit__(self, *a):
        self.P.barrier()
        self.P.pop_scope()
        self.P.es = self.saved
        return self.es.__exit__(*a)


def D(ap):
    return V(ap, [])


class K:
    pass


def setup_consts(P, k):
    k.ones = Tile(P, "ones", [128, 128], F32)
    k.ident = Tile(P, "ident", [128, 128], F32)
    k.identb = Tile(P, "identb", [128, 128], BF16)
    k.onesb = Tile(P, "onesb", [1, 512], BF16)
    k.mask01 = Tile(P, "mask01", [128, 128], F32)
    k.mask01b = Tile(P, "mask01b", [128, 128], BF16)
    k.triu_s = Tile(P, "triu_s", [128, 128], F32)
    k.tril_s = Tile(P, "tril_s", [128, 128], F32)
    P.memset(k.ones[:], 1.0)
    P.op("pool", lambda g: g.affine_select(k.ident[:].ap, k.ones[:, 0:128].ap, [[-1, 128]], ALU.is_equal, 0.0,
                                           base=0, channel_multiplier=1), reads=[k.ones[:]], writes=[k.ident[:]])
    P.copy(k.identb[:], k.ident[:], e="pool")
    P.memset(k.onesb[:], 1.0)
    P.op("pool", lambda g: g.affine_select(k.mask01[:].ap, k.ones[:, 0:128].ap, [[1, 128]], ALU.is_ge, 0.0,
                                           base=0, channel_multiplier=-1), reads=[k.ones[:]], writes=[k.mask01[:]])
    P.copy(k.mask01b[:], k.mask01[:], e="pool")
    P.ts(k.triu_s[:], k.mask01[:], -1.0 / 16, None, ALU.mult, e="pool")
    P.ts(k.tril_s[:], k.mask01[:], 1.0 / 16, -1.0 / 16, ALU.mult, ALU.add, e="pool")
    k.ps = [Tile(P, f"ps{i}", [128, 512], F32, space="psum") for i in range(8)]


def load_x(P, k, x_dram):
    for j in range(NT):
        P.dma("sp", k.xres.sub(j), D(x_dram[j * 128:(j + 1) * 128, :]), k.xres.sub(j))
        make_xT(P, k, j)


def make_xT(P, k, j, xtf=None, banks=(0, 1)):
    for half in range(2):
        pst = k.ps[banks[half]]
        for q in range(4):
            kt = half * 4 + q
            P.tr(pst[:, q * 128:(q + 1) * 128], k.xres.sub(j)[:, kt * 128:(kt + 1) * 128], k.ident[:])
        dst = k.xT.reg(j, (slice(None), slice(half * 4, half * 4 + 4), slice(j * 128, (j + 1) * 128)))
        src = pst[:].ap.rearrange("p (q t) -> p q t", q=4)
        srcv = V(src, pst.regs)
        P.copy(dst, srcv, e="act" if half == 0 else "dve")
        if xtf is not None:
            P.copy(xtf[:, half * 4:half * 4 + 4, :], srcv, e="dve" if half == 0 else "act")


def proj_fm(P, k, ps_out, W, wc, brow, bc, tsl, n):
    P.mm(ps_out, brow[0:1, bc], k.ones[0:1, 0:n], start=True, stop=False)
    for kt in range(8):
        P.mm(ps_out, W[:, kt, wc], k.xT[:, kt, tsl], start=False, stop=(kt == 7))


def proj_tm(P, k, ps_out, W, wc, brow, bc, tsl):
    P.mm(ps_out, k.ones[0:1, 0:128], brow[0:1, bc], start=True, stop=False)
    for kt in range(8):
        P.mm(ps_out, k.xT[:, kt, tsl], W[:, kt, wc], start=False, stop=(kt == 7))


def rstd_from_var(P, out, var, eps, tmp):
    P.ts(tmp, var, eps, None, ALU.add)
    P.act(tmp, tmp, AF.Ln)
    P.act(out, tmp, AF.Exp, scale=-0.5)


def gla_phase(P, k, L, w):
    ps = k.ps
    with Scope(P):
        WG = Tile(P, "WG", [128, 8, 1552], BF16)
        brow = Tile(P, "g_brow", [1, 1552], BF16)
        wa2 = Tile(P, "wa2", [16, 256], F32)
        ba2 = Tile(P, "ba2", [1, 256], F32)
        ng = Tile(P, "g_ng", [128, 512], F32)
        P.dma("pool", WG[:], D(w['w_in'][L, :, 0:1552].rearrange("(kt p) n -> p kt n", p=128)), WG[:])
        P.dma("pool", brow[:], D(w['b_in'][L:L + 1, 0:1552]), brow[:])
        P.dma("sp", wa2[:], D(w['gla_w_a2'][L]), wa2[:])
        P.dma("sp", ba2[:], D(w['gla_b_a2'][L:L + 1, :]), ba2[:])
        P.dma("sp", ng[:], D(w['gla_norm_g'][L].partition_broadcast(128)), ng[:])
        qk_raw = Tile(P, "g_qkraw", [128, 4, 512], BF16)
        alr_all = Tile(P, "g_alr", [16, 512], F32)
        sp = Tile(P, "g_sp", [128, 256], F32)
        eT = Tile(P, "g_eT", [128, 2, 128], F32)
        einvT = Tile(P, "g_einvT", [128, 2, 128], F32)
        erev = Tile(P, "g_erev", [128, 256], F32)
        dec = [Tile(P, f"g_dec{i}", [128, 2], F32) for i in range(2)]
        qdT = [Tile(P, f"g_qdT{i}", [128, 2, 128], BF16) for i in range(2)]
        kiT = [Tile(P, f"g_kiT{i}", [128, 2, 128], BF16) for i in range(2)]
        ktail = [Tile(P, f"g_ktail{i}", [128, 256], BF16) for i in range(2)]
        vbf = [Tile(P, f"g_vbf{i}", [128, 512], BF16) for i in range(2)]
        sg = [Tile(P, f"g_sg{i}", [128, 512], BF16) for i in range(2)]
        sgf = Tile(P, "g_sgf", [128, 512], F32)
        attT = Tile(P, "g_attT", [128, 4, 128], BF16)
        S32 = Tile(P, "g_S32", [128, 2, 128], F32)
        Sbf = Tile(P, "g_Sbf", [128, 2, 128], BF16)
        st = Tile(P, "g_st", [128, 4, 6], F32)
        mv = Tile(P, "g_mv", [128, 4, 2], F32)
        rstd = Tile(P, "g_rstd", [128, 4], F32)
        tmp4 = Tile(P, "g_tmp4", [128, 4], F32)
        yn = Tile(P, "g_yn", [128, 512], F32)
        ybf = Tile(P, "g_ybf", [128, 512], BF16)
        P.memset(S32[:], 0.0)
        P.memset(Sbf[:], 0.0)

        def bias_fm(ps_out, bc, n):
            P.mm(ps_out, brow[0:1, bc], k.onesb[0:1, 0:n], start=True, stop=False)

        def bias_tm(ps_out, bc):
            P.mm(ps_out, k.onesb[0:1, 0:128], brow[0:1, bc], start=True, stop=False)

        def stage_a(c):
            par = c % 2
            tsl = slice(c * 128, (c + 1) * 128)
            off = (c % 4) * 128
            osl = slice(off, off + 128)
            if c % 4 == 0:
                t4 = slice(c * 128, c * 128 + 512)
                for i in range(4):
                    cols = slice(i * 128, (i + 1) * 128)
                    bias_fm(ps[0][:], cols, 512)
                    for kt in range(8):
                        P.mm(ps[0][:], WG[:, kt, cols], k.xT[:, kt, t4], start=False, stop=(kt == 7))
                    P.copy(qk_raw[:, i, :], ps[0][:], e="act")
                ac = slice(OFF['ga'], OFF['ga'] + 16)
                bias_fm(ps[0][0:16, :], ac, 512)
                for kt in range(8):
                    P.mm(ps[0][0:16, :], WG[:, kt, ac], k.xT[:, kt, t4], start=False, stop=(kt == 7))
                P.copy(alr_all[:], ps[0][0:16, :], e="act")
            P.mm(ps[1][:, 0:256], k.ones[0:1, 0:128], ba2[:], start=True, stop=False)
            P.mm(ps[1][:, 0:256], alr_all[:, osl], wa2[:], start=False, stop=True)
            P.act(sp[:], ps[1][:, 0:256], AF.Exp, scale=-1.0)
            P.act(sp[:], sp[:], AF.Ln, bias=1.0)
            P.mm(ps[1][:, 256:512], k.tril_s[:], sp[:], start=True, stop=True)
            for i in range(2):
                P.mm(ps[2][:, i * 128:(i + 1) * 128], sp[:, i * 128:(i + 1) * 128], k.triu_s[:], start=True, stop=True)
            cumT = v3(ps[2][:, 0:256], "p (i t) -> p i t", i=2)
            P.act(eT[:], cumT, AF.Exp)
            P.act(einvT[:], cumT, AF.Exp, scale=-1.0)
            P.act(erev[:], ps[1][:, 256:512], AF.Exp)
            P.copy(dec[par][:], eT[:, :, 127], e="act")
            P.stt(qdT[par][:], qk_raw[:, 0:2, osl], 0.125, eT[:], ALU.mult, ALU.mult)
            P.tt(kiT[par][:], qk_raw[:, 2:4, osl], einvT[:], ALU.mult, e="pool")
            kc = slice(OFF['gk'], OFF['gk'] + 256)
            bias_tm(ps[2][:, 256:512], kc)
            for kt in range(8):
                P.mm(ps[2][:, 256:512], k.xT[:, kt, tsl], WG[:, kt, kc], start=False, stop=(kt == 7))
            vc = slice(OFF['gv'], OFF['gv'] + 512)
            bias_tm(ps[3][:], vc)
            for kt in range(8):
                P.mm(ps[3][:], k.xT[:, kt, tsl], WG[:, kt, vc], start=False, stop=(kt == 7))
            gc = slice(OFF['gg'], OFF['gg'] + 512)
            bias_tm(ps[4][:], gc)
            for kt in range(8):
                P.mm(ps[4][:], k.xT[:, kt, tsl], WG[:, kt, gc], start=False, stop=(kt == 7))
            P.tt(ktail[par][:], ps[2][:, 256:512], erev[:], ALU.mult)
            P.copy(vbf[par][:], ps[3][:], e="act")
            P.act(sgf[:], ps[4][:], AF.Silu)
            P.tt(sg[par][:], sgf[:], ng[:], ALU.mult, e="pool")

        def stage_b(c):
            par = c % 2
            tsl = slice(c * 128, (c + 1) * 128)
            abank = {0: (ps[5], 0), 2: (ps[5], 128), 1: (ps[6], 0), 3: (ps[6], 128)}
            for h in (0, 2, 1, 3):
                hp, ho = h // 2, (h % 2) * 64
                bk, co = abank[h]
                P.mm(bk[:, co:co + 128], kiT[par][ho:ho + 64, hp, :], qdT[par][ho:ho + 64, hp, :], start=True, stop=True)
            for h in range(4):
                bk, co = abank[h]
                P.tt(attT[:, h, :], bk[:, co:co + 128], k.mask01[:], ALU.mult)
            for h in range(4):
                hp, ho = h // 2, (h % 2) * 64
                P.mm(ps[7][:, h * 128:(h + 1) * 128], attT[:, h, :], vbf[par][:, h * 128:(h + 1) * 128], start=True, stop=False)
                P.mm(ps[7][:, h * 128:(h + 1) * 128], qdT[par][ho:ho + 64, hp, :], Sbf[ho:ho + 64, hp, :], start=False, stop=True)
            sbank = [ps[5], ps[6]]
            for hp in range(2):
                P.mm(sbank[hp][:, 256:512], ktail[par][:, hp * 128:(hp + 1) * 128], vbf[par][:, hp * 256:(hp + 1) * 256], start=True, stop=True)
            for hp in range(2):
                for hh in range(2):
                    ho = hh * 64
                    P.stt(S32[ho:ho + 64, hp, :], S32[ho:ho + 64, hp, :], dec[par][ho:ho + 64, hp:hp + 1],
                          sbank[hp][ho:ho + 64, 256 + hh * 128:256 + (hh + 1) * 128], ALU.mult, ALU.add)
            P.copy(Sbf[:], S32[:], e="pool")
            for h in range(4):
                P.op("dve", lambda g, h=h: g.bn_stats(st[:, h, :].ap, ps[7][:, h * 128:(h + 1) * 128].ap),
                     reads=[ps[7][:]], writes=[st[:]])
                P.op("dve", lambda g, h=h: g.bn_aggr(mv[:, h, :].ap, st[:, h, :].ap), reads=[st[:]], writes=[mv[:]])
            rstd_from_var(P, rstd[:], mv[:, :, 1], LN_EPS, tmp4[:])
            for h in range(4):
                P.ts(yn[:, h * 128:(h + 1) * 128], ps[7][:, h * 128:(h + 1) * 128], mv[:, h, 0:1], rstd[:, h:h + 1],
                     ALU.subtract, ALU.mult)
            P.tt(ybf[:], yn[:], sg[par][:], ALU.mult, e="pool")
            psb = V(ps[5][:].ap.bitcast(BF16), ps[5].regs)
            for q in range(4):
                P.tr(psb[:, q * 128:(q + 1) * 128], ybf[:, q * 128:(q + 1) * 128], k.identb[:])
            P.copy(k.yT[0][:, :, tsl],
                   V(psb[:, 0:512].ap.rearrange("p (q t) -> p q t", q=4), ps[5].regs), e="act")

        stage_a(0)
        for c in range(NCHUNK):
            if c + 1 < NCHUNK:
                stage_a(c + 1)
            stage_b(c)


def v3(v, pat, **kw):
    return V(v.ap.rearrange(pat, **kw), v.regs)


def mlstm_phase(P, k, L, w):
    ps = k.ps
    o0 = OFF['mq']
    with Scope(P):
        WM = Tile(P, "WM", [128, 8, 1544], BF16)
        brow = Tile(P, "m_brow", [1, 1544], BF16)
        gb8 = Tile(P, "m_gb8", [1, 8], F32)
        WIF = Tile(P, "WIF", [128, 8, 512], BF16)
        browIF = Tile(P, "m_browIF", [1, 512], F32)
        cw = Tile(P, "m_cw", [128, 4, 4], F32)
        cb = Tile(P, "m_cb", [128, 4], F32)
        ngm = Tile(P, "m_ng", [128, 512], F32)
        P.dma("sp", gb8[:], D(w['b_in'][L:L + 1, o0 + 1024:o0 + 1032]), gb8[:])
        P.dma("pool", WM[:], D(w['w_in'][L, :, o0:o0 + 1544].rearrange("(kt p) n -> p kt n", p=128)), WM[:])
        P.dma("pool", brow[:], D(w['b_in'][L:L + 1, o0:o0 + 1544]), brow[:])
        for tap in range(4):
            P.dma("sp", cw[:, tap, :], D(w['ml_conv_w'][L, tap].rearrange("(j p) -> p j", p=128)), cw[:], allow_slow_non_contiguous=True)
        P.dma("sp", cb[:], D(w['ml_conv_b'][L].rearrange("(j p) -> p j", p=128)), cb[:], allow_slow_non_contiguous=True)
        P.dma("sp", ngm[:], D(w['ml_norm_g'][L].partition_broadcast(128)), ngm[:])
        for gi in range(2):
            for h in range(4):
                col = 1024 + gi * 4 + h
                dst = slice(gi * 256 + h * 64, gi * 256 + (h + 1) * 64)
                P.copy(WIF[:, :, dst], WM[:, :, col:col + 1].bcast([128, 8, 64]), e="dve")
                P.copy(browIF[0:1, dst], gb8[0:1, gi * 4 + h:gi * 4 + h + 1].bcast([1, 64]), e="dve")
        bif_hi = Tile(P, "m_bifhi", [1, 512], BF16)
        bif_lo = Tile(P, "m_biflo", [1, 512], BF16)
        P.copy(bif_hi[:], browIF[:], e="dve")
        P.tt(browIF[:], browIF[:], bif_hi[:], ALU.subtract)
        P.copy(bif_lo[:], browIF[:], e="dve")
        raw = Tile(P, "m_raw4", [128, 4, 515], BF16)
        gif = Tile(P, "m_gif", [128, 4, 512], F32)
        acc = Tile(P, "m_acc", [128, 4, 128], F32)
        kf = Tile(P, "m_kf", [128, 2, 128], F32)
        spf = Tile(P, "m_spf", [128, 2, 128], F32)
        halo = Tile(P, "m_halo", [128, 4, 3], BF16)
        ncum = Tile(P, "m_ncum", [128, 2, 128], F32)
        aT = Tile(P, "m_aT", [128, 2, 128], F32)
        ksc = spf
        clT = Tile(P, "m_clT", [128, 2, 128], F32)
        gfacf = Tile(P, "m_gfacf", [128, 512], BF16)
        mst = Tile(P, "m_mst", [128, 2], F32)
        sm = Tile(P, "m_sm", [128, 16], F32)
        qfT = [Tile(P, f"m_qfT{i}", [128, 2, 128], BF16) for i in range(2)]
        kpT = [Tile(P, f"m_kpT{i}", [128, 2, 128], BF16) for i in range(2)]
        kptok = [Tile(P, f"m_kptok{i}", [128, 256], BF16) for i in range(2)]
        clampv = [Tile(P, f"m_clampv{i}", [128, 4], F32) for i in range(2)]
        vaug = [Tile(P, f"m_vaug{i}", [128, 4, 129], BF16) for i in range(2)]
        gfac = [Tile(P, f"m_gfac{i}", [128, 512], BF16) for i in range(2)]
        wdec = [Tile(P, f"m_wdec{i}", [128, 2], F32) for i in range(2)]
        C32 = Tile(P, "m_C32", [128, 2, 129], F32)
        Cbf = Tile(P, "m_Cbf", [128, 2, 129], BF16)
        attTb = Tile(P, "m_attTb", [128, 4, 128], BF16)
        st = Tile(P, "m_st", [128, 4, 6], F32)
        mv = Tile(P, "m_mv", [128, 4, 2], F32)
        sm2 = Tile(P, "m_sm2", [128, 6, 4], F32)
        yn = Tile(P, "m_yn", [128, 512], F32)
        ybf = Tile(P, "m_ybf", [128, 512], BF16)
        P.memset(raw[:], 0.0)
        P.memset(C32[:], 0.0)
        P.memset(mst[:], 0.0)
        for i in range(2):
            P.memset(vaug[i][:], 1.0)

        def stage_a(c):
            par = c % 2
            tsl = slice(c * 128, (c + 1) * 128)
            off = (c % 4) * 128
            osl = slice(off, off + 128)
            if c % 4 == 0:
                t4 = slice(c * 128, c * 128 + 512)
                P.copy(halo[:], raw[:, :, 512:515], e="pool")
                for i in range(8):
                    cols = slice((i % 4) * 128, (i % 4 + 1) * 128)
                    bank = ps[i % 2]
                    if i < 4:
                        P.mm(bank[:], brow[0:1, cols], k.onesb[0:1, 0:512], start=True, stop=False)
                        Wsrc = WM
                    else:
                        P.mm(bank[:], bif_hi[0:1, cols], k.onesb[0:1, 0:512], start=True, stop=False)
                        P.mm(bank[:], bif_lo[0:1, cols], k.onesb[0:1, 0:512], start=False, stop=False)
                        Wsrc = WIF
                    for kt in range(8):
                        P.mm(bank[:], Wsrc[:, kt, cols], k.xT[:, kt, t4], start=False, stop=(kt == 7))
                    if i < 4:
                        P.copy(raw[:, i, 3:515], bank[:], e="act")
                    else:
                        P.copy(gif[:, i - 4, :], bank[:], e="act")
                P.copy(raw[:, :, 0:3], halo[:], e="pool")
            for i in range(4):
                P.ts(acc[:, i, :], raw[:, i, off:off + 128], cw[:, 0, i:i + 1], cb[:, i:i + 1], ALU.mult, ALU.add)
                for tap in range(1, 4):
                    P.stt(acc[:, i, :], raw[:, i, off + tap:off + tap + 128], cw[:, tap, i:i + 1], acc[:, i, :], ALU.mult, ALU.add)
            P.act(qfT[par][:], acc[:, 0:2, :], AF.Silu)
            P.act(kf[:], acc[:, 2:4, :], AF.Silu)
            gi_ps = gif[:, 0:2, osl]
            gf_ps = gif[:, 2:4, osl]
            P.act(spf[:], gf_ps, AF.Exp, scale=-1.0)
            P.act(spf[:], spf[:], AF.Ln, bias=1.0)
            for i in range(2):
                P.op("dve", lambda g, i=i: g.tensor_tensor_scan(ncum[:, i, :].ap, k.ones[:, 0:128].ap, spf[:, i, :].ap, 0.0,
                                                                 ALU.mult, ALU.add),
                     reads=[k.ones[:], spf[:]], writes=[ncum[:]])
            P.tt(aT[:], gi_ps, ncum[:], ALU.add)
            P.reduce(sm[:, 0:2], aT[:], ALU.max)
            P.tt(sm[:, 2:4], sm[:, 0:2], mst[:], ALU.max)
            P.ts(sm[:, 4:6], sm[:, 2:4], -1.0, None, ALU.mult)
            P.tt(sm[:, 8:10], mst[:], sm[:, 2:4], ALU.subtract)
            P.act(wdec[par][:], sm[:, 8:10], AF.Exp)
            for i in range(2):
                P.act(ksc[:, i, :], aT[:, i, :], AF.Exp, bias=sm[:, 4 + i:5 + i])
                P.act(clT[:, i, :], ncum[:, i, :], AF.Exp, bias=sm[:, 4 + i:5 + i])
            P.stt(kpT[par][:], kf[:], 0.125, ksc[:], ALU.mult, ALU.mult)
            P.tt(mst[:], sm[:, 2:4], ncum[:, :, 127], ALU.subtract)
            for i in range(2):
                P.tr(ps[1][:, i * 128:(i + 1) * 128], clT[:, i, :], k.ident[:])
            psb1 = V(ps[1][:].ap.bitcast(BF16), ps[1].regs)
            for i in range(2):
                P.tr(psb1[:, 512 + i * 128:512 + (i + 1) * 128], kpT[par][:, i, :], k.identb[:])
            P.copy(clampv[par][:], v3(ps[1][:, 0:256], "p (h r) -> p h r", r=64)[:, :, 0], e="act")
            P.copy(kptok[par][:], psb1[:, 512:768], e="act")
            vc = slice(512, 1024)
            P.mm(ps[2][:], k.onesb[0:1, 0:128], brow[0:1, vc], start=True, stop=False)
            for kt in range(8):
                P.mm(ps[2][:], k.xT[:, kt, tsl], WM[:, kt, vc], start=False, stop=(kt == 7))
            oc = slice(1032, 1544)
            P.mm(ps[3][:], k.onesb[0:1, 0:128], brow[0:1, oc], start=True, stop=False)
            for kt in range(8):
                P.mm(ps[3][:], k.xT[:, kt, tsl], WM[:, kt, oc], start=False, stop=(kt == 7))
            P.copy(vaug[par][:, :, 0:128], v3(ps[2][:], "p (h e) -> p h e", h=4), e="act")
            P.act(gfacf[:], ps[3][:], AF.Sigmoid)
            P.tt(gfac[par][:], gfacf[:], ngm[:], ALU.mult, e="pool")

        def stage_b(c):
            par = c % 2
            tsl = slice(c * 128, (c + 1) * 128)
            for i in range(2):
                P.ts(C32[:, i, :], C32[:, i, :], wdec[par][:, i:i + 1], None, ALU.mult)
            P.copy(Cbf[:], C32[:], e="pool")
            abank = {0: (ps[4], 0), 2: (ps[4], 128), 1: (ps[5], 0), 3: (ps[5], 128)}
            for h in (0, 2, 1, 3):
                hp, ho = h // 2, (h % 2) * 64
                bk, co = abank[h]
                P.mm(bk[:, co:co + 128], kpT[par][ho:ho + 64, hp, :], qfT[par][ho:ho + 64, hp, :], start=True, stop=True)
            for h in range(4):
                bk, co = abank[h]
                P.tt(attTb[:, h, :], bk[:, co:co + 128], k.mask01[:], ALU.mult)
            oa = []
            for h in range(4):
                hp, ho = h // 2, (h % 2) * 64
                bank = ps[6] if h < 2 else ps[7]
                o_ = bank[:, (h % 2) * 129:(h % 2) * 129 + 129]
                oa.append(o_)
                P.mm(o_, attTb[:, h, :], vaug[par][:, h, :], start=True, stop=False)
                P.mm(o_, qfT[par][ho:ho + 64, hp, :], Cbf[ho:ho + 64, hp, :], start=False, stop=True)
            for hp in range(2):
                pss = ps[4 + hp]
                P.mm(pss[:, 0:258], kptok[par][:, hp * 128:(hp + 1) * 128],
                     v3(vaug[par][:, 2 * hp:2 * hp + 2, :], "p h e -> p (h e)"), start=True, stop=True)
            for hp in range(2):
                pss = ps[4 + hp]
                for hh in range(2):
                    ho = hh * 64
                    P.tt(C32[ho:ho + 64, hp, :], C32[ho:ho + 64, hp, :], pss[ho:ho + 64, hh * 129:(hh + 1) * 129], ALU.add)
            for h in range(4):
                P.act(sm2[:, 0, h:h + 1], oa[h][:, 128:129], AF.Abs)
            for h in range(4):
                P.op("dve", lambda g, h=h: g.bn_stats(st[:, h, :].ap, oa[h][:, 0:128].ap), reads=[oa[h]], writes=[st[:]])
                P.op("dve", lambda g, h=h: g.bn_aggr(mv[:, h, :].ap, st[:, h, :].ap), reads=[st[:]], writes=[mv[:]])
            P.tt(sm2[:, 0, :], sm2[:, 0, :], clampv[par][:], ALU.max)
            P.recip(sm2[:, 1, :], sm2[:, 0, :])
            P.tt(sm2[:, 2, :], sm2[:, 1, :], sm2[:, 1, :], ALU.mult)
            P.tt(sm2[:, 3, :], mv[:, :, 1], sm2[:, 2, :], ALU.mult)
            rstd_from_var(P, sm2[:, 4, :], sm2[:, 3, :], LN_EPS, sm2[:, 4, :])
            P.tt(sm2[:, 5, :], sm2[:, 4, :], sm2[:, 1, :], ALU.mult)
            for h in range(4):
                P.ts(yn[:, h * 128:(h + 1) * 128], oa[h][:, 0:128], mv[:, h, 0:1], sm2[:, 5, h:h + 1], ALU.subtract, ALU.mult)
            P.tt(ybf[:], yn[:], gfac[par][:], ALU.mult, e="pool")
            psb = V(ps[4][:].ap.bitcast(BF16), ps[4].regs)
            for q in range(4):
                P.tr(psb[:, q * 128:(q + 1) * 128], ybf[:, q * 128:(q + 1) * 128], k.identb[:])
            P.copy(k.yT[2][:, :, tsl],
                   V(psb[:, 0:512].ap.rearrange("p (q t) -> p q t", q=4), ps[4].regs), e="act")

        stage_a(0)
        for c in range(NCHUNK):
            if c + 1 < NCHUNK:
                stage_a(c + 1)
            stage_b(c)


import math
TWO_PI = 2.0 * math.pi


class StopBuild(Exception):
    pass


def stop_at(n):
    if S5_STOP <= n:
        MUTE[0] = True


MUTE = [False]


def s5_phase(P, k, L, w):
    ps = k.ps
    T16 = [128, 16]
    with Scope(P):
        ETre = Tile(P, "s_ETre", [128, 16, 128], BF16)
        ETim = Tile(P, "s_ETim", [128, 16, 128], BF16)
        Epre = Tile(P, "s_Epre", [128, 16, 128], BF16)
        Epim = Tile(P, "s_Epim", [128, 16, 128], BF16)
        lam_re = Tile(P, "s_lamre", T16, F32)
        lam_im = Tile(P, "s_lamim", T16, F32)
        with Scope(P):
            are = Tile(P, "s_are", T16, F32)
            aim = Tile(P, "s_aim", T16, F32)
            ldt = Tile(P, "s_ldt", T16, F32)
            P.dma("sp", are[:], D(w['s5_a_re'][L].rearrange("(j gl) p -> (gl p) j", gl=2)), are[:], allow_slow_non_contiguous=True)
            P.dma("sp", aim[:], D(w['s5_a_im'][L].rearrange("(j gl) p -> (gl p) j", gl=2)), aim[:], allow_slow_non_contiguous=True)
            ldt_src = w['s5_log_dt'][L].rearrange("(j gl) -> gl j", gl=2)
            for gl in range(2):
                P.dma("sp", ldt[gl * 64:(gl + 1) * 64, :], D(ldt_src[gl].partition_broadcast(64)), ldt[:], allow_slow_non_contiguous=True)
            sm = {n: Tile(P, "sq_" + n, T16, F32) for n in
                  ["dt", "re", "im", "mag", "magi", "t0", "t1", "sinv", "cosv", "li_re", "li_im", "cf_re", "cf_im", "pw_re", "pw_im", "q_re", "q_im"]}
            ni = Tile(P, "s_ni", T16, I32)
            e = "dve"
            P.act(sm["dt"][:], ldt[:], AF.Exp)
            P.tt(sm["re"][:], are[:], sm["dt"][:], ALU.mult)
            P.tt(sm["im"][:], aim[:], sm["dt"][:], ALU.mult)
            P.act(sm["mag"][:], sm["re"][:], AF.Exp)
            P.act(sm["magi"][:], sm["re"][:], AF.Exp, scale=-1.0)

            def wrap(t):
                P.ts(sm["t1"][:], t, math.pi, TWO_PI, ALU.is_gt, ALU.mult)
                P.tt(t, t, sm["t1"][:], ALU.subtract)
                P.ts(sm["t1"][:], t, -math.pi, TWO_PI, ALU.is_lt, ALU.mult)
                P.tt(t, t, sm["t1"][:], ALU.add)

            P.ts(sm["t0"][:], sm["im"][:], 1.0 / TWO_PI, None, ALU.mult)
            P.copy(ni[:], sm["t0"][:])
            P.copy(sm["t0"][:], ni[:])
            P.stt(sm["im"][:], sm["t0"][:], -TWO_PI, sm["im"][:], ALU.mult, ALU.add)
            wrap(sm["im"][:])
            P.act(sm["sinv"][:], sm["im"][:], AF.Sin)
            P.ts(sm["t0"][:], sm["im"][:], math.pi / 2, None, ALU.add)
            wrap(sm["t0"][:])
            P.act(sm["cosv"][:], sm["t0"][:], AF.Sin)
            P.tt(lam_re[:], sm["mag"][:], sm["cosv"][:], ALU.mult)
            P.tt(lam_im[:], sm["mag"][:], sm["sinv"][:], ALU.mult)
            P.tt(sm["li_re"][:], sm["magi"][:], sm["cosv"][:], ALU.mult)
            P.stt(sm["li_im"][:], sm["magi"][:], -1.0, sm["sinv"][:], ALU.mult, ALU.mult)
            P.ts(sm["q_re"][:], lam_re[:], -1.0, None, ALU.add)
            P.tt(sm["t0"][:], are[:], are[:], ALU.mult)
            P.tt(sm["t1"][:], aim[:], aim[:], ALU.mult)
            P.tt(sm["t0"][:], sm["t0"][:], sm["t1"][:], ALU.add)
            P.recip(sm["q_im"][:], sm["t0"][:])
            P.tt(sm["t0"][:], sm["q_re"][:], are[:], ALU.mult)
            P.tt(sm["t1"][:], lam_im[:], aim[:], ALU.mult)
            P.tt(sm["t0"][:], sm["t0"][:], sm["t1"][:], ALU.add)
            P.tt(sm["cf_re"][:], sm["t0"][:], sm["q_im"][:], ALU.mult)
            P.tt(sm["t0"][:], lam_im[:], are[:], ALU.mult)
            P.tt(sm["t1"][:], sm["q_re"][:], aim[:], ALU.mult)
            P.tt(sm["t0"][:], sm["t0"][:], sm["t1"][:], ALU.subtract)
            P.tt(sm["cf_im"][:], sm["t0"][:], sm["q_im"][:], ALU.mult)
            if S5_STOP <= 1:
                return
            Tre = Tile(P, "s_Tre", [128, 16, 128], F32)
            Tim = Tile(P, "s_Tim", [128, 16, 128], F32)
            tA = Tile(P, "s_tA", [128, 16, 64], F32)
            tB = Tile(P, "s_tB", [128, 16, 64], F32)

            def build_table(bre, bim):
                P.memset(Tre[:, :, 0:1], 1.0)
                P.memset(Tim[:, :, 0:1], 0.0)
                P.copy(Tre[:, :, 1], bre)
                P.copy(Tim[:, :, 1], bim)
                P.copy(sm["pw_re"][:], bre)
                P.copy(sm["pw_im"][:], bim)
                for kk in range(1, 7):
                    n = 1 << kk
                    P.tt(sm["t0"][:], sm["pw_re"][:], sm["pw_re"][:], ALU.mult)
                    P.tt(sm["t1"][:], sm["pw_im"][:], sm["pw_im"][:], ALU.mult)
                    P.tt(sm["q_re"][:], sm["pw_re"][:], sm["pw_im"][:], ALU.mult)
                    P.tt(sm["pw_re"][:], sm["t0"][:], sm["t1"][:], ALU.subtract)
                    P.ts(sm["pw_im"][:], sm["q_re"][:], 2.0, None, ALU.mult)
                    pr = v3(sm["pw_re"][:], "p (j o) -> p j o", o=1).bcast([128, 16, n])
                    pi_ = v3(sm["pw_im"][:], "p (j o) -> p j o", o=1).bcast([128, 16, n])
                    P.tt(tA[:, :, 0:n], Tre[:, :, 0:n], pr, ALU.mult)
                    P.tt(tB[:, :, 0:n], Tim[:, :, 0:n], pi_, ALU.mult, e="pool")
                    P.tt(Tre[:, :, n:2 * n], tA[:, :, 0:n], tB[:, :, 0:n], ALU.subtract)
                    P.tt(tA[:, :, 0:n], Tre[:, :, 0:n], pi_, ALU.mult)
                    P.tt(tB[:, :, 0:n], Tim[:, :, 0:n], pr, ALU.mult, e="pool")
                    P.tt(Tim[:, :, n:2 * n], tA[:, :, 0:n], tB[:, :, 0:n], ALU.add)

            build_table(lam_re[:], lam_im[:])
            P.copy(ETre[:], Tre[:], e="act")
            P.copy(ETim[:], Tim[:], e="act")
            build_table(sm["li_re"][:], sm["li_im"][:])
            cr = v3(sm["cf_re"][:], "p (j o) -> p j o", o=1)
            ci = v3(sm["cf_im"][:], "p (j o) -> p j o", o=1)
            for hh in range(2):
                sl = slice(hh * 64, (hh + 1) * 64)
                crb, cib = cr.bcast([128, 16, 64]), ci.bcast([128, 16, 64])
                P.tt(tA[:], Tre[:, :, sl], crb, ALU.mult)
                P.tt(tB[:], Tim[:, :, sl], cib, ALU.mult, e="pool")
                P.tt(tA[:], tA[:], tB[:], ALU.subtract)
                P.tt(tB[:], Tre[:, :, sl], cib, ALU.mult, e="pool")
                P.tt(Tim[:, :, sl], Tim[:, :, sl], crb, ALU.mult)
                P.tt(Tim[:, :, sl], Tim[:, :, sl], tB[:], ALU.add)
                P.copy(Tre[:, :, sl], tA[:])
            for (src, dst) in ((Tre, Epre), (Tim, Epim)):
                for g4 in range(4):
                    bank = ps[g4 % 2]
                    for jj in range(4):
                        P.tr(bank[:, jj * 128:(jj + 1) * 128], src[:, g4 * 4 + jj, :], k.ident[:])
                    P.copy(dst[:, g4 * 4:g4 * 4 + 4, :], v3(bank[:], "p (j q) -> p j q", j=4), e="act" if g4 % 2 == 0 else "dve")
        if S5_STOP <= 2:
            return
        BTre = Tile(P, "s_BTre", [128, 16, 128], BF16)
        BTim = Tile(P, "s_BTim", [128, 16, 128], BF16)
        CTre = Tile(P, "s_CTre", [128, 16, 128], BF16)
        CTimn = Tile(P, "s_CTimn", [128, 16, 128], BF16)
        Wu = Tile(P, "s_Wu", [128, 8, 512], BF16)
        Wglu = Tile(P, "s_Wglu", [128, 4, 512], BF16)
        browu = Tile(P, "s_browu", [1, 512], F32)
        bglu = Tile(P, "s_bglu", [128, 4], F32)
        dvec = Tile(P, "s_dvec", [128, 4], F32)
        o0 = OFF['su']
        P.dma("pool", Wu[:], D(w['w_in'][L, :, o0:o0 + 512].rearrange("(kt p) n -> p kt n", p=128)), Wu[:])
        P.dma("pool", Wglu[:], D(w['s5_w_glu'][L].rearrange("(kt p) n -> p kt n", p=128)), Wglu[:])
        P.dma("sp", browu[:], D(w['b_in'][L:L + 1, o0:o0 + 512]), browu[:])
        P.dma("sp", bglu[:], D(w['s5_b_glu'][L].rearrange("(j p) -> p j", p=128)), bglu[:], allow_slow_non_contiguous=True)
        P.dma("sp", dvec[:], D(w['s5_d'][L].rearrange("(j p) -> p j", p=128)), dvec[:], allow_slow_non_contiguous=True)
        with Scope(P):
            Bnat = Tile(P, "s_Bnat", [128, 16, 16], F32)
            Bpad = Tile(P, "s_Bpad", [128, 16, 128], F32)
            Cnat = Tile(P, "s_Cnat", [128, 4, 128], F32)
            Cfull = Tile(P, "s_Cfull", [128, 4, 128], F32)
            P.memset(Bpad[:], 0.0)
            P.memset(CTre[:], 0.0)
            P.memset(CTimn[:], 0.0)
            for (nm, BT) in (("s5_b_re", BTre), ("s5_b_im", BTim)):
                P.dma("sp", Bnat[:], D(w[nm][L].rearrange("g p n -> (g p) n").rearrange("(j q) n -> q j n", q=128)), Bnat[:])
                Bp4 = v3(Bpad[:], "q (kt jj) c -> q kt jj c", jj=4)
                Bn4 = v3(Bnat[:], "q (kt jj) n -> q kt jj n", jj=4)
                for jj in range(4):
                    for half in range(2):
                        c0 = (2 * jj + half) * 16
                        P.copy(Bp4[half * 64:(half + 1) * 64, :, jj, c0:c0 + 16], Bn4[half * 64:(half + 1) * 64, :, jj, :])
                for g4 in range(4):
                    bank = ps[g4 % 2]
                    for jj in range(4):
                        P.tr(bank[:, jj * 128:(jj + 1) * 128], Bpad[:, g4 * 4 + jj, :], k.ident[:])
                    P.copy(BT[:, g4 * 4:g4 * 4 + 4, :], v3(bank[:], "p (j q) -> p j q", j=4), e="act" if g4 % 2 == 0 else "dve")
            for (nm, CT, sgn) in (("s5_c_re", CTre, 1.0), ("s5_c_im", CTimn, -1.0)):
                csrc = w[nm][L].rearrange("(kt g8) n p -> (g8 n) kt p", g8=8)
                P.dma("sp", Cnat[:, :, 0:64], D(csrc), Cnat[:])
                P.dma("sp", Cnat[:, :, 64:128], D(csrc), Cnat[:])
                for kt in range(4):
                    P.tr(ps[2][:, kt * 128:(kt + 1) * 128], Cnat[:, kt, :], k.ident[:])
                P.copy(Cfull[:], v3(ps[2][:], "p (kt c) -> p kt c", kt=4), e="act")
                C4 = v3(CT[:], "q (kt jj) c -> q kt jj c", jj=4)
                for jj in range(4):
                    for half in range(2):
                        c0 = (2 * jj + half) * 16
                        P.ts(C4[half * 64:(half + 1) * 64, :, jj, c0:c0 + 16], Cfull[half * 64:(half + 1) * 64, :, c0:c0 + 16],
                             sgn, None, ALU.mult)
        if S5_STOP <= 3:
            return
        uT32s = [Tile(P, f"s_uT32{i}", [128, 4, 128], F32) for i in range(2)]
        uTbs = [Tile(P, f"s_uTb{i}", [128, 4, 128], BF16) for i in range(2)]
        bure = Tile(P, "s_bure", [128, 4, 128], F32)
        buim = Tile(P, "s_buim", [128, 4, 128], F32)
        t1 = Tile(P, "s_t1", [128, 4, 128], F32)
        t2 = Tile(P, "s_t2", [128, 4, 128], F32)
        t3 = Tile(P, "s_t3", [128, 4, 128], F32)
        t4 = Tile(P, "s_t4", [128, 4, 128], F32)
        Wre = Tile(P, "s_Wre", [128, 4, 128], BF16)
        Wim = Tile(P, "s_Wim", [128, 4, 128], BF16)
        Zre = Tile(P, "s_Zre", [128, 4, 128], F32)
        Zim = Tile(P, "s_Zim", [128, 4, 128], F32)
        Xre = Tile(P, "s_Xre", [128, 4, 128], BF16)
        Xim = Tile(P, "s_Xim", [128, 4, 128], BF16)
        xe_re = Tile(P, "s_xere", T16, F32)
        xe_im = Tile(P, "s_xeim", T16, F32)
        c_re = Tile(P, "s_cre", T16, F32)
        c_im = Tile(P, "s_cim", T16, F32)
        s1 = Tile(P, "s_s1", T16, F32)
        s2 = Tile(P, "s_s2", T16, F32)
        ysk = Tile(P, "s_ysk", [128, 4, 128], F32)
        ygb = Tile(P, "s_ygb", [128, 4, 128], BF16)
        P.memset(c_re[:], 0.0)
        P.memset(c_im[:], 0.0)
        zbank = [(ps[3], ps[4]), (ps[5], ps[6])]

        def front1(u):
            c, g = divmod(u, 4)
            tsl = slice(c * 128, (c + 1) * 128)
            uT32, uTb = uT32s[c % 2], uTbs[c % 2]
            if g == 0:
                for i in range(4):
                    cols = slice(i * 128, (i + 1) * 128)
                    proj_fm(P, k, ps[0][:, i * 128:(i + 1) * 128], Wu, cols, browu, cols, tsl, 128)
                u_ps = v3(ps[0][:], "p (i t) -> p i t", i=4)
                P.copy(uT32[:], u_ps, e="act")
                P.copy(uTb[:], u_ps, e="act")
            js = slice(4 * g, 4 * g + 4)
            zr, zi = zbank[u % 2]
            P.mm(ps[1][:], uTb[:, g, :], v3(BTre[:, js, :], "p j q -> p (j q)"), start=True, stop=True)
            P.mm(ps[2][:], uTb[:, g, :], v3(BTim[:, js, :], "p j q -> p (j q)"), start=True, stop=True)
            P.copy(bure[:], v3(ps[1][:], "p (j q) -> p j q", j=4), e="act")
            P.copy(buim[:], v3(ps[2][:], "p (j q) -> p j q", j=4), e="act")

        def front2(u):
            c, g = divmod(u, 4)
            js = slice(4 * g, 4 * g + 4)
            zr, zi = zbank[u % 2]
            P.tt(t1[:], Epre[:, js, :], bure[:], ALU.mult)
            P.tt(t2[:], Epim[:, js, :], buim[:], ALU.mult)
            P.tt(Wre[:], t1[:], t2[:], ALU.subtract)
            P.tt(t3[:], Epre[:, js, :], buim[:], ALU.mult)
            P.tt(t4[:], Epim[:, js, :], bure[:], ALU.mult, e="pool")
            P.tt(Wim[:], t3[:], t4[:], ALU.add, e="pool")
            for jj in range(4):
                P.mm(zr[:, jj * 128:(jj + 1) * 128], Wre[:, jj, :], k.mask01b[:], start=True, stop=True)
            for jj in range(4):
                P.mm(zi[:, jj * 128:(jj + 1) * 128], Wim[:, jj, :], k.mask01b[:], start=True, stop=True)


        def front(u):
            front1(u)
            front2(u)

        def back(u):
            c, g = divmod(u, 4)
            tsl = slice(c * 128, (c + 1) * 128)
            uT32 = uT32s[c % 2]
            js = slice(4 * g, 4 * g + 4)
            zr, zi = zbank[u % 2]
            for jj in range(4):
                j = 4 * g + jj
                P.act(Zre[:, jj, :], zr[:, jj * 128:(jj + 1) * 128], AF.Identity, bias=c_re[:, j:j + 1])
            for jj in range(4):
                j = 4 * g + jj
                P.act(Zim[:, jj, :], zi[:, jj * 128:(jj + 1) * 128], AF.Identity, bias=c_im[:, j:j + 1])
            P.tt(t1[:], ETre[:, js, :], Zre[:], ALU.mult)
            P.tt(t2[:], ETim[:, js, :], Zim[:], ALU.mult)
            P.tt(Xre[:], t1[:], t2[:], ALU.subtract)
            P.tt(t3[:], ETim[:, js, :], Zre[:], ALU.mult)
            P.tt(t4[:], ETre[:, js, :], Zim[:], ALU.mult, e="pool")
            P.tt(Xim[:], t3[:], t4[:], ALU.add, e="pool")
            P.tt(xe_re[:, js], t1[:, :, 127], t2[:, :, 127], ALU.subtract)
            P.tt(xe_im[:, js], t3[:, :, 127], t4[:, :, 127], ALU.add, e="pool")
            yo = ps[7][:, g * 128:(g + 1) * 128]
            for jj in range(4):
                P.mm(yo, CTre[:, 4 * g + jj, :], Xre[:, jj, :], start=(jj == 0), stop=False)
                P.mm(yo, CTimn[:, 4 * g + jj, :], Xim[:, jj, :], start=False, stop=(jj == 3))
            if g != 3:
                return
            P.tt(s1[:], lam_re[:], xe_re[:], ALU.mult)
            P.tt(s2[:], lam_im[:], xe_im[:], ALU.mult)
            P.tt(c_re[:], s1[:], s2[:], ALU.subtract)
            P.tt(s1[:], lam_re[:], xe_im[:], ALU.mult)
            P.tt(s2[:], lam_im[:], xe_re[:], ALU.mult)
            P.tt(c_im[:], s1[:], s2[:], ALU.add)
            for kt in range(4):
                P.stt(ysk[:, kt, :], uT32[:, kt, :], dvec[:, kt:kt + 1], ps[7][:, kt * 128:(kt + 1) * 128], ALU.mult, ALU.add)
            P.tt(t3[:], ysk[:], ysk[:], ALU.mult, e="pool")
            P.ts(t3[:], t3[:], 0.044715, 1.0, ALU.mult, ALU.add, e="pool")
            P.tt(t3[:], t3[:], ysk[:], ALU.mult, e="pool")
            P.act(t4[:], t3[:], AF.Sigmoid, scale=2.0 * math.sqrt(2.0 / math.pi))
            P.tt(t3[:], ysk[:], t4[:], ALU.mult)
            P.copy(ygb[:], t3[:], e="act")
            for kp in range(4):
                for kt in range(4):
                    P.mm(ps[0][:, kp * 128:(kp + 1) * 128], Wglu[:, kt, kp * 128:(kp + 1) * 128], ygb[:, kt, :],
                         start=(kt == 0), stop=(kt == 3))
            for kp in range(4):
                P.act(t4[:, kp, :], ps[0][:, kp * 128:(kp + 1) * 128], AF.Sigmoid, bias=bglu[:, kp:kp + 1])
            P.tt(k.yT[1][:, :, tsl], t3[:], t4[:], ALU.mult)

        nu = 4 * NCHUNK
        front(0)
        for u in range(nu):
            if S5_ORDER == 1:
                if u + 1 < nu:
                    front1(u + 1)
                back(u)
                if u + 1 < nu:
                    front2(u + 1)
            else:
                if u + 1 < nu:
                    front(u + 1)
                back(u)


def layer_norm_tile(P, k, xv, g, b, wk):
    st, mv, rs, tmp = wk["st"], wk["mv"], wk["rs"], wk["tmp"]
    for hf in range(2):
        P.op("dve", lambda gg, hf=hf: gg.bn_stats(st[:, hf, :].ap, xv[:, hf * 512:(hf + 1) * 512].ap), reads=[xv], writes=[st[:]])
    P.op("dve", lambda gg: gg.bn_aggr(mv[:].ap, v3(st[:], "p a b -> p (a b)").ap), reads=[st[:]], writes=[mv[:]])
    rstd_from_var(P, rs[:], mv[:, 1:2], LN_EPS, tmp[:])
    P.ts(xv, xv, mv[:, 0:1], rs[:, 0:1], ALU.subtract, ALU.mult)
    P.tt(xv, xv, g[:], ALU.mult, e="pool")
    P.tt(xv, xv, b[:], ALU.add, e="pool")


def ln_work(P, pfx):
    return dict(st=Tile(P, pfx + "st", [128, 2, 6], F32), mv=Tile(P, pfx + "mv", [128, 2], F32),
                rs=Tile(P, pfx + "rs", [128, 1], F32), tmp=Tile(P, pfx + "tmp", [128, 1], F32))


def merge_phase(P, k, L, w, moe):
    ps = k.ps
    go = OFF['gate']
    with Scope(P):
        mixedT = Tile(P, "mixedT", [128, 8, SEQ], BF16)
        with Scope(P):
            wgt = [Tile(P, f"wgt{i}", [128, 8, 3, 128], BF16) for i in range(2)]
            wup = [Tile(P, f"wup{i}", [128, 3, 4, 128], BF16) for i in range(2)]
            bnat = Tile(P, "bg_nat", [24, 128], F32)
            bgate = Tile(P, "bgate", [128, 24], F32)
            sig = [Tile(P, f"mg_sig{i}", [128, 512], F32) for i in range(2)]
            acc = Tile(P, "mg_acc", [128, 512], F32)
            tmp = [Tile(P, "mg_tmp0", [128, 512], F32)] * 2
            P.dma("sp", bnat[:], D(w['b_in'][L, go:go + 3072].rearrange("(r p) -> r p", p=128)), bnat[:])
            P.tr(ps[7][:, 0:24], bnat[:], k.ident[0:24, 0:24])
            P.copy(bgate[:], ps[7][:, 0:24])
            it = 0
            for dti in range(8):
                slot = dti % 2
                for b in range(3):
                    c0 = go + b * 1024 + dti * 128
                    P.dma("pool", wgt[slot][:, :, b, :], D(w['w_in'][L, :, c0:c0 + 128].rearrange("(kt p) n -> p kt n", p=128)), wgt[slot][:])
                    P.dma("pool", wup[slot][:, b, :, :], D(w['w_up'][L, b, :, dti * 128:(dti + 1) * 128].rearrange("(kt p) n -> p kt n", p=128)), wup[slot][:])
                for tb in range(4):
                    tsl = slice(tb * 512, (tb + 1) * 512)
                    for b in range(3):
                        pg, pu = ps[2 * (it % 2)], ps[2 * (it % 2) + 1]
                        for kt in range(8):
                            P.mm(pg[:], wgt[slot][:, kt, b, :], k.xT[:, kt, tsl], start=(kt == 0), stop=(kt == 7))
                        for kt in range(4):
                            P.mm(pu[:], wup[slot][:, b, kt, :], k.yT[b][:, kt, tsl], start=(kt == 0), stop=(kt == 3))
                        sg = sig[it % 2]
                        P.act(sg[:], pg[:], AF.Sigmoid, bias=bgate[:, b * 8 + dti:b * 8 + dti + 1])
                        if b == 0:
                            P.tt(acc[:], sg[:], pu[:], ALU.mult)
                        elif b == 1:
                            P.tt(tmp[0][:], sg[:], pu[:], ALU.mult)
                            P.tt(acc[:], acc[:], tmp[0][:], ALU.add, e="pool")
                        else:
                            P.tt(tmp[1][:], sg[:], pu[:], ALU.mult)
                            P.tt(mixedT[:, dti, tsl], acc[:], tmp[1][:], ALU.add, e="pool")
                        it += 1
        if MERGE_STOP == 1:
            return
        with Scope(P):
            Wo = Tile(P, "Wo", [128, 8, 1024], BF16)
            P.dma("pool", Wo[:], D(w['w_o'][L].rearrange("(kt p) n -> p kt n", p=128)), Wo[:])
            for tt in range(NT):
                xv = k.xres.sub(tt)
                tsl = slice(tt * 128, (tt + 1) * 128)
                for hf in range(2):
                    po = ps[2 + (2 * tt + hf) % 4]
                    for dti in range(8):
                        P.mm(po[:], mixedT[:, dti, tsl], Wo[:, dti, hf * 512:(hf + 1) * 512], start=(dti == 0), stop=(dti == 7))
                    P.stt(xv[:, hf * 512:(hf + 1) * 512], xv[:, hf * 512:(hf + 1) * 512], DN_ALPHA, po[:], ALU.mult, ALU.add)
        with Scope(P):
            g1 = Tile(P, "ln1g", [128, 1024], F32)
            b1 = Tile(P, "ln1b", [128, 1024], F32)
            wks = [ln_work(P, "ln1a_"), ln_work(P, "ln1b_")]
            P.dma("sp", g1[:], D(w['ln1_g'][L].partition_broadcast(128)), g1[:])
            P.dma("sp", b1[:], D(w['ln1_b'][L].partition_broadcast(128)), b1[:])
            if moe:
                Wr = Tile(P, "Wr", [128, 8, 8], F32)
                br = Tile(P, "br", [1, 8], F32)
                xTf = Tile(P, "xTf", [128, 8, 128], F32)
                rt = Tile(P, "rt", [128, 6, 8], F32)
                P.dma("sp", Wr[:], D(w['moe_router'][0].rearrange("(kt p) n -> p kt n", p=128)), Wr[:])
                P.dma("sp", br[:], D(w['moe_router_b'][0:1, :]), br[:])
            for tt in range(NT):
                xv = k.xres.sub(tt)
                layer_norm_tile(P, k, xv, g1, b1, wks[tt % 2])
                make_xT(P, k, tt, xtf=(xTf if moe else None), banks=((0, 1) if tt % 2 == 0 else (2, 3)))
                if moe:
                    lg = ps[4][:, 0:8]
                    P.mm(lg, k.ones[0:1, 0:128], br[:], start=True, stop=False)
                    for kt in range(8):
                        P.mm(lg, xTf[:, kt, :], Wr[:, kt, :], start=False, stop=(kt == 7))
                    P.copy(rt[:, 0, :], lg)
                    P.op("dve", lambda g_: g_.max(rt[:, 1, :].ap, rt[:, 0, :].ap), reads=[rt[:]], writes=[rt[:]])
                    P.ts(rt[:, 2, :], rt[:, 0, :], rt[:, 1, 1:2], None, ALU.is_ge)
                    P.ts(rt[:, 5, 0:1], rt[:, 1, 0:1], -1.0, None, ALU.mult)
                    P.act(rt[:, 3, :], rt[:, 0, :], AF.Exp, bias=rt[:, 5, 0:1])
                    P.tt(rt[:, 4, :], rt[:, 3, :], rt[:, 2, :], ALU.mult)
                    P.reduce(rt[:, 5, 1:2], rt[:, 4, :], ALU.add)
                    P.recip(rt[:, 5, 2:3], rt[:, 5, 1:2])
                    P.ts(k.comb[:, tt, :], rt[:, 4, :], rt[:, 5, 2:3], None, ALU.mult)


def ffn_phase(P, k, L, w, moe, out_dram, last):
    ps = k.ps
    with Scope(P):
        wg = [Tile(P, f"f_wg{i}", [128, 8, 512], BF16) for i in range(2)]
        wu = [Tile(P, f"f_wu{i}", [128, 8, 512], BF16) for i in range(2)]
        wd = [Tile(P, f"f_wd{i}", [128, 4, 1024], BF16) for i in range(2)]
        if moe:
            experts = [(w['moe_wg'][0, e], w['moe_wu'][0, e], w['moe_wd'][0, e], 3584, e) for e in range(N_EXP)]
        else:
            experts = [(w['ffn_wg'][0], w['ffn_wu'][0], w['ffn_wd'][0], 2816, None)]

        g2 = Tile(P, "ln2g", [128, 1024], F32)
        b2 = Tile(P, "ln2b", [128, 1024], F32)
        P.dma("sp", g2[:], D(w['ln2_g'][L].partition_broadcast(128)), g2[:])
        P.dma("sp", b2[:], D(w['ln2_b'][L].partition_broadcast(128)), b2[:])

        def issue_block(slot, Wg, Wu_, Wd, f0t, nf):
            cs = slice(f0t * 128, (f0t + nf) * 128)
            P.dma("pool", wg[slot][:, :, 0:nf * 128], D(Wg[:, cs].rearrange("(kt p) n -> p kt n", p=128)), wg[slot][:])
            P.dma("pool", wu[slot][:, :, 0:nf * 128], D(Wu_[:, cs].rearrange("(kt p) n -> p kt n", p=128)), wu[slot][:])
            P.dma("pool", wd[slot][:, 0:nf, :], D(Wd[cs, :].rearrange("(ft p) n -> p ft n", p=128)), wd[slot][:])

        with Scope(P):
            Wpg = Tile(P, "Wpg", [128, 8, 1024], BF16)
            Wpp = Tile(P, "Wpp", [128, 2, 1024], BF16)
            pt = [Tile(P, f"pt{i}", [128, 256], F32) for i in range(2)]
            pTb = Tile(P, "pTb", [128, 2, 128], BF16)
            sg = Tile(P, "ple_sg", [128, 1024], F32)
            P.dma("pool", Wpg[:], D(w['ple_w_gate'][L].rearrange("(kt p) n -> p kt n", p=128)), Wpg[:])
            P.dma("pool", Wpp[:], D(w['ple_w_proj'][L].rearrange("(kt p) n -> p kt n", p=128)), Wpp[:])
            issue_block(0, experts[0][0], experts[0][1], experts[0][2], 0, min(4, experts[0][3] // 128))
            for tt in range(NT):
                xv = k.xres.sub(tt)
                tsl = slice(tt * 128, (tt + 1) * 128)
                ptt = pt[tt % 2]
                P.dma("sp", ptt[:], D(w['p'][L, tt * 128:(tt + 1) * 128, :]), ptt[:])
                for q in range(2):
                    P.tr(ps[6][:, q * 128:(q + 1) * 128], ptt[:, q * 128:(q + 1) * 128], k.ident[:])
                P.copy(pTb[:], v3(ps[6][:, 0:256], "p (q t) -> p q t", q=2), e="act")
                for hf in range(2):
                    hs = slice(hf * 512, (hf + 1) * 512)
                    pg, pp = ps[2 + hf], ps[4 + hf]
                    for kt in range(8):
                        P.mm(pg[:], k.xT[:, kt, tsl], Wpg[:, kt, hs], start=(kt == 0), stop=(kt == 7))
                    for kt in range(2):
                        P.mm(pp[:], pTb[:, kt, :], Wpp[:, kt, hs], start=(kt == 0), stop=(kt == 1))
                    P.act(sg[:, hs], pg[:], AF.Sigmoid)
                    P.tt(sg[:, hs], sg[:, hs], pp[:], ALU.mult)
                    P.stt(xv[:, hs], xv[:, hs], DN_ALPHA, sg[:, hs], ALU.mult, ALU.add)
        if FFN_STOP == 1:
            return
        with Scope(P):
            actT = [Tile(P, f"f_act{i}", [128, 4, 512], BF16) for i in range(2)]
            sgl = [Tile(P, f"f_sg{i}", [128, 512], F32) for i in range(2)]
            cnt = 0
            it = 0
            ia = 0
            io = 0
            for (Wg, Wu_, Wd, dff, e) in experts:
                nft = dff // 128
                f0t = 0
                while f0t < nft:
                    nf = min(4, nft - f0t)
                    slot = cnt % 2
                    if cnt > 0:
                        issue_block(slot, Wg, Wu_, Wd, f0t, nf)
                    for tb in range(4):
                        tsl = slice(tb * 512, (tb + 1) * 512)
                        at = actT[ia % 2]
                        ia += 1
                        for ft in range(nf):
                            pg, pu = ps[it % 2], ps[2 + it % 2]
                            fs = slice(ft * 128, (ft + 1) * 128)
                            for kt in range(8):
                                P.mm(pg[:], wg[slot][:, kt, fs], k.xT[:, kt, tsl], start=(kt == 0), stop=(kt == 7))
                            for kt in range(8):
                                P.mm(pu[:], wu[slot][:, kt, fs], k.xT[:, kt, tsl], start=(kt == 0), stop=(kt == 7))
                            P.act(sgl[it % 2][:], pg[:], AF.Silu)
                            P.tt(at[:, ft, :], sgl[it % 2][:], pu[:], ALU.mult)
                            it += 1
                        for t4 in range(4):
                            tt = tb * 4 + t4
                            xv = k.xres.sub(tt)
                            for hf in range(2):
                                hs = slice(hf * 512, (hf + 1) * 512)
                                po = ps[4 + io % 4]
                                io += 1
                                for ft in range(nf):
                                    P.mm(po[:], at[:, ft, t4 * 128:(t4 + 1) * 128], wd[slot][:, ft, hs], start=(ft == 0), stop=(ft == nf - 1))
                                if e is None:
                                    P.tt(xv[:, hs], xv[:, hs], po[:], ALU.add)
                                else:
                                    P.stt(xv[:, hs], po[:], k.comb[:, tt, e:e + 1], xv[:, hs], ALU.mult, ALU.add)
                    f0t += nf
                    cnt += 1
        if FFN_STOP == 2:
            return
        with Scope(P):
            wks = [ln_work(P, "ln2a_"), ln_work(P, "ln2b_")]
            for tt in range(NT):
                xv = k.xres.sub(tt)
                layer_norm_tile(P, k, xv, g2, b2, wks[tt % 2])
                if last:
                    P.dma("sp", D(out_dram[tt * 128:(tt + 1) * 128, :]), xv, xv)
                else:
                    make_xT(P, k, tt, banks=((0, 1) if tt % 2 == 0 else (2, 3)))


N_EXP = int(os.environ.get('N_EXP', '8'))
MERGE_STOP = int(os.environ.get('MERGE_STOP', '0'))
FFN_STOP = int(os.environ.get('FFN_STOP', '0'))
N_LAYERS = int(os.environ.get('N_LAYERS', '2'))
DUMP = os.environ.get('DUMP', '')


def dump_xres(P, k, out_dram):
    for tt in range(NT):
        xv = k.xres.sub(tt)
        P.dma("sp", D(out_dram[tt * 128:(tt + 1) * 128, :]), xv, xv)


def build(n_layers=None, debug=None):
    n_layers = N_LAYERS if n_layers is None else n_layers
    nc = bass.Bass("TRN2", target_bir_lowering=False)
    w = {}
    for name, shp in INPUT_SHAPES.items():
        w[name] = nc.dram_tensor(name, list(shp), F32, kind="ExternalInput").ap()
    out = nc.dram_tensor("out", [SEQ, DM], F32, kind="ExternalOutput").ap()
    dbg = None
    if debug == "yT":
        dbg = nc.dram_tensor("dbg", [128, 3 * 4 * SEQ], BF16, kind="ExternalOutput").ap()
    with ExitStack() as es:
        P = Prog(nc, es)
        P.mute = MUTE
        k = K()
        setup_consts(P, k)
        k.xres = Tile(P, "xres", [128, NT, DM], F32, nreg=NT)
        k.xT = Tile(P, "xT", [128, 8, SEQ], BF16, nreg=NT)
        k.comb = Tile(P, "comb", [128, NT, 8], F32)
        load_x(P, k, w['x'])
        stop = False
        for L in range(n_layers):
            moe = (L % 2 == 1)
            with Scope(P):
                k.yT = {}
                k.yT[1] = Tile(P, "yT_s5", [128, 4, SEQ], BF16)
                if debug == 'yT':
                    P.memset(k.yT[1][:], 0.0)
                if 's5' in PHASES:
                    s5_phase(P, k, L, w)
                    MUTE[0] = False
                k.yT[2] = Tile(P, "yT_ml", [128, 4, SEQ], BF16)
                if debug == 'yT':
                    P.memset(k.yT[2][:], 0.0)
                if 'ml' in PHASES:
                    mlstm_phase(P, k, L, w)
                k.yT[0] = Tile(P, "yT_gla", [128, 4, SEQ], BF16)
                if debug == 'yT':
                    P.memset(k.yT[0][:], 0.0)
                if 'gla' in PHASES:
                    gla_phase(P, k, L, w)
                if debug == "yT":
                    for b in range(3):
                        P.dma("sp", D(dbg[:, b * 4 * SEQ:(b + 1) * 4 * SEQ]),
                              v3(k.yT[b][:], "p a t -> p (a t)"), k.yT[b][:])
                    stop = True
                else:
                    merge_phase(P, k, L, w, moe)
            if stop:
                break
            if DUMP == f"{L}:ln1":
                dump_xres(P, k, out)
                break
            ffn_phase(P, k, L, w, moe, out, last=(L == n_layers - 1))
            if DUMP == f"{L}:ln2" and L != n_layers - 1:
                dump_xres(P, k, out)
                break
        P.finish()
        print("ops", P.n_ops, "waits", P.n_waits, {e: P.cnt[e] for e in ENG}, "dsems", P.n_dsem)
    return nc


NCHUNK = 16
PHASES = ['gla', 's5', 'ml']
S5_STOP = 99
GLA_STOP = 0
GLA_VAR = 0
S5_ORDER = 0
N_EXP = 8
MERGE_STOP = 0
FFN_STOP = 0
N_LAYERS = 2
DUMP = ''

def kernel(**inputs):
    nc = build()
    shared = {k_: np.ascontiguousarray(np.asarray(v, dtype=np.float32)) for k_, v in inputs.items() if k_ not in ("x", "p")}
    xs = np.asarray(inputs["x"], dtype=np.float32)
    ps_ = np.asarray(inputs["p"], dtype=np.float32)
    n = xs.shape[0]
    maps = []
    for b in range(n):
        m = dict(shared)
        m["x"] = np.ascontiguousarray(xs[b])
        m["p"] = np.ascontiguousarray(ps_[:, b])
        maps.append(m)
    res = run_bass_kernel_spmd(nc, maps, core_ids=list(range(n)))
    return np.stack([np.asarray(res.results[b]["out"]) for b in range(n)]).astype(np.float32)
```
